# Optimizing a Trainium2 kernel written in Bass

```python
import jax
import jax.numpy as jnp
from jax import lax
import numpy as np

D_MODEL = 2048
BATCH = 2
SEQ = 4096
DEPTH = 4

MIX_WIDTH = 2048
CONV_WIDTH = 1024
CONV_KERNEL = 31
MOBA_HEADS = 8
MOBA_HEAD_DIM = 128
MOBA_WIDTH = MOBA_HEADS * MOBA_HEAD_DIM
MOBA_BLOCK = 256
MOBA_TOPK = 3
MOBA_QCHUNK = 32
EVEN_IN = 3 * CONV_WIDTH + 4 * MOBA_WIDTH
DSA_HEADS = 16
DSA_HEAD_DIM = 128
DSA_WIDTH = DSA_HEADS * DSA_HEAD_DIM
Q_LORA = 512
KV_LORA = 256
IDX_HEADS = 16
IDX_DIM = 64
DSA_TOPK_MAX = 256
DSA_QCHUNK = 128
ODD_IN = Q_LORA + KV_LORA + IDX_DIM + IDX_HEADS + DSA_WIDTH
N_EVEN = (DEPTH + 1) // 2
N_ODD = DEPTH // 2
EPS = 1e-6

kernel_name = 'hybrid_conformer_moba_dsa_block'


def _rmsnorm(x, g):
    xf = x.astype(jnp.float32)
    y = xf * lax.rsqrt(jnp.mean(xf * xf, axis=-1, keepdims=True) + EPS)
    return (y * g.astype(jnp.float32)).astype(x.dtype)


def _layernorm(x, g, b):
    xf = x.astype(jnp.float32)
    mu = jnp.mean(xf, axis=-1, keepdims=True)
    xc = xf - mu
    y = xc * lax.rsqrt(jnp.mean(xc * xc, axis=-1, keepdims=True) + EPS)
    return (y * g.astype(jnp.float32) + b.astype(jnp.float32)).astype(x.dtype)


def _split_cols(z, sizes):
    out = []
    off = 0
    for s in sizes:
        out.append(z[..., off:off + s])
        off += s
    return out


def _conformer_conv(a_val, a_gt, w_dw, b_dw, ln_g, ln_b, w_pw, b_pw):
    u = a_val * jax.nn.sigmoid(a_gt)
    y = lax.conv_general_dilated(
        u, w_dw[:, None, :], window_strides=(1,), padding=[(CONV_KERNEL - 1, 0)],
        dimension_numbers=('NWC', 'WIO', 'NWC'), feature_group_count=CONV_WIDTH) + b_dw
    y = jax.nn.silu(_layernorm(y, ln_g, ln_b))
    return y @ w_pw + b_pw


def _moba_attention(q, k, v):
    b, t, h, dh = q.shape
    nb = -(-t // MOBA_BLOCK)
    pad = nb * MOBA_BLOCK - t
    q = q.transpose(0, 2, 1, 3)
    k = jnp.pad(k.transpose(0, 2, 1, 3), ((0, 0), (0, 0), (0, pad), (0, 0)))
    v = jnp.pad(v.transpose(0, 2, 1, 3), ((0, 0), (0, 0), (0, pad), (0, 0)))
    kb = k.reshape(b, h, nb, MOBA_BLOCK, dh)
    vb = v.reshape(b, h, nb, MOBA_BLOCK, dh)
    n_sel = min(MOBA_TOPK, nb - 1)
    scale = dh ** -0.5
    sel = None
    if n_sel > 0:
        k_mean = jnp.mean(kb, axis=3)
        gate = jnp.einsum('bhtd,bhnd->bhtn', q, k_mean)
        cur = jnp.arange(t) // MOBA_BLOCK
        fully_past = jnp.arange(nb)[None, :] < cur[:, None]
        gate = jnp.where(fully_past, gate, -jnp.inf)
        sel = lax.top_k(gate, n_sel)[1]
    bi = jnp.arange(b)[:, None, None, None]
    hi = jnp.arange(h)[None, :, None, None]

    def chunk(c):
        t0 = c * MOBA_QCHUNK
        blk = t0 // MOBA_BLOCK
        qc = lax.dynamic_slice_in_dim(q, t0, MOBA_QCHUNK, axis=2)
        k_own = lax.dynamic_index_in_dim(kb, blk, axis=2, keepdims=False)
        v_own = lax.dynamic_index_in_dim(vb, blk, axis=2, keepdims=False)
        qpos = t0 + jnp.arange(MOBA_QCHUNK)
        kpos = blk * MOBA_BLOCK + jnp.arange(MOBA_BLOCK)
        s_own = jnp.einsum('bhqd,bhsd->bhqs', qc, k_own) * scale
        s_own = jnp.where(kpos[None, :] <= qpos[:, None], s_own, -jnp.inf)
        if n_sel == 0:
            p = jax.nn.softmax(s_own.astype(jnp.float32), axis=-1).astype(v.dtype)
            return jnp.einsum('bhqs,bhsd->bhqd', p, v_own)
        sel_c = lax.dynamic_slice_in_dim(sel, t0, MOBA_QCHUNK, axis=2)
        k_sel = kb[bi, hi, sel_c]
        v_sel = vb[bi, hi, sel_c]
        s_past = jnp.einsum('bhqd,bhqksd->bhqks', qc, k_sel) * scale
        s_past = jnp.where((sel_c < blk)[..., None], s_past, -jnp.inf)
        logits = jnp.concatenate(
            [s_past.reshape(b, h, MOBA_QCHUNK, n_sel * MOBA_BLOCK), s_own], axis=-1)
        p = jax.nn.softmax(logits.astype(jnp.float32), axis=-1).astype(v.dtype)
        p_past = p[..., :n_sel * MOBA_BLOCK].reshape(b, h, MOBA_QCHUNK, n_sel, MOBA_BLOCK)
        p_own = p[..., n_sel * MOBA_BLOCK:]
        return (jnp.einsum('bhqks,bhqksd->bhqd', p_past, v_sel)
                + jnp.einsum('bhqs,bhsd->bhqd', p_own, v_own))

    out = lax.map(chunk, jnp.arange(t // MOBA_QCHUNK))
    return out.transpose(1, 0, 3, 2, 4).reshape(b, t, h * dh)


def _dsa_attention(q_lat, ckv, iq, ik, iw):
    b, t, h, c = q_lat.shape
    k_top = min(DSA_TOPK_MAX, t // 4)
    scale = DSA_HEAD_DIM ** -0.5
    bi = jnp.arange(b)[:, None, None]
    kpos = jnp.arange(t)

    def chunk(ci):
        t0 = ci * DSA_QCHUNK
        qpos = t0 + jnp.arange(DSA_QCHUNK)
        iq_c = lax.dynamic_slice_in_dim(iq, t0, DSA_QCHUNK, axis=1)
        iw_c = lax.dynamic_slice_in_dim(iw, t0, DSA_QCHUNK, axis=1)
        ql_c = lax.dynamic_slice_in_dim(q_lat, t0, DSA_QCHUNK, axis=1)
        idx = jnp.einsum('bqhd,bsd->bqhs', iq_c, ik)
        score = jnp.einsum('bqhs,bqh->bqs', jax.nn.relu(idx), iw_c)
        score = jnp.where(kpos[None, :] <= qpos[:, None], score, -jnp.inf)
        sel = lax.top_k(score, k_top)[1]
        kv = ckv[bi, sel]
        s = jnp.einsum('bqhc,bqkc->bqhk', ql_c, kv) * scale
        s = jnp.where((sel <= qpos[None, :, None])[:, :, None, :], s, -jnp.inf)
        p = jax.nn.softmax(s.astype(jnp.float32), axis=-1).astype(kv.dtype)
        return jnp.einsum('bqhk,bqkc->bqhc', p, kv)

    out = lax.map(chunk, jnp.arange(t // DSA_QCHUNK))
    return out.transpose(1, 0, 2, 3, 4).reshape(b, t, h, c)


def _even_layer(x, norm_g, w_in, b_glu, w_dw, b_dw, ln_g, ln_b, w_pw, b_pw, w_out):
    b, t, _ = x.shape
    z = _rmsnorm(x, norm_g) @ w_in
    a_val, a_gt, a_gate, q, k, v, b_gate = _split_cols(
        z, [CONV_WIDTH, CONV_WIDTH, CONV_WIDTH, MOBA_WIDTH, MOBA_WIDTH, MOBA_WIDTH, MOBA_WIDTH])
    ya = _conformer_conv(a_val + b_glu[:CONV_WIDTH], a_gt + b_glu[CONV_WIDTH:],
                         w_dw, b_dw, ln_g, ln_b, w_pw, b_pw) * jax.nn.silu(a_gate)
    shp = (b, t, MOBA_HEADS, MOBA_HEAD_DIM)
    yb = _moba_attention(q.reshape(shp), k.reshape(shp), v.reshape(shp)) * jax.nn.silu(b_gate)
    return jnp.concatenate([ya, yb], axis=-1) @ w_out


def _odd_layer(x, norm_g, w_in, q_norm, w_qb, kv_norm, w_uk, w_uv, w_iq, ik_g, ik_b, w_out):
    b, t, _ = x.shape
    z = _rmsnorm(x, norm_g) @ w_in
    cq, ckv, ik, iw, gate = _split_cols(z, [Q_LORA, KV_LORA, IDX_DIM, IDX_HEADS, DSA_WIDTH])
    cq = _rmsnorm(cq, q_norm)
    q = (cq @ w_qb).reshape(b, t, DSA_HEADS, DSA_HEAD_DIM)
    q_lat = jnp.einsum('bthd,hdc->bthc', q, w_uk)
    ckv = _rmsnorm(ckv, kv_norm)
    iq = (cq @ w_iq).reshape(b, t, IDX_HEADS, IDX_DIM)
    ik = _layernorm(ik, ik_g, ik_b)
    iw = iw * (IDX_HEADS ** -0.5 * IDX_DIM ** -0.5)
    o_lat = _dsa_attention(q_lat, ckv, iq, ik, iw)
    o = jnp.einsum('bthc,hcd->bthd', o_lat, w_uv).reshape(b, t, DSA_WIDTH)
    return (o * jax.nn.silu(gate)) @ w_out


def setup_inputs(seed: int = 0) -> dict:
    key = jax.random.key(seed)
    ks = jax.random.split(key, 24)
    f32 = jnp.float32
    out_mult = (2.0 * DEPTH) ** -0.5

    def w(k, shape, fan_in, mult=1.0):
        return jax.random.normal(k, shape, f32) * (mult * fan_in ** -0.5)

    def gain(k, shape):
        return 1.0 + 0.02 * jax.random.normal(k, shape, f32)

    def bias(k, shape):
        return 0.02 * jax.random.normal(k, shape, f32)

    return {
        'x': jax.random.normal(ks[0], (BATCH, SEQ, D_MODEL), f32),
        'even_norm': gain(ks[1], (N_EVEN, D_MODEL)),
        'even_w_in': w(ks[2], (N_EVEN, D_MODEL, EVEN_IN), D_MODEL),
        'even_b_glu': bias(ks[3], (N_EVEN, 2 * CONV_WIDTH)),
        'even_w_dw': w(ks[4], (N_EVEN, CONV_KERNEL, CONV_WIDTH), CONV_KERNEL),
        'even_b_dw': bias(ks[5], (N_EVEN, CONV_WIDTH)),
        'even_conv_ln_g': gain(ks[6], (N_EVEN, CONV_WIDTH)),
        'even_conv_ln_b': bias(ks[7], (N_EVEN, CONV_WIDTH)),
        'even_w_pw': w(ks[8], (N_EVEN, CONV_WIDTH, CONV_WIDTH), CONV_WIDTH),
        'even_b_pw': bias(ks[9], (N_EVEN, CONV_WIDTH)),
        'even_w_out': w(ks[10], (N_EVEN, MIX_WIDTH, D_MODEL), MIX_WIDTH, out_mult),
        'odd_norm': gain(ks[11], (N_ODD, D_MODEL)),
        'odd_w_in': w(ks[12], (N_ODD, D_MODEL, ODD_IN), D_MODEL),
        'odd_q_norm': gain(ks[13], (N_ODD, Q_LORA)),
        'odd_w_qb': w(ks[14], (N_ODD, Q_LORA, DSA_WIDTH), Q_LORA),
        'odd_kv_norm': gain(ks[15], (N_ODD, KV_LORA)),
        'odd_w_uk': w(ks[16], (N_ODD, DSA_HEADS, DSA_HEAD_DIM, KV_LORA), DSA_HEAD_DIM),
        'odd_w_uv': w(ks[17], (N_ODD, DSA_HEADS, KV_LORA, DSA_HEAD_DIM), KV_LORA),
        'odd_w_iq': w(ks[18], (N_ODD, Q_LORA, IDX_HEADS * IDX_DIM), Q_LORA),
        'odd_ik_ln_g': gain(ks[19], (N_ODD, IDX_DIM)),
        'odd_ik_ln_b': bias(ks[20], (N_ODD, IDX_DIM)),
        'odd_w_out': w(ks[21], (N_ODD, DSA_WIDTH, D_MODEL), DSA_WIDTH, out_mult),
        'final_norm': gain(ks[22], (D_MODEL,)),
    }


def reference(x, even_norm, even_w_in, even_b_glu, even_w_dw, even_b_dw, even_conv_ln_g,
              even_conv_ln_b, even_w_pw, even_b_pw, even_w_out, odd_norm, odd_w_in, odd_q_norm,
              odd_w_qb, odd_kv_norm, odd_w_uk, odd_w_uv, odd_w_iq, odd_ik_ln_g, odd_ik_ln_b,
              odd_w_out, final_norm):
    for layer in range(DEPTH):
        i = layer // 2
        if layer % 2 == 0:
            x = x + _even_layer(x, even_norm[i], even_w_in[i], even_b_glu[i], even_w_dw[i],
                                even_b_dw[i], even_conv_ln_g[i], even_conv_ln_b[i],
                                even_w_pw[i], even_b_pw[i], even_w_out[i])
        else:
            x = x + _odd_layer(x, odd_norm[i], odd_w_in[i], odd_q_norm[i], odd_w_qb[i],
                               odd_kv_norm[i], odd_w_uk[i], odd_w_uv[i], odd_w_iq[i],
                               odd_ik_ln_g[i], odd_ik_ln_b[i], odd_w_out[i])
    return _rmsnorm(x, final_norm)
```

```python
import contextlib
import numpy as np
import concourse.bass as bass
import concourse.mybir as mybir
from concourse.bass_utils import run_bass_kernel_spmd

F32 = mybir.dt.float32
BF16 = mybir.dt.bfloat16
AF = mybir.ActivationFunctionType
ALU = mybir.AluOpType
AX = mybir.AxisListType

COMPUTE = ("pe", "act", "dve", "pool")
ND = 12


class Prog:
    def __init__(self, nc):
        self.nc = nc
        self.ops = {e: [] for e in ("pe", "act", "dve", "pool", "sp")}
        self.last_w = {}
        self.readers = {}
        self.dmas_since_bar = []

    def op(self, eng, fn, r=(), w=(), dma=False):
        idx = len(self.ops[eng])
        me = (eng, idx)
        deps = set()
        for k in r:
            if k in self.last_w:
                deps.add(self.last_w[k])
        for k in w:
            if k in self.last_w:
                deps.add(self.last_w[k])
            rd = self.readers.get(k)
            if rd:
                for d in rd[0].items():
                    deps.add(d)
                for d in rd[1]:
                    deps.add(d)
        if eng == "pe" and not dma:
            deps = {d for d in deps if d[0] != "pe"}
        deps.discard(me)
        self.ops[eng].append(dict(fn=fn, deps=deps, dma=dma))
        for k in r:
            rd = self.readers.setdefault(k, ({}, []))
            if dma:
                rd[1].append(me)
            else:
                rd[0][eng] = idx
        for k in w:
            self.last_w[k] = me
            self.readers[k] = ({}, [])
        if dma:
            self.dmas_since_bar.append(me)
        return me

    def barrier(self):
        deps = set()
        for e in self.ops:
            for i in range(len(self.ops[e]) - 1, -1, -1):
                if not self.ops[e][i]["dma"] and self.ops[e][i]["fn"] is not None:
                    deps.add((e, i))
                    break
        deps.update(self.dmas_since_bar)
        for e in self.ops:
            self.ops[e].append(dict(fn=None, deps=set(deps), dma=False))
        self.last_w = {}
        self.readers = {}
        self.dmas_since_bar = []

    def dma(self, out, in_, r=(), w=(), q="sp"):
        return self.op(q, lambda e: e.dma_start(out=out, in_=in_), r=r, w=w, dma=True)

    def emit(self):
        nc = self.nc
        flagged = set()
        for e in self.ops:
            for o in self.ops[e]:
                flagged.update(o["deps"])
        with contextlib.ExitStack() as es:
            sems = {e: es.enter_context(nc.semaphore("s_" + e)) for e in COMPUTE}
            dsems = {q: [es.enter_context(nc.semaphore("d_%s%d" % (q, i))) for i in range(ND)]
                     for q in ("sp", "pool", "act")}
            sig = {}
            for e in self.ops:
                cnt = 0
                j = 0
                for i, o in enumerate(self.ops[e]):
                    if o["dma"]:
                        s = dsems[e][j % ND]
                        sig[(e, i)] = (s, 16 * (j // ND + 1))
                        o["pre"] = (s, 16 * (j // ND)) if j >= ND else None
                        j += 1
                    elif (e, i) in flagged:
                        cnt += 1
                        sig[(e, i)] = (sems[e], cnt)

            def run(ename, eng):
                known = {}
                for i, o in enumerate(self.ops[ename]):
                    waits = {}
                    if o.get("pre"):
                        s, v = o["pre"]
                        waits[s] = (s, v)
                    for d in o["deps"]:
                        s, v = sig[d]
                        key = id(s)
                        if known.get(key, 0) >= v:
                            continue
                        if key not in waits or waits[key][1] < v:
                            waits[key] = (s, v)
                    for key, (s, v) in waits.items():
                        eng.wait_ge(s, v)
                        known[key] = v
                    if o["fn"] is None:
                        continue
                    ins = o["fn"](eng)
                    if (ename, i) in sig:
                        s, v = sig[(ename, i)]
                        ins.then_inc(s, 16 if o["dma"] else 1)

            with nc.Block() as block:
                @block.tensor
                def _(e):
                    run("pe", e)

                @block.scalar
                def _(e):
                    run("act", e)

                @block.vector
                def _(e):
                    run("dve", e)

                @block.gpsimd
                def _(e):
                    run("pool", e)

                @block.sync
                def _(e):
                    run("sp", e)


import ml_dtypes
NPBF = ml_dtypes.bfloat16
EPS = 1e-6
NT = 8
TOK = 1024


class Ctx:
    def __init__(self):
        self.nc = bass.Bass("TRN2", target_bir_lowering=False)
        self.P = Prog(self.nc)
        self.es = contextlib.ExitStack()
        self.n = 0

    def din(self, name, shape, dt=F32):
        return self.nc.dram_tensor(name, list(shape), dt, kind="ExternalInput").ap()

    def dout(self, name, shape, dt=F32):
        return self.nc.dram_tensor(name, list(shape), dt, kind="ExternalOutput").ap()

    def sb(self, shape, dt, name=None):
        self.n += 1
        return self.es.enter_context(self.nc.sbuf_tensor(name or ("sb%d" % self.n), list(shape), dt))

    def ps(self, shape, dt, name=None):
        self.n += 1
        return self.es.enter_context(self.nc.psum_tensor(name or ("ps%d" % self.n), list(shape), dt))

    def finish(self):
        self.P.barrier()
        self.P.emit()
        self.es.close()
        return self.nc


def norm_transpose(C, x_d, gT, xnT, identb, nchunk, D):
    P = C.P
    xt = [C.sb([128, D], F32) for _ in range(2)]
    xn = [C.sb([128, D], BF16) for _ in range(2)]
    junk = C.sb([128, D], BF16)
    ss = C.sb([128, NT], F32)
    rs = C.sb([128, NT], F32)
    pT = [C.ps([128, 8, 128], BF16) for _ in range(2)]
    for t in range(NT):
        s = t % 2
        P.dma(xt[s][:], x_d[t * 128:(t + 1) * 128, :], w=[("xt", s)])
        P.op("act", lambda e, s=s, t=t: e.activation(out=junk[:], in_=xt[s][:], func=AF.Square, accum_out=ss[:, t:t + 1]),
             r=[("xt", s)], w=["junk", ("ss", t)])
        P.op("act", lambda e, t=t: e.activation(out=rs[:, t:t + 1], in_=ss[:, t:t + 1], func=AF.Sqrt, scale=1.0 / D, bias=EPS),
             r=[("ss", t)], w=[("rs", t)])
        P.op("dve", lambda e, t=t: e.reciprocal(out=rs[:, t:t + 1], in_=rs[:, t:t + 1]), r=[("rs", t)], w=[("rs", t)])
        P.op("dve", lambda e, s=s, t=t: e.tensor_scalar(out=xn[s][:], in0=xt[s][:], scalar1=rs[:, t:t + 1], scalar2=None, op0=ALU.mult),
             r=[("xt", s), ("rs", t)], w=[("xn", s)])
        for c0 in range(0, nchunk, 8):
            b = (c0 // 8) % 2
            for c in range(c0, min(c0 + 8, nchunk)):
                P.op("pe", lambda e, c=c, s=s, b=b: e.transpose(out=pT[b][:, c % 8, :], in_=xn[s][:, c * 128:(c + 1) * 128], identity=identb[:]),
                     r=[("xn", s), "identb"], w=[("pT", b)])
            for c in range(c0, min(c0 + 8, nchunk)):
                P.op("dve", lambda e, c=c, t=t, b=b: e.tensor_scalar(out=xnT[:, c, t * 128:(t + 1) * 128], in0=pT[b][:, c % 8, :],
                                                                  scalar1=gT[:, c:c + 1], scalar2=None, op0=ALU.mult),
                     r=[("pT", b), "gT"], w=[("xnT", t)])


def load_const(C, dram, shape, dt, key, q="sp"):
    t = C.sb(shape, dt)
    C.P.dma(t[:], dram, w=[key], q=q)
    return t


def build_E1():
    C = Ctx()
    P = C.P
    x_d = C.din("x", [TOK, 2048])
    gT_d = C.din("gT", [128, 16])
    w_d = C.din("w", [2048, 7168])
    bgT_d = C.din("bgT", [128, 16])
    id_d = C.din("ident", [128, 128])
    outs = {n: C.dout(n, [1024, 1024], BF16) for n in ("uT", "gaT", "qT", "kT", "v", "gb")}
    identf = load_const(C, id_d, [128, 128], F32, "identf")
    gT = load_const(C, gT_d, [128, 16], F32, "gT")
    bgT = load_const(C, bgT_d, [128, 16], F32, "bgT")
    identb = C.sb([128, 128], BF16)
    P.op("dve", lambda e: e.tensor_copy(out=identb[:], in_=identf[:]), r=["identf"], w=["identb"])
    xnT = C.sb([128, 16, TOK], BF16)
    norm_transpose(C, x_d, gT, xnT, identb, 16, 2048)
    XN = [("xnT", t) for t in range(NT)]
    wb = [C.sb([128, 16, 512], BF16) for _ in range(2)]
    psb = [C.ps([128, 512], F32) for _ in range(4)]
    ob = [C.sb([128, 512], BF16) for _ in range(4)]
    tv = [C.sb([128, 512], F32) for _ in range(2)]
    tg = [C.sb([128, 512], F32) for _ in range(2)]
    w_v = w_d.rearrange("(c p) n -> p c n", p=128)
    cnt = {"ps": 0, "ob": 0, "wb": 0, "tv": 0}

    def loadw(cols):
        s = cnt["wb"] % 2
        cnt["wb"] += 1
        off = 0
        for (c0, n) in cols:
            P.dma(wb[s][:, :, off:off + n], w_v[:, :, c0:c0 + n], w=[("wb", s)], q="pool")
            off += n
        return s

    def mm_fm(s, j, h):
        b = cnt["ps"] % 4
        cnt["ps"] += 1
        for c in range(16):
            P.op("pe", lambda e, c=c, b=b: e.matmul(psb[b][:], lhsT=wb[s][:, c, j * 128:(j + 1) * 128], rhs=xnT[:, c, h * 512:(h + 1) * 512],
                                                  start=(c == 0), stop=(c == 15)),
                 r=[("wb", s)] + XN, w=[("ps", b)])
        return b

    def mm_tm(s, t):
        b = cnt["ps"] % 4
        cnt["ps"] += 1
        for c in range(16):
            P.op("pe", lambda e, c=c, b=b: e.matmul(psb[b][:], lhsT=xnT[:, c, t * 128:(t + 1) * 128], rhs=wb[s][:, c, :],
                                                  start=(c == 0), stop=(c == 15)),
                 r=[("wb", s)] + XN, w=[("ps", b)])
        return b

    def store(dst, o):
        P.dma(dst, ob[o][:], r=[("ob", o)])

    def newob():
        o = cnt["ob"] % 4
        cnt["ob"] += 1
        return o

    for g in range(4):
        s = loadw([(g * 256, 256), (1024 + g * 256, 256)])
        for jj in range(2):
            ch = 2 * g + jj
            for h in range(2):
                k = cnt["tv"] % 2
                cnt["tv"] += 1
                b = mm_fm(s, jj, h)
                P.op("act", lambda e, b=b, k=k, ch=ch: e.activation(out=tv[k][:], in_=psb[b][:], func=AF.Identity, bias=bgT[:, ch:ch + 1]),
                     r=[("ps", b), "bgT"], w=[("tv", k)])
                b2 = mm_fm(s, 2 + jj, h)
                P.op("act", lambda e, b2=b2, k=k, ch=ch: e.activation(out=tg[k][:], in_=psb[b2][:], func=AF.Sigmoid, bias=bgT[:, 8 + ch:9 + ch]),
                     r=[("ps", b2), "bgT"], w=[("tg", k)])
                o = newob()
                P.op("dve", lambda e, o=o, k=k: e.tensor_tensor(out=ob[o][:], in0=tv[k][:], in1=tg[k][:], op=ALU.mult),
                     r=[("tv", k), ("tg", k)], w=[("ob", o)])
                store(outs["uT"][ch * 128:(ch + 1) * 128, h * 512:(h + 1) * 512], o)
    for name, col0, fn in (("gaT", 2048, AF.Silu), ("qT", 3072, AF.Copy), ("kT", 4096, AF.Copy)):
        for g in range(2):
            s = loadw([(col0 + g * 512, 512)])
            for j in range(4):
                ch = g * 4 + j
                for h in range(2):
                    b = mm_fm(s, j, h)
                    o = newob()
                    P.op("act", lambda e, b=b, o=o, fn=fn: e.activation(out=ob[o][:], in_=psb[b][:], func=fn),
                         r=[("ps", b)], w=[("ob", o)])
                    store(outs[name][ch * 128:(ch + 1) * 128, h * 512:(h + 1) * 512], o)
    for name, col0, fn in (("v", 5120, AF.Copy), ("gb", 6144, AF.Silu)):
        for g in range(2):
            s = loadw([(col0 + g * 512, 512)])
            for t in range(NT):
                b = mm_tm(s, t)
                o = newob()
                P.op("act", lambda e, b=b, o=o, fn=fn: e.activation(out=ob[o][:], in_=psb[b][:], func=fn),
                     r=[("ps", b)], w=[("ob", o)])
                store(outs[name][t * 128:(t + 1) * 128, g * 512:(g + 1) * 512], o)
    return C.finish()


def build_E2a():
    C = Ctx()
    P = C.P
    uT_d = C.din("uT", [1024, 1024], BF16)
    uh_d = C.din("uh", [1024, 4, 32], BF16)
    gaT_d = C.din("gaT", [1024, 1024], BF16)
    wdw_d = C.din("wdwT", [128, 8, 31])
    vec_d = C.din("vecs", [128, 4, 8])
    wpw_d = C.din("wpw", [1024, 1024])
    id_d = C.din("ident", [128, 128])
    ya_d = C.dout("yaT", [1024, 1024], BF16)
    identf = load_const(C, id_d, [128, 128], F32, "identf")
    wdw = load_const(C, wdw_d, [128, 8, 31], F32, "wdw")
    vecs = load_const(C, vec_d, [128, 4, 8], F32, "vecs")
    identb = C.sb([128, 128], BF16)
    onesf = C.sb([128, 128], F32)
    P.op("dve", lambda e: e.tensor_copy(out=identb[:], in_=identf[:]), r=["identf"], w=["identb"])
    P.op("pool", lambda e: e.memset(onesf[:], 1.0), w=["onesf"])
    ucv = C.sb([128, 8, 4, 288], BF16)
    gaT = C.sb([128, 8, 1024], BF16)
    for c in range(8):
        P.dma(ucv[:, c, :, 32:288], uT_d[c * 128:(c + 1) * 128, :].rearrange("p (i t) -> p i t", i=4), w=[("ucv", c)])
        P.dma(ucv[:, c, :, 0:32], uh_d[c * 128:(c + 1) * 128, :, :], w=[("ucv", c)])
        P.dma(gaT[:, c, :], gaT_d[c * 128:(c + 1) * 128, :], w=[("gaT", c)])
    D = [C.sb([128, 31, 128], BF16) for _ in range(2)]
    ycv = C.sb([128, 8, 1024], F32)
    ysq = C.sb([128, 8, 1024], F32)
    psb = [C.ps([128, 512], F32) for _ in range(6)]
    pc = [0]

    def bank():
        b = pc[0] % 6
        pc[0] += 1
        return b
    for c in range(8):
        s = c % 2
        for k in range(31):
            P.op("pool", lambda e, s=s, c=c, k=k: e.tensor_scalar(out=D[s][:, k, :], in0=identb[:], scalar1=wdw[:, c, k:k + 1], scalar2=0.0,
                                                                op0=ALU.mult, op1=ALU.add),
                 r=["identb", "wdw"], w=[("D", s)])
        for half in range(2):
            b = bank()
            for ii in range(2):
                i = half * 2 + ii
                for k in range(31):
                    P.op("pe", lambda e, s=s, c=c, k=k, i=i, ii=ii, b=b: e.matmul(psb[b][:, ii * 256:(ii + 1) * 256], lhsT=D[s][:, k, :],
                                                                             rhs=ucv[:, c, i, 2 + k:2 + k + 256], start=(k == 0), stop=(k == 30)),
                         r=[("D", s), ("ucv", c)], w=[("ps", b)])
            P.op("act", lambda e, c=c, half=half, b=b: e.activation(out=ycv[:, c, half * 512:(half + 1) * 512], in_=psb[b][:], func=AF.Identity,
                                                                  bias=vecs[:, 0, c:c + 1]),
                 r=[("ps", b), "vecs"], w=[("ycv", c, half)])
            P.op("act", lambda e, c=c, half=half, b=b: e.activation(out=ysq[:, c, half * 512:(half + 1) * 512], in_=psb[b][:], func=AF.Square,
                                                                  bias=vecs[:, 0, c:c + 1]),
                 r=[("ps", b), "vecs"], w=[("ysq", c, half)])
    mean = C.sb([128, 1024], F32)
    msq = C.sb([128, 1024], F32)
    rstd = C.sb([128, 1024], F32)
    for half in range(2):
        sl = slice(half * 512, (half + 1) * 512)
        b1 = bank()
        for c in range(8):
            P.op("pe", lambda e, c=c, b1=b1, sl=sl: e.matmul(psb[b1][:], lhsT=onesf[:], rhs=ycv[:, c, sl], start=(c == 0), stop=(c == 7)),
                 r=["onesf", ("ycv", c, half)], w=[("ps", b1)])
        b2 = bank()
        for c in range(8):
            P.op("pe", lambda e, c=c, b2=b2, sl=sl: e.matmul(psb[b2][:], lhsT=onesf[:], rhs=ysq[:, c, sl], start=(c == 0), stop=(c == 7)),
                 r=["onesf", ("ysq", c, half)], w=[("ps", b2)])
        P.op("dve", lambda e, b1=b1, sl=sl: e.tensor_scalar(out=mean[:, sl], in0=psb[b1][:], scalar1=1.0 / 1024, scalar2=None, op0=ALU.mult),
             r=[("ps", b1)], w=[("mean", half)])
        P.op("dve", lambda e, sl=sl: e.tensor_tensor(out=msq[:, sl], in0=mean[:, sl], in1=mean[:, sl], op=ALU.mult),
             r=[("mean", half)], w=[("msq", half)])
        P.op("dve", lambda e, b2=b2, sl=sl: e.scalar_tensor_tensor(out=rstd[:, sl], in0=psb[b2][:], scalar=1.0 / 1024, in1=msq[:, sl],
                                                                 op0=ALU.mult, op1=ALU.subtract),
             r=[("ps", b2), ("msq", half)], w=[("rstd", half)])
        P.op("act", lambda e, sl=sl: e.activation(out=rstd[:, sl], in_=rstd[:, sl], func=AF.Sqrt, bias=EPS), r=[("rstd", half)], w=[("rstd", half)])
        P.op("dve", lambda e, sl=sl: e.reciprocal(out=rstd[:, sl], in_=rstd[:, sl]), r=[("rstd", half)], w=[("rstd", half)])
    yact = C.sb([128, 8, 1024], BF16)
    for c in range(8):
        for half in range(2):
            sl = slice(half * 512, (half + 1) * 512)
            P.op("dve", lambda e, c=c, sl=sl: e.tensor_tensor(out=ycv[:, c, sl], in0=ycv[:, c, sl], in1=mean[:, sl], op=ALU.subtract),
                 r=[("ycv", c, half), ("mean", half)], w=[("ycv", c, half)])
            P.op("dve", lambda e, c=c, sl=sl: e.tensor_tensor(out=ycv[:, c, sl], in0=ycv[:, c, sl], in1=rstd[:, sl], op=ALU.mult),
                 r=[("ycv", c, half), ("rstd", half)], w=[("ycv", c, half)])
            P.op("act", lambda e, c=c, sl=sl: e.activation(out=yact[:, c, sl], in_=ycv[:, c, sl], func=AF.Silu, scale=vecs[:, 1, c:c + 1],
                                                         bias=vecs[:, 2, c:c + 1]),
                 r=[("ycv", c, half), "vecs"], w=[("yact", c, half)])
    YA = [("yact", c, h) for c in range(8) for h in range(2)]
    wb = [C.sb([128, 8, 512], BF16) for _ in range(2)]
    ob = [C.sb([128, 512], BF16) for _ in range(4)]
    w_v = wpw_d.rearrange("(c p) n -> p c n", p=128)
    oc = 0
    for g in range(2):
        P.dma(wb[g][:], w_v[:, :, g * 512:(g + 1) * 512], w=[("wb", g)], q="pool")
        for j in range(4):
            ch = g * 4 + j
            for half in range(2):
                sl = slice(half * 512, (half + 1) * 512)
                b = bank()
                for c in range(8):
                    P.op("pe", lambda e, c=c, b=b, g=g, j=j, sl=sl: e.matmul(psb[b][:], lhsT=wb[g][:, c, j * 128:(j + 1) * 128], rhs=yact[:, c, sl],
                                                                          start=(c == 0), stop=(c == 7)),
                         r=[("wb", g)] + YA, w=[("ps", b)])
                o = oc % 4
                oc += 1
                P.op("dve", lambda e, b=b, o=o, ch=ch, sl=sl: e.scalar_tensor_tensor(out=ob[o][:], in0=psb[b][:], scalar=vecs[:, 3, ch:ch + 1],
                                                                                in1=gaT[:, ch, sl], op0=ALU.add, op1=ALU.mult),
                     r=[("ps", b), "vecs", ("gaT", ch)], w=[("ob", o)])
                P.dma(ya_d[ch * 128:(ch + 1) * 128, sl], ob[o][:], r=[("ob", o)])
    return C.finish()


def build_E2c():
    C = Ctx()
    P = C.P
    cat_d = C.din("catT", [2048, 1024], BF16)
    x_d = C.din("x", [TOK, 2048])
    w_d = C.din("w", [2048, 2048])
    xo_d = C.dout("xo", [TOK, 2048])
    cat = C.sb([128, 16, 1024], BF16)
    for c in range(16):
        P.dma(cat[:, c, :], cat_d[c * 128:(c + 1) * 128, :], w=[("cat", c)])
    CAT = [("cat", c) for c in range(16)]
    xs = C.sb([128, NT, 2048], F32)
    for t in range(NT):
        P.dma(xs[:, t, :], x_d[t * 128:(t + 1) * 128, :], w=[("xs", t)])
    wb = [C.sb([128, 16, 512], BF16) for _ in range(2)]
    psb = [C.ps([128, 512], F32) for _ in range(4)]
    ob = [C.sb([128, 512], F32) for _ in range(4)]
    w_v = w_d.rearrange("(c p) n -> p c n", p=128)
    n = 0
    for g in range(4):
        s = g % 2
        P.dma(wb[s][:], w_v[:, :, g * 512:(g + 1) * 512], w=[("wb", s)], q="pool")
        for t in range(NT):
            b = n % 4
            n += 1
            for c in range(16):
                P.op("pe", lambda e, c=c, b=b, s=s, t=t: e.matmul(psb[b][:], lhsT=cat[:, c, t * 128:(t + 1) * 128], rhs=wb[s][:, c, :],
                                                               start=(c == 0), stop=(c == 15)),
                     r=[("wb", s)] + CAT, w=[("ps", b)])
            P.op("dve", lambda e, b=b, t=t, g=g: e.tensor_tensor(out=ob[b][:], in0=psb[b][:], in1=xs[:, t, g * 512:(g + 1) * 512], op=ALU.add),
                 r=[("ps", b), ("xs", t)], w=[("ob", b)])
            P.dma(xo_d[t * 128:(t + 1) * 128, g * 512:(g + 1) * 512], ob[b][:], r=[("ob", b)])
    return C.finish()


def build_E2b():
    C = Ctx()
    P = C.P
    SC = 128 ** -0.5
    qT_d = C.din("qT", [1024, 1024], BF16)
    kT_d = C.din("kTall", [4, 1024, 1024], BF16)
    v_d = C.din("vall", [4, 1024, 1024], BF16)
    gb_d = C.din("gb", [1024, 1024], BF16)
    gbias_d = C.din("gbias", [128, 4, 16])
    pflag_d = C.din("pflag", [128, 4, 16])
    tmask_d = C.din("tmask", [128, 2, 4, 256])
    id_d = C.din("ident", [128, 128])
    yb_d = C.dout("ybT", [1024, 1024], BF16)
    identf = load_const(C, id_d, [128, 128], F32, "identf")
    gbias = load_const(C, gbias_d, [128, 4, 16], F32, "gbias")
    pflag = load_const(C, pflag_d, [128, 4, 16], F32, "pflag")
    tmask = load_const(C, tmask_d, [128, 2, 4, 256], F32, "tmask")
    identb = C.sb([128, 128], BF16)
    onesb = C.sb([128, 128], BF16)
    P.op("dve", lambda e: e.tensor_copy(out=identb[:], in_=identf[:]), r=["identf"], w=["identb"])
    P.op("pool", lambda e: e.memset(onesb[:], 1.0), w=["onesb"])
    gbt = C.sb([128, NT, 1024], BF16)
    for t in range(NT):
        P.dma(gbt[:, t, :], gb_d[t * 128:(t + 1) * 128, :], w=[("gbt", t)])
    kTh = [C.sb([128, 4, 1024], BF16) for _ in range(2)]
    vh = [C.sb([128, 32, 128], BF16) for _ in range(2)]
    qTh = [C.sb([128, 1024], BF16) for _ in range(2)]
    sq = C.sb([128, 4096], BF16)
    qsq = C.sb([128, 1024], BF16)
    kmf = C.sb([128, 16], F32)
    kmb = C.sb([128, 16], BF16)
    kmx = C.sb([128, 8], F32)
    kmax2 = C.sb([128, 1], F32)
    ybTh = [C.sb([128, 1024], BF16) for _ in range(2)]
    ps_g = C.ps([128, 512], F32)
    ps_s = [C.ps([128, 512], F32) for _ in range(2)]
    ps_t = [C.ps([128, 2, 2, 128], BF16) for _ in range(2)]
    ps_o = [C.ps([128, 512], F32) for _ in range(2)]
    ps_y = C.ps([128, 8, 128], BF16)
    NR = 3
    gm = [C.sb([128, 16], F32) for _ in range(NR)]
    top8 = [C.sb([128, 8], F32) for _ in range(NR)]
    sel = [C.sb([128, 16], F32) for _ in range(NR)]
    ebias = [C.sb([128, 16], F32) for _ in range(NR)]
    mq = [C.sb([128, 1], F32) for _ in range(NR)]
    rsum = [C.sb([128, 16], F32) for _ in range(NR)]
    rtot = [C.sb([128, 1], F32) for _ in range(NR)]
    s2 = [C.sb([128, 512], F32) for _ in range(2)]
    pb = [C.sb([128, 512], BF16) for _ in range(3)]
    pTs = [C.sb([128, 2, 2, 128], BF16) for _ in range(3)]
    ybt = [C.sb([128, 128], BF16) for _ in range(2)]
    cn = dict(s=0, t=0, o=0, pb=0, pts=0, s2=0, y=0)
    for h in range(8):
        hs = h % 2
        for j in range(4):
            P.dma(kTh[hs][:, j, :], kT_d[j, h * 128:(h + 1) * 128, :], w=[("kTh", hs)])
            P.dma(vh[hs][:, j * 8:(j + 1) * 8, :], v_d[j].rearrange("(t p) c -> p t c", p=128)[:, :, h * 128:(h + 1) * 128], w=[("vh", hs)])
        P.dma(qTh[hs][:], qT_d[h * 128:(h + 1) * 128, :], w=[("qTh", hs)])
        P.op("dve", lambda e, hs=hs: e.tensor_reduce(out=kmf[:], in_=kTh[hs][:].rearrange("p j (i t) -> p (j i) t", i=4), axis=AX.X, op=ALU.add),
             r=[("kTh", hs)], w=["kmf"])
        P.op("dve", lambda e: e.tensor_copy(out=kmb[:], in_=kmf[:]), r=["kmf"], w=["kmb"])
        P.op("act", lambda e, hs=hs: e.activation(out=sq[:], in_=kTh[hs][:].rearrange("p j t -> p (j t)"), func=AF.Square), r=[("kTh", hs)], w=["sq"])
        P.op("act", lambda e, hs=hs: e.activation(out=qsq[:], in_=qTh[hs][:], func=AF.Square), r=[("qTh", hs)], w=["qsq"])
        for cc in range(8):
            P.op("pe", lambda e, cc=cc: e.matmul(ps_g[:], lhsT=onesb[:], rhs=sq[:, cc * 512:(cc + 1) * 512], start=True, stop=True),
                 r=["onesb", "sq"], w=["ps_g"])
            P.op("dve", lambda e, cc=cc: e.tensor_reduce(out=kmx[:, cc:cc + 1], in_=ps_g[:], axis=AX.X, op=ALU.max), r=["ps_g"], w=["kmx"])
        P.op("dve", lambda e: e.tensor_reduce(out=kmax2[:], in_=kmx[:], axis=AX.X, op=ALU.max), r=["kmx"], w=["kmax2"])
        for t in range(NT):
            i, qi = t // 2, t % 2
            z = (h * NT + t) % NR
            tq = slice(t * 128, (t + 1) * 128)
            P.op("pe", lambda e, hs=hs, tq=tq: e.matmul(ps_g[:, 0:16], lhsT=qTh[hs][:, tq], rhs=kmb[:], start=True, stop=True),
                 r=[("qTh", hs), "kmb"], w=["ps_g"])
            P.op("pe", lambda e, tq=tq: e.matmul(ps_g[:, 16:17], lhsT=qsq[:, tq], rhs=onesb[:, 0:1], start=True, stop=True),
                 r=["qsq", "onesb"], w=["ps_g"])
            P.op("dve", lambda e, z=z, i=i: e.tensor_tensor(out=gm[z][:], in0=ps_g[:, 0:16], in1=gbias[:, i, :], op=ALU.add),
                 r=["ps_g", "gbias"], w=[("gm", z)])
            P.op("dve", lambda e, z=z: e.tensor_tensor(out=mq[z][:], in0=ps_g[:, 16:17], in1=kmax2[:], op=ALU.mult),
                 r=["ps_g", "kmax2"], w=[("mq", z)])
            P.op("act", lambda e, z=z: e.activation(out=mq[z][:], in_=mq[z][:], func=AF.Sqrt, scale=SC * SC), r=[("mq", z)], w=[("mq", z)])
            P.op("dve", lambda e, z=z: e.max(out=top8[z][:], in_=gm[z][:]), r=[("gm", z)], w=[("top8", z)])
            P.op("dve", lambda e, z=z: e.tensor_scalar(out=sel[z][:], in0=gm[z][:], scalar1=top8[z][:, 2:3], scalar2=None, op0=ALU.is_ge),
                 r=[("gm", z), ("top8", z)], w=[("sel", z)])
            P.op("dve", lambda e, z=z: e.tensor_scalar(out=sel[z][:], in0=sel[z][:], scalar1=-1.0, scalar2=30000.0, op0=ALU.add, op1=ALU.mult),
                 r=[("sel", z)], w=[("sel", z)])
            P.op("dve", lambda e, z=z, i=i: e.tensor_tensor(out=sel[z][:], in0=sel[z][:], in1=pflag[:, i, :], op=ALU.mult),
                 r=[("sel", z), "pflag"], w=[("sel", z)])
            P.op("dve", lambda e, z=z: e.tensor_scalar(out=ebias[z][:], in0=sel[z][:], scalar1=mq[z][:, 0:1], scalar2=None, op0=ALU.subtract),
                 r=[("sel", z), ("mq", z)], w=[("ebias", z)])
            P.op("pool", lambda e, z=z: e.memset(rsum[z][:], 0.0), w=[("rsum", z)])
            ob_ = cn["o"] % 2
            cn["o"] += 1
            pairs = [(ip, jp) for ip in range(i + 1) for jp in range(2)]
            nmm = len(pairs) * 4
            mi = 0
            for (ip, jp) in pairs:
                sb_ = cn["s"] % 2
                cn["s"] += 1
                for jj in range(2):
                    j = jp * 2 + jj
                    P.op("pe", lambda e, hs=hs, tq=tq, sb_=sb_, jj=jj, j=j, ip=ip: e.matmul(ps_s[sb_][:, jj * 256:(jj + 1) * 256], lhsT=qTh[hs][:, tq],
                                                                                     rhs=kTh[hs][:, j, ip * 256:(ip + 1) * 256], start=True, stop=True),
                         r=[("qTh", hs), ("kTh", hs)], w=[("ps_s", sb_)])
                p_ = cn["pb"] % 3
                cn["pb"] += 1
                for jj in range(2):
                    j = jp * 2 + jj
                    m = j * 4 + ip
                    src = ps_s[sb_][:, jj * 256:(jj + 1) * 256]
                    rk = [("ps_s", sb_)]
                    if ip == i:
                        k2 = cn["s2"] % 2
                        cn["s2"] += 1
                        P.op("dve", lambda e, k2=k2, src=src, qi=qi, j=j: e.tensor_tensor(out=s2[k2][:, 0:256], in0=src, in1=tmask[:, qi, j, :], op=ALU.add),
                             r=[("ps_s", sb_), "tmask"], w=[("s2", k2)])
                        src = s2[k2][:, 0:256]
                        rk = [("s2", k2)]
                    P.op("act", lambda e, src=src, p_=p_, jj=jj, z=z, m=m: e.activation(out=pb[p_][:, jj * 256:(jj + 1) * 256], in_=src, func=AF.Exp, scale=SC,
                                                                                   bias=ebias[z][:, m:m + 1], accum_out=rsum[z][:, m:m + 1]),
                         r=rk + [("ebias", z)], w=[("pb", p_), ("rsum", z)])
                tb_ = cn["t"] % 2
                cn["t"] += 1
                for jj in range(2):
                    for hf in range(2):
                        P.op("pe", lambda e, p_=p_, jj=jj, hf=hf, tb_=tb_: e.transpose(out=ps_t[tb_][:, jj, hf, :], in_=pb[p_][:, jj * 256 + hf * 128:jj * 256 + hf * 128 + 128],
                                                                                identity=identb[:]),
                             r=[("pb", p_), "identb"], w=[("ps_t", tb_)])
                q_ = cn["pts"] % 3
                cn["pts"] += 1
                P.op("dve", lambda e, q_=q_, tb_=tb_: e.tensor_copy(out=pTs[q_][:], in_=ps_t[tb_][:]), r=[("ps_t", tb_)], w=[("pTs", q_)])
                for jj in range(2):
                    j = jp * 2 + jj
                    for hf in range(2):
                        kt = j * 8 + ip * 2 + hf
                        P.op("pe", lambda e, q_=q_, jj=jj, hf=hf, kt=kt, hs=hs, ob_=ob_, mi=mi: e.matmul(ps_o[ob_][:, 0:128], lhsT=pTs[q_][:, jj, hf, :], rhs=vh[hs][:, kt, :],
                                                                                               start=(mi == 0), stop=(mi == nmm - 1)),
                             r=[("pTs", q_), ("vh", hs)], w=[("ps_o", ob_)])
                        mi += 1
            P.op("dve", lambda e, z=z: e.tensor_reduce(out=rtot[z][:], in_=rsum[z][:], axis=AX.X, op=ALU.add), r=[("rsum", z)], w=[("rtot", z)])
            P.op("dve", lambda e, z=z: e.reciprocal(out=rtot[z][:], in_=rtot[z][:]), r=[("rtot", z)], w=[("rtot", z)])
            y_ = cn["y"] % 2
            cn["y"] += 1
            P.op("dve", lambda e, z=z, ob_=ob_, y_=y_, t=t, h=h: e.scalar_tensor_tensor(out=ybt[y_][:], in0=ps_o[ob_][:, 0:128], scalar=rtot[z][:, 0:1],
                                                                                 in1=gbt[:, t, h * 128:(h + 1) * 128], op0=ALU.mult, op1=ALU.mult),
                 r=[("ps_o", ob_), ("rtot", z), ("gbt", t)], w=[("ybt", y_)])
            P.op("pe", lambda e, y_=y_, t=t: e.transpose(out=ps_y[:, t, :], in_=ybt[y_][:], identity=identb[:]), r=[("ybt", y_), "identb"], w=["ps_y"])
        P.op("act", lambda e, hs=hs: e.copy(out=ybTh[hs][:], in_=ps_y[:].rearrange("p t q -> p (t q)")), r=["ps_y"], w=[("ybTh", hs)])
        P.dma(yb_d[h * 128:(h + 1) * 128, :], ybTh[hs][:], r=[("ybTh", hs)])
    return C.finish()


def local_tokens(c):
    b, r = c // 4, c % 4
    idx = np.concatenate([np.arange((4 * ii + r) * 256, (4 * ii + r + 1) * 256) for ii in range(4)])
    return b, idx


_CACHE = {}


def prog(name):
    if name not in _CACHE:
        _CACHE[name] = globals()["build_" + name]()
    return _CACHE[name]


def launch(name, ins):
    res = run_bass_kernel_spmd(prog(name), ins, core_ids=list(range(8)))
    return res.results


IDENT = np.eye(128, dtype=np.float32)


def fmvec(v):
    return np.ascontiguousarray(v.reshape(-1, 128).T)


def moba_consts(r):
    gbias = np.zeros((4, 16), np.float32)
    pflag = np.zeros((4, 16), np.float32)
    for i in range(4):
        for j in range(4):
            for ip in range(4):
                past = (4 * ip + j) < (4 * i + r)
                gbias[i, j * 4 + ip] = 0.0 if past else -1e30
                pflag[i, j * 4 + ip] = 1.0 if past else 0.0
    tm = np.zeros((128, 2, 4, 256), np.float32)
    for qi in range(2):
        for j in range(4):
            if j > r:
                tm[:, qi, j, :] = -30000.0
            elif j == r:
                qpos = qi * 128 + np.arange(128)[:, None]
                tm[:, qi, j, :] = np.where(np.arange(256)[None, :] <= qpos, 0.0, -30000.0)
    return (np.ascontiguousarray(np.broadcast_to(gbias, (128, 4, 16))), np.ascontiguousarray(np.broadcast_to(pflag, (128, 4, 16))), tm)


def even_layer(xl, p):
    ins = [dict(x=xl[c], gT=fmvec(p["norm"]), w=p["w_in"], bgT=fmvec(p["b_glu"]), ident=IDENT) for c in range(8)]
    e1 = launch("E1", ins)
    ins = []
    wdwT = np.ascontiguousarray(p["w_dw"].T.reshape(8, 128, 31).transpose(1, 0, 2))
    vecs = np.ascontiguousarray(np.stack([fmvec(p["b_dw"]), fmvec(p["ln_g"]), fmvec(p["ln_b"]), fmvec(p["b_pw"])], axis=1))
    for c in range(8):
        b, r = c // 4, c % 4
        uh = np.zeros((1024, 4, 32), NPBF)
        for i in range(4):
            gblk = 4 * i + r - 1
            if gblk < 0:
                continue
            src = e1[b * 4 + gblk % 4]["uT"]
            li = gblk // 4
            uh[:, i, :] = src[:, li * 256 + 224: li * 256 + 256]
        ins.append(dict(uT=e1[c]["uT"], uh=uh, gaT=e1[c]["gaT"], wdwT=wdwT, vecs=vecs, wpw=p["w_pw"], ident=IDENT))
    e2a = launch("E2a", ins)
    ins = []
    for c in range(8):
        b, r = c // 4, c % 4
        kT = np.stack([e1[b * 4 + j]["kT"] for j in range(4)])
        vv = np.stack([e1[b * 4 + j]["v"] for j in range(4)])
        gbias, pflag, tm = moba_consts(r)
        ins.append(dict(qT=e1[c]["qT"], kTall=kT, vall=vv, gb=e1[c]["gb"], gbias=gbias, pflag=pflag, tmask=tm, ident=IDENT))
    e2b = launch("E2b", ins)
    ins = [dict(catT=np.concatenate([e2a[c]["yaT"], e2b[c]["ybT"]], axis=0), x=xl[c], w=p["w_out"]) for c in range(8)]
    e2c = launch("E2c", ins)
    return [e2c[c]["xo"] for c in range(8)]


def build_O1():
    C = Ctx()
    P = C.P
    x_d = C.din("x", [TOK, 2048])
    gT_d = C.din("gT", [128, 16])
    w_d = C.din("w", [2048, 2896])
    qnT_d = C.din("qnT", [128, 4])
    rows_d = C.din("rows", [128, 3, 256])
    wqb_d = C.din("wqb", [512, 2048])
    wiq_d = C.din("wiq", [512, 1024])
    wuk_d = C.din("wuk", [16, 128, 256])
    id_d = C.din("ident", [128, 128])
    ckv_o = C.dout("ckv", [1024, 256], BF16)
    ckvT_o = C.dout("ckvT", [256, 1024], BF16)
    ikT_o = C.dout("ikT", [64, 1024], BF16)
    iw_o = C.dout("iw", [1024, 16])
    gT_o = C.dout("gateT", [2048, 1024], BF16)
    ql_o = C.dout("qlatT", [16, 256, 1024], BF16)
    iq_o = C.dout("iqT", [1024, 1024], BF16)
    identf = load_const(C, id_d, [128, 128], F32, "identf")
    gT = load_const(C, gT_d, [128, 16], F32, "gT")
    qnT = load_const(C, qnT_d, [128, 4], F32, "qnT")
    rows = load_const(C, rows_d, [128, 3, 256], F32, "rows")
    identb = C.sb([128, 128], BF16)
    P.op("dve", lambda e: e.tensor_copy(out=identb[:], in_=identf[:]), r=["identf"], w=["identb"])
    xnT = C.sb([128, 16, TOK], BF16)
    norm_transpose(C, x_d, gT, xnT, identb, 16, 2048)
    XN = [("xnT", t) for t in range(NT)]
    w_v = w_d.rearrange("(c p) n -> p c n", p=128)
    wA = C.sb([128, 16, 512], BF16)
    wB = C.sb([128, 16, 336], BF16)
    P.dma(wA[:], w_v[:, :, 0:512], w=["wA"], q="pool")
    P.dma(wB[:], w_v[:, :, 512:848], w=["wB"], q="pool")
    psb = [C.ps([128, 512], F32) for _ in range(4)]
    psT = [C.ps([128, 8, 128], BF16) for _ in range(2)]
    pc = [0]

    def bank():
        b = pc[0] % 4
        pc[0] += 1
        return b
    cqT = C.sb([128, 4, TOK], BF16)
    cqf = C.sb([128, 512], F32)
    cqn = C.sb([128, 512], BF16)
    bf = C.sb([128, 336], F32)
    junk = C.sb([128, 512], F32)
    st = C.sb([128, 8], F32)
    ckvn = C.sb([128, 256], BF16)
    ckvTt = C.sb([128, 2, 128], BF16)
    ikc = C.sb([128, 64], F32)
    ikn = C.sb([128, 64], BF16)
    ikTt = C.sb([64, 128], BF16)
    iws = C.sb([128, 16], F32)
    for t in range(NT):
        tq = slice(t * 128, (t + 1) * 128)
        b = bank()
        for c in range(16):
            P.op("pe", lambda e, c=c, b=b, tq=tq: e.matmul(psb[b][:], lhsT=xnT[:, c, tq], rhs=wA[:, c, :], start=(c == 0), stop=(c == 15)),
                 r=["wA"] + XN, w=[("ps", b)])
        P.op("act", lambda e, b=b: e.copy(out=cqf[:], in_=psb[b][:]), r=[("ps", b)], w=["cqf"])
        P.op("act", lambda e: e.activation(out=junk[:], in_=cqf[:], func=AF.Square, accum_out=st[:, 0:1]), r=["cqf"], w=["junk", "st0"])
        P.op("act", lambda e: e.activation(out=st[:, 0:1], in_=st[:, 0:1], func=AF.Sqrt, scale=1.0 / 512, bias=EPS), r=["st0"], w=["st0"])
        P.op("dve", lambda e: e.reciprocal(out=st[:, 0:1], in_=st[:, 0:1]), r=["st0"], w=["st0"])
        P.op("dve", lambda e: e.tensor_scalar(out=cqn[:], in0=cqf[:], scalar1=st[:, 0:1], scalar2=None, op0=ALU.mult), r=["cqf", "st0"], w=["cqn"])
        for c in range(4):
            P.op("pe", lambda e, c=c: e.transpose(out=psT[0][:, c, :], in_=cqn[:, c * 128:(c + 1) * 128], identity=identb[:]), r=["cqn", "identb"], w=["psT0"])
        for c in range(4):
            P.op("dve", lambda e, c=c, tq=tq: e.tensor_scalar(out=cqT[:, c, tq], in0=psT[0][:, c, :], scalar1=qnT[:, c:c + 1], scalar2=None, op0=ALU.mult),
                 r=["psT0", "qnT"], w=[("cqT", t)])
        b = bank()
        for c in range(16):
            P.op("pe", lambda e, c=c, b=b, tq=tq: e.matmul(psb[b][:, 0:336], lhsT=xnT[:, c, tq], rhs=wB[:, c, :], start=(c == 0), stop=(c == 15)),
                 r=["wB"] + XN, w=[("ps", b)])
        P.op("act", lambda e, b=b: e.copy(out=bf[:], in_=psb[b][:, 0:336]), r=[("ps", b)], w=["bf"])
        P.op("act", lambda e: e.activation(out=junk[:, 0:256], in_=bf[:, 0:256], func=AF.Square, accum_out=st[:, 1:2]), r=["bf"], w=["junk", "st1"])
        P.op("act", lambda e: e.activation(out=st[:, 1:2], in_=st[:, 1:2], func=AF.Sqrt, scale=1.0 / 256, bias=EPS), r=["st1"], w=["st1"])
        P.op("dve", lambda e: e.reciprocal(out=st[:, 1:2], in_=st[:, 1:2]), r=["st1"], w=["st1"])
        P.op("dve", lambda e: e.scalar_tensor_tensor(out=ckvn[:], in0=bf[:, 0:256], scalar=st[:, 1:2], in1=rows[:, 0, :], op0=ALU.mult, op1=ALU.mult),
             r=["bf", "st1", "rows"], w=["ckvn"])
        P.dma(ckv_o[tq, :], ckvn[:], r=["ckvn"])
        for c in range(2):
            P.op("pe", lambda e, c=c: e.transpose(out=psT[1][:, c, :], in_=ckvn[:, c * 128:(c + 1) * 128], identity=identb[:]), r=["ckvn", "identb"], w=["psT1"])
        P.op("act", lambda e: e.copy(out=ckvTt[:], in_=psT[1][:, 0:2, :]), r=["psT1"], w=["ckvTt"])
        P.dma(ckvT_o[:, tq].rearrange("(c p) t -> p c t", p=128), ckvTt[:], r=["ckvTt"])
        P.op("dve", lambda e: e.tensor_reduce(out=st[:, 2:3], in_=bf[:, 256:320], axis=AX.X, op=ALU.add), r=["bf"], w=["st2"])
        P.op("dve", lambda e: e.tensor_scalar(out=st[:, 2:3], in0=st[:, 2:3], scalar1=1.0 / 64, scalar2=None, op0=ALU.mult), r=["st2"], w=["st2"])
        P.op("dve", lambda e: e.tensor_scalar(out=ikc[:], in0=bf[:, 256:320], scalar1=st[:, 2:3], scalar2=None, op0=ALU.subtract), r=["bf", "st2"], w=["ikc"])
        P.op("act", lambda e: e.activation(out=junk[:, 0:64], in_=ikc[:], func=AF.Square, accum_out=st[:, 3:4]), r=["ikc"], w=["junk", "st3"])
        P.op("act", lambda e: e.activation(out=st[:, 3:4], in_=st[:, 3:4], func=AF.Sqrt, scale=1.0 / 64, bias=EPS), r=["st3"], w=["st3"])
        P.op("dve", lambda e: e.reciprocal(out=st[:, 3:4], in_=st[:, 3:4]), r=["st3"], w=["st3"])
        P.op("dve", lambda e: e.scalar_tensor_tensor(out=ikc[:], in0=ikc[:], scalar=st[:, 3:4], in1=rows[:, 1, 0:64], op0=ALU.mult, op1=ALU.mult),
             r=["ikc", "st3", "rows"], w=["ikc"])
        P.op("dve", lambda e: e.tensor_tensor(out=ikn[:], in0=ikc[:], in1=rows[:, 2, 0:64], op=ALU.add), r=["ikc", "rows"], w=["ikn"])
        P.op("pe", lambda e: e.transpose(out=psT[1][0:64, 4, :], in_=ikn[:], identity=identb[:]), r=["ikn", "identb"], w=["psT1"])
        P.op("act", lambda e: e.copy(out=ikTt[:], in_=psT[1][0:64, 4, :]), r=["psT1"], w=["ikTt"])
        P.dma(ikT_o[:, tq], ikTt[:], r=["ikTt"])
        P.op("dve", lambda e: e.tensor_scalar(out=iws[:], in0=bf[:, 320:336], scalar1=1.0 / 32, scalar2=None, op0=ALU.mult), r=["bf"], w=["iws"])
        P.dma(iw_o[tq, :], iws[:], r=["iws"])
    CQ = [("cqT", t) for t in range(NT)]
    wb = [C.sb([128, 16, 512], BF16) for _ in range(2)]
    ob = [C.sb([128, 512], BF16) for _ in range(4)]
    oc = [0]

    def newob():
        o = oc[0] % 4
        oc[0] += 1
        return o
    for g in range(4):
        s = g % 2
        P.dma(wb[s][:], w_v[:, :, 848 + g * 512:848 + (g + 1) * 512], w=[("wb", s)], q="pool")
        for j in range(4):
            ch = g * 4 + j
            for h in range(2):
                b = bank()
                for c in range(16):
                    P.op("pe", lambda e, c=c, b=b, s=s, j=j, h=h: e.matmul(psb[b][:], lhsT=wb[s][:, c, j * 128:(j + 1) * 128], rhs=xnT[:, c, h * 512:(h + 1) * 512],
                                                                        start=(c == 0), stop=(c == 15)),
                         r=[("wb", s)] + XN, w=[("ps", b)])
                o = newob()
                P.op("act", lambda e, b=b, o=o: e.activation(out=ob[o][:], in_=psb[b][:], func=AF.Silu), r=[("ps", b)], w=[("ob", o)])
                P.dma(gT_o[ch * 128:(ch + 1) * 128, h * 512:(h + 1) * 512], ob[o][:], r=[("ob", o)])
    wqb = C.sb([128, 4, 2048], BF16)
    wiq = C.sb([128, 4, 1024], BF16)
    wuk = C.sb([128, 16, 256], BF16)
    P.dma(wqb[:], wqb_d.rearrange("(c p) n -> p c n", p=128), w=["wqb"], q="pool")
    P.dma(wiq[:], wiq_d.rearrange("(c p) n -> p c n", p=128), w=["wiq"], q="pool")
    P.dma(wuk[:], wuk_d.rearrange("h d c -> d h c"), w=["wuk"], q="pool")
    qTh = [C.sb([128, 1024], BF16) for _ in range(2)]
    for h in range(16):
        s = h % 2
        for hf in range(2):
            b = bank()
            for c in range(4):
                P.op("pe", lambda e, c=c, b=b, h=h, hf=hf: e.matmul(psb[b][:], lhsT=wqb[:, c, h * 128:(h + 1) * 128], rhs=cqT[:, c, hf * 512:(hf + 1) * 512],
                                                                 start=(c == 0), stop=(c == 3)),
                     r=["wqb"] + CQ, w=[("ps", b)])
            P.op("act", lambda e, b=b, s=s, hf=hf: e.copy(out=qTh[s][:, hf * 512:(hf + 1) * 512], in_=psb[b][:]), r=[("ps", b)], w=[("qTh", s, hf)])
        for cc in range(2):
            for hf in range(2):
                b = bank()
                P.op("pe", lambda e, b=b, h=h, cc=cc, hf=hf, s=s: e.matmul(psb[b][:], lhsT=wuk[:, h, cc * 128:(cc + 1) * 128], rhs=qTh[s][:, hf * 512:(hf + 1) * 512],
                                                                        start=True, stop=True),
                     r=["wuk", ("qTh", s, hf)], w=[("ps", b)])
                o = newob()
                P.op("act", lambda e, b=b, o=o: e.copy(out=ob[o][:], in_=psb[b][:]), r=[("ps", b)], w=[("ob", o)])
                P.dma(ql_o[h, cc * 128:(cc + 1) * 128, hf * 512:(hf + 1) * 512], ob[o][:], r=[("ob", o)])
    for ch in range(8):
        for hf in range(2):
            b = bank()
            for c in range(4):
                P.op("pe", lambda e, c=c, b=b, ch=ch, hf=hf: e.matmul(psb[b][:], lhsT=wiq[:, c, ch * 128:(ch + 1) * 128], rhs=cqT[:, c, hf * 512:(hf + 1) * 512],
                                                                   start=(c == 0), stop=(c == 3)),
                     r=["wiq"] + CQ, w=[("ps", b)])
            o = newob()
            P.op("act", lambda e, b=b, o=o: e.copy(out=ob[o][:], in_=psb[b][:]), r=[("ps", b)], w=[("ob", o)])
            P.dma(iq_o[ch * 128:(ch + 1) * 128, hf * 512:(hf + 1) * 512], ob[o][:], r=[("ob", o)])
    return C.finish()


def build_O2():
    C = Ctx()
    P = C.P
    SC = 128 ** -0.5
    ql_d = C.din("qlatT", [16, 256, 1024], BF16)
    iq_d = C.din("iqT", [1024, 1024], BF16)
    iw_d = C.din("iw", [1024, 16])
    g_d = C.din("gateT", [2048, 1024], BF16)
    ckvT_d = C.din("ckvTall", [4, 256, 1024], BF16)
    ckv_d = C.din("ckvall", [4, 1024, 256], BF16)
    ikT_d = C.din("ikTall", [4, 64, 1024], BF16)
    tmask_d = C.din("tmask", [128, 2, 4, 256])
    wuv_d = C.din("wuv", [16, 256, 128])
    id_d = C.din("ident", [128, 128])
    cat_o = C.dout("catT", [2048, 1024], BF16)
    identf = load_const(C, id_d, [128, 128], F32, "identf")
    tmask = load_const(C, tmask_d, [128, 2, 4, 256], F32, "tmask")
    identb = C.sb([128, 128], BF16)
    onesb = C.sb([128, 128], BF16)
    P.op("dve", lambda e: e.tensor_copy(out=identb[:], in_=identf[:]), r=["identf"], w=["identb"])
    P.op("pool", lambda e: e.memset(onesb[:], 1.0), w=["onesb"])
    ckvT = C.sb([128, 2, 4, 1024], BF16)
    ckva = C.sb([128, 32, 257], BF16)
    ikT2 = C.sb([128, 4, 1024], BF16)
    iqT = C.sb([128, 8, 1024], BF16)
    iwt = C.sb([128, NT, 16], F32)
    wuv = C.sb([128, 16, 2, 128], BF16)
    P.op("pool", lambda e: e.memset(ckva[:], 1.0), w=["ckva"])
    for j in range(4):
        P.dma(ckvT[:, :, j, :], ckvT_d[j].rearrange("(c p) t -> p c t", p=128), w=["ckvT"])
        P.dma(ckva[:, j * 8:(j + 1) * 8, 0:256], ckv_d[j].rearrange("(t p) c -> p t c", p=128), w=["ckva"])
        P.dma(ikT2[0:64, j, :], ikT_d[j], w=["ikT2"])
        P.dma(ikT2[64:128, j, :], ikT_d[j], w=["ikT2"])
    for c in range(8):
        P.dma(iqT[:, c, :], iq_d[c * 128:(c + 1) * 128, :], w=["iqT"])
    P.dma(iwt[:], iw_d.rearrange("(t p) h -> p t h", p=128), w=["iwt"])
    P.dma(wuv[:], wuv_d.rearrange("h (cc c) d -> c h cc d", cc=2), w=["wuv"], q="pool")
    sq = C.sb([128, 4096], BF16)
    kmx = C.sb([128, 16], F32)
    kmax2 = C.sb([128, 1], F32)
    ps_g = C.ps([128, 512], F32)
    ps_s = [C.ps([128, 512], F32) for _ in range(2)]
    ps_t = [C.ps([128, 4, 128], BF16) for _ in range(2)]
    ps_o = [C.ps([128, 512], F32) for _ in range(2)]
    ps_y = C.ps([128, 512], F32)
    for cc in range(2):
        P.op("act", lambda e, cc=cc: e.activation(out=sq[:], in_=ckvT[:, cc, :, :].rearrange("p j t -> p (j t)"), func=AF.Square), r=["ckvT"], w=["sq"])
        for k in range(8):
            if cc == 0:
                continue
        for k in range(8):
            P.op("pe", lambda e, k=k: e.matmul(ps_g[:], lhsT=onesb[:], rhs=sq[:, k * 512:(k + 1) * 512], start=True, stop=True), r=["onesb", "sq"], w=["ps_g"])
            P.op("dve", lambda e, k=k, cc=cc: e.tensor_reduce(out=kmx[:, cc * 8 + k:cc * 8 + k + 1], in_=ps_g[:], axis=AX.X, op=ALU.max), r=["ps_g"], w=["kmx"])
    km2 = C.sb([128, 2], F32)
    P.op("dve", lambda e: e.tensor_reduce(out=km2[:], in_=kmx[:].rearrange("p (a b) -> p a b", a=2), axis=AX.X, op=ALU.max), r=["kmx"], w=["km2"])
    P.op("dve", lambda e: e.tensor_reduce(out=kmax2[:], in_=km2[:], axis=AX.X, op=ALU.add), r=["km2"], w=["kmax2"])
    score = C.sb([128, 4096], F32)
    junk = C.sb([128, 4096], BF16)
    m01 = C.sb([128, 4096], BF16)
    tmp = [C.sb([128, 512], F32) for _ in range(2)]
    lo = C.sb([128, 1], F32)
    hi = C.sb([128, 1], F32)
    mid = C.sb([128, 1], F32)
    cntt = C.sb([128, 1], F32)
    ge = C.sb([128, 1], F32)
    dd = C.sb([128, 1], F32)
    qlt = [C.sb([128, 16, 2, 128], BF16) for _ in range(2)]
    qsq = C.sb([128, 16, 2, 128], BF16)
    mq = C.sb([128, 16], F32)
    pb = [C.sb([128, 512], BF16) for _ in range(3)]
    pTs = [C.sb([128, 4, 128], BF16) for _ in range(3)]
    on = [C.sb([128, 256], BF16) for _ in range(2)]
    rinv = [C.sb([128, 1], F32) for _ in range(2)]
    olT = [C.sb([128, 2, 128], BF16) for _ in range(2)]
    gT = C.sb([128, 16, 1024], BF16)
    for c in range(16):
        P.dma(gT[:, c, :], g_d[c * 128:(c + 1) * 128, :], w=["gT"])
    catt = [C.sb([128, 16, 128], BF16) for _ in range(2)]
    cn = dict(s=0, t=0, o=0, pb=0, pts=0, tmp=0)
    for t in range(NT):
        i, qi = t // 2, t % 2
        tq = slice(t * 128, (t + 1) * 128)
        nk = (i + 1) * 256
        chunks = [(j, k0, min(512, nk - k0)) for j in range(4) for k0 in range(0, nk, 512)]
        qs = t % 2
        P.dma(qlt[qs][:], ql_d[:, :, tq].rearrange("h (cc c) q -> c h cc q", cc=2), w=[("qlt", qs)])
        P.op("pool", lambda e: e.memset(score[:], -30000.0), w=["score"])
        for (j, k0, n) in chunks:
            for hh in range(16):
                sb_ = cn["s"] % 2
                cn["s"] += 1
                pr = slice((hh % 2) * 64, (hh % 2) * 64 + 64)
                P.op("pe", lambda e, sb_=sb_, hh=hh, pr=pr, j=j, k0=k0, n=n, tq=tq: e.matmul(ps_s[sb_][:, 0:n], lhsT=iqT[pr, hh // 2, tq], rhs=ikT2[pr, j, k0:k0 + n],
                                                                                      start=True, stop=True),
                     r=["iqT", "ikT2"], w=[("ps_s", sb_)])
                dst = score[:, j * 1024 + k0:j * 1024 + k0 + n]
                if hh == 0:
                    P.op("dve", lambda e, sb_=sb_, n=n, dst=dst, t=t, hh=hh: e.tensor_scalar(out=dst, in0=ps_s[sb_][:, 0:n], scalar1=0.0, scalar2=iwt[:, t, hh:hh + 1],
                                                                                      op0=ALU.max, op1=ALU.mult),
                         r=[("ps_s", sb_), "iwt"], w=["score"])
                else:
                    k2 = cn["tmp"] % 2
                    cn["tmp"] += 1
                    P.op("dve", lambda e, sb_=sb_, n=n, k2=k2, t=t, hh=hh: e.tensor_scalar(out=tmp[k2][:, 0:n], in0=ps_s[sb_][:, 0:n], scalar1=0.0, scalar2=iwt[:, t, hh:hh + 1],
                                                                                    op0=ALU.max, op1=ALU.mult),
                         r=[("ps_s", sb_), "iwt"], w=[("tmp", k2)])
                    P.op("pool", lambda e, dst=dst, k2=k2, n=n: e.tensor_tensor(out=dst, in0=dst, in1=tmp[k2][:, 0:n], op=ALU.add), r=[("tmp", k2), "score"], w=["score"])
        for j in range(4):
            dst = score[:, j * 1024 + i * 256:j * 1024 + (i + 1) * 256]
            P.op("dve", lambda e, dst=dst, j=j, qi=qi: e.tensor_tensor(out=dst, in0=dst, in1=tmask[:, qi, j, :], op=ALU.add), r=["score", "tmask"], w=["score"])
        P.op("dve", lambda e: e.tensor_reduce(out=hi[:], in_=score[:], axis=AX.X, op=ALU.max), r=["score"], w=["hi"])
        P.op("dve", lambda e: e.tensor_scalar(out=lo[:], in0=hi[:], scalar1=-16.0, scalar2=None, op0=ALU.add), r=["hi"], w=["lo"])
        for it in range(20):
            P.op("dve", lambda e: e.tensor_tensor(out=mid[:], in0=lo[:], in1=hi[:], op=ALU.add), r=["lo", "hi"], w=["mid"])
            P.op("dve", lambda e: e.tensor_scalar(out=mid[:], in0=mid[:], scalar1=0.5, scalar2=None, op0=ALU.mult), r=["mid"], w=["mid"])
            P.op("dve", lambda e: e.tensor_scalar(out=junk[:], in0=score[:], scalar1=mid[:, 0:1], scalar2=None, op0=ALU.is_ge, op1=ALU.add, accum_out=cntt[:]),
                 r=["score", "mid"], w=["junk", "cntt"])
            P.op("dve", lambda e: e.tensor_scalar(out=ge[:], in0=cntt[:], scalar1=255.5, scalar2=None, op0=ALU.is_ge), r=["cntt"], w=["ge"])
            P.op("dve", lambda e: e.tensor_tensor(out=dd[:], in0=mid[:], in1=lo[:], op=ALU.subtract), r=["mid", "lo"], w=["dd"])
            P.op("dve", lambda e: e.scalar_tensor_tensor(out=lo[:], in0=dd[:], scalar=ge[:, 0:1], in1=lo[:], op0=ALU.mult, op1=ALU.add), r=["dd", "ge", "lo"], w=["lo"])
            P.op("dve", lambda e: e.tensor_tensor(out=dd[:], in0=hi[:], in1=mid[:], op=ALU.subtract), r=["mid", "hi"], w=["dd"])
            P.op("dve", lambda e: e.scalar_tensor_tensor(out=hi[:], in0=dd[:], scalar=ge[:, 0:1], in1=mid[:], op0=ALU.mult, op1=ALU.add), r=["dd", "ge", "mid"], w=["hi"])
        P.op("dve", lambda e: e.tensor_scalar(out=m01[:], in0=score[:], scalar1=lo[:, 0:1], scalar2=None, op0=ALU.is_ge), r=["score", "lo"], w=["m01"])
        P.op("act", lambda e, qs=qs: e.activation(out=qsq[:], in_=qlt[qs][:], func=AF.Square), r=[("qlt", qs)], w=["qsq"])
        for hh in range(16):
            for cc in range(2):
                P.op("pe", lambda e, hh=hh, cc=cc: e.matmul(ps_g[:, hh:hh + 1], lhsT=qsq[:, hh, cc, :], rhs=onesb[:, 0:1], start=(cc == 0), stop=(cc == 1)),
                     r=["qsq", "onesb"], w=["ps_g"])
        P.op("dve", lambda e: e.tensor_scalar(out=mq[:], in0=ps_g[:, 0:16], scalar1=kmax2[:, 0:1], scalar2=None, op0=ALU.mult), r=["ps_g", "kmax2"], w=["mq"])
        P.op("act", lambda e: e.activation(out=mq[:], in_=mq[:], func=AF.Sqrt, scale=SC * SC), r=["mq"], w=["mq"])
        P.op("dve", lambda e: e.tensor_scalar(out=mq[:], in0=mq[:], scalar1=-1.0, scalar2=None, op0=ALU.mult), r=["mq"], w=["mq"])
        cs = t % 2
        for hh in range(16):
            ob_ = cn["o"] % 2
            cn["o"] += 1
            nmm = sum(n // 128 for (_, _, n) in chunks)
            mi = 0
            for (j, k0, n) in chunks:
                sb_ = cn["s"] % 2
                cn["s"] += 1
                for cc in range(2):
                    P.op("pe", lambda e, sb_=sb_, hh=hh, cc=cc, j=j, k0=k0, n=n, qs=qs: e.matmul(ps_s[sb_][:, 0:n], lhsT=qlt[qs][:, hh, cc, :], rhs=ckvT[:, cc, j, k0:k0 + n],
                                                                                          start=(cc == 0), stop=(cc == 1)),
                         r=[("qlt", qs), "ckvT"], w=[("ps_s", sb_)])
                p_ = cn["pb"] % 3
                cn["pb"] += 1
                P.op("act", lambda e, sb_=sb_, p_=p_, n=n, hh=hh: e.activation(out=pb[p_][:, 0:n], in_=ps_s[sb_][:, 0:n], func=AF.Exp, scale=SC, bias=mq[:, hh:hh + 1]),
                     r=[("ps_s", sb_), "mq"], w=[("pb", p_)])
                P.op("pool", lambda e, p_=p_, n=n, j=j, k0=k0: e.tensor_tensor(out=pb[p_][:, 0:n], in0=pb[p_][:, 0:n], in1=m01[:, j * 1024 + k0:j * 1024 + k0 + n], op=ALU.mult),
                     r=[("pb", p_), "m01"], w=[("pb", p_)])
                tb_ = cn["t"] % 2
                cn["t"] += 1
                for a in range(n // 128):
                    P.op("pe", lambda e, p_=p_, a=a, tb_=tb_: e.transpose(out=ps_t[tb_][:, a, :], in_=pb[p_][:, a * 128:(a + 1) * 128], identity=identb[:]),
                         r=[("pb", p_), "identb"], w=[("ps_t", tb_)])
                q_ = cn["pts"] % 3
                cn["pts"] += 1
                P.op("dve", lambda e, q_=q_, tb_=tb_, n=n: e.tensor_copy(out=pTs[q_][:, 0:n // 128, :], in_=ps_t[tb_][:, 0:n // 128, :]), r=[("ps_t", tb_)], w=[("pTs", q_)])
                for a in range(n // 128):
                    kt = j * 8 + k0 // 128 + a
                    P.op("pe", lambda e, q_=q_, a=a, kt=kt, ob_=ob_, mi=mi, nmm=nmm: e.matmul(ps_o[ob_][:, 0:257], lhsT=pTs[q_][:, a, :], rhs=ckva[:, kt, :],
                                                                                        start=(mi == 0), stop=(mi == nmm - 1)),
                         r=[("pTs", q_), "ckva"], w=[("ps_o", ob_)])
                    mi += 1
            z = hh % 2
            P.op("dve", lambda e, z=z, ob_=ob_: e.reciprocal(out=rinv[z][:], in_=ps_o[ob_][:, 256:257]), r=[("ps_o", ob_)], w=[("rinv", z)])
            P.op("dve", lambda e, z=z, ob_=ob_: e.tensor_scalar(out=on[z][:], in0=ps_o[ob_][:, 0:256], scalar1=rinv[z][:, 0:1], scalar2=None, op0=ALU.mult),
                 r=[("ps_o", ob_), ("rinv", z)], w=[("on", z)])
            tb_ = cn["t"] % 2
            cn["t"] += 1
            for cc in range(2):
                P.op("pe", lambda e, z=z, cc=cc, tb_=tb_: e.transpose(out=ps_t[tb_][:, cc, :], in_=on[z][:, cc * 128:(cc + 1) * 128], identity=identb[:]),
                     r=[("on", z), "identb"], w=[("ps_t", tb_)])
            P.op("act", lambda e, z=z, tb_=tb_: e.copy(out=olT[z][:], in_=ps_t[tb_][:, 0:2, :]), r=[("ps_t", tb_)], w=[("olT", z)])
            for cc in range(2):
                P.op("pe", lambda e, z=z, cc=cc, hh=hh: e.matmul(ps_y[:, 0:128], lhsT=wuv[:, hh, cc, :], rhs=olT[z][:, cc, :], start=(cc == 0), stop=(cc == 1)),
                     r=[("olT", z), "wuv"], w=["ps_y"])
            P.op("dve", lambda e, hh=hh, tq=tq, cs=cs: e.tensor_tensor(out=catt[cs][:, hh, :], in0=ps_y[:, 0:128], in1=gT[:, hh, tq], op=ALU.mult),
                 r=["ps_y", "gT"], w=[("catt", cs)])
        P.dma(cat_o[:, tq].rearrange("(h p) q -> p h q", p=128), catt[cs][:], r=[("catt", cs)])
    return C.finish()


def build_F():
    C = Ctx()
    P = C.P
    x_d = C.din("x", [TOK, 2048])
    g_d = C.din("grow", [128, 2048])
    o_d = C.dout("y", [TOK, 2048])
    grow = load_const(C, g_d, [128, 2048], F32, "grow")
    xt = [C.sb([128, 2048], F32) for _ in range(2)]
    yo = [C.sb([128, 2048], F32) for _ in range(2)]
    junk = C.sb([128, 2048], BF16)
    ss = C.sb([128, NT], F32)
    for t in range(NT):
        s = t % 2
        P.dma(xt[s][:], x_d[t * 128:(t + 1) * 128, :], w=[("xt", s)])
        P.op("act", lambda e, s=s, t=t: e.activation(out=junk[:], in_=xt[s][:], func=AF.Square, accum_out=ss[:, t:t + 1]), r=[("xt", s)], w=["junk", ("ss", t)])
        P.op("act", lambda e, t=t: e.activation(out=ss[:, t:t + 1], in_=ss[:, t:t + 1], func=AF.Sqrt, scale=1.0 / 2048, bias=EPS), r=[("ss", t)], w=[("ss", t)])
        P.op("dve", lambda e, t=t: e.reciprocal(out=ss[:, t:t + 1], in_=ss[:, t:t + 1]), r=[("ss", t)], w=[("ss", t)])
        P.op("dve", lambda e, s=s, t=t: e.scalar_tensor_tensor(out=yo[s][:], in0=xt[s][:], scalar=ss[:, t:t + 1], in1=grow[:], op0=ALU.mult, op1=ALU.mult),
             r=[("xt", s), ("ss", t), "grow"], w=[("yo", s)])
        P.dma(o_d[t * 128:(t + 1) * 128, :], yo[s][:], r=[("yo", s)])
    return C.finish()


def odd_layer(xl, p):
    rows = np.zeros((128, 3, 256), np.float32)
    rows[:, 0, :] = p["kv_norm"][None, :]
    rows[:, 1, :64] = p["ik_g"][None, :]
    rows[:, 2, :64] = p["ik_b"][None, :]
    ins = [dict(x=xl[c], gT=fmvec(p["norm"]), w=p["w_in"], qnT=fmvec(p["q_norm"]), rows=rows, wqb=p["w_qb"], wiq=p["w_iq"], wuk=p["w_uk"], ident=IDENT)
           for c in range(8)]
    o1 = launch("O1", ins)
    ins = []
    for c in range(8):
        b, r = c // 4, c % 4
        _, _, tm = moba_consts(r)
        ins.append(dict(qlatT=o1[c]["qlatT"], iqT=o1[c]["iqT"], iw=o1[c]["iw"], gateT=o1[c]["gateT"],
                        ckvTall=np.stack([o1[b * 4 + j]["ckvT"] for j in range(4)]), ckvall=np.stack([o1[b * 4 + j]["ckv"] for j in range(4)]),
                        ikTall=np.stack([o1[b * 4 + j]["ikT"] for j in range(4)]), tmask=tm, wuv=p["w_uv"], ident=IDENT))
    o2 = launch("O2", ins)
    ins = [dict(catT=o2[c]["catT"], x=xl[c], w=p["w_out"]) for c in range(8)]
    e2c = launch("E2c", ins)
    return [e2c[c]["xo"] for c in range(8)]


def kernel(**z):
    x = np.asarray(z["x"], np.float32)
    xl = []
    for c in range(8):
        b, idx = local_tokens(c)
        xl.append(np.ascontiguousarray(x[b, idx]))
    for layer in range(4):
        i = layer // 2
        if layer % 2 == 0:
            p = dict(norm=z["even_norm"][i], w_in=z["even_w_in"][i], b_glu=z["even_b_glu"][i], w_dw=z["even_w_dw"][i], b_dw=z["even_b_dw"][i],
                     ln_g=z["even_conv_ln_g"][i], ln_b=z["even_conv_ln_b"][i], w_pw=z["even_w_pw"][i], b_pw=z["even_b_pw"][i], w_out=z["even_w_out"][i])
            p = {k: np.ascontiguousarray(np.asarray(v, np.float32)) for k, v in p.items()}
            xl = even_layer(xl, p)
        else:
            p = dict(norm=z["odd_norm"][i], w_in=z["odd_w_in"][i], q_norm=z["odd_q_norm"][i], w_qb=z["odd_w_qb"][i], kv_norm=z["odd_kv_norm"][i],
                     w_uk=z["odd_w_uk"][i], w_uv=z["odd_w_uv"][i], w_iq=z["odd_w_iq"][i], ik_g=z["odd_ik_ln_g"][i], ik_b=z["odd_ik_ln_b"][i], w_out=z["odd_w_out"][i])
            p = {k: np.ascontiguousarray(np.asarray(v, np.float32)) for k, v in p.items()}
            xl = odd_layer(xl, p)
    grow = np.ascontiguousarray(np.broadcast_to(np.asarray(z["final_norm"], np.float32)[None, :], (128, 2048)))
    f = launch("F", [dict(x=xl[c], grow=grow) for c in range(8)])
    out = np.zeros_like(x)
    for c in range(8):
        b, idx = local_tokens(c)
        out[b, idx] = f[c]["y"]
    return out
```

```python
import contextlib
import numpy as np
import concourse.bass as bass
import concourse.mybir as mybir
from concourse.bass_utils import run_bass_kernel_spmd

F32 = mybir.dt.float32
BF16 = mybir.dt.bfloat16
AF = mybir.ActivationFunctionType
ALU = mybir.AluOpType
AX = mybir.AxisListType

COMPUTE = ("pe", "act", "dve", "pool")
ND = 12


class Prog:
    def __init__(self, nc):
        self.nc = nc
        self.ops = {e: [] for e in ("pe", "act", "dve", "pool", "sp")}
        self.last_w = {}
        self.readers = {}
        self.dmas_since_bar = []

    def op(self, eng, fn, r=(), w=(), dma=False, cc=False):
        idx = len(self.ops[eng])
        me = (eng, idx)
        deps = set()
        for k in r:
            if k in self.last_w:
                deps.add(self.last_w[k])
        for k in w:
            if k in self.last_w:
                deps.add(self.last_w[k])
            rd = self.readers.get(k)
            if rd:
                for d in rd[0].items():
                    deps.add(d)
                for d in rd[1]:
                    deps.add(d)
        if eng == "pe" and not dma:
            deps = {d for d in deps if d[0] != "pe"}
        deps.discard(me)
        self.ops[eng].append(dict(fn=fn, deps=deps, dma=dma, cc=cc))
        for k in r:
            rd = self.readers.setdefault(k, ({}, []))
            if dma or cc:
                rd[1].append(me)
            else:
                rd[0][eng] = idx
        for k in w:
            self.last_w[k] = me
            self.readers[k] = ({}, [])
        if dma or cc:
            self.dmas_since_bar.append(me)
        return me

    def barrier(self):
        deps = set()
        for e in self.ops:
            for i in range(len(self.ops[e]) - 1, -1, -1):
                if not self.ops[e][i]["dma"] and not self.ops[e][i].get("cc") and self.ops[e][i]["fn"] is not None:
                    deps.add((e, i))
                    break
        deps.update(self.dmas_since_bar)
        for e in self.ops:
            self.ops[e].append(dict(fn=None, deps=set(deps), dma=False))
        self.last_w = {}
        self.readers = {}
        self.dmas_since_bar = []

    def dma(self, out, in_, r=(), w=(), q="sp"):
        return self.op(q, lambda e: e.dma_start(out=out, in_=in_), r=r, w=w, dma=True)

    def emit(self):
        nc = self.nc
        flagged = set()
        for e in self.ops:
            for o in self.ops[e]:
                flagged.update(o["deps"])
        with contextlib.ExitStack() as es:
            sems = {e: es.enter_context(nc.semaphore("s_" + e)) for e in COMPUTE}
            dsems = {q: [es.enter_context(nc.semaphore("d_%s%d" % (q, i))) for i in range(ND)]
                     for q in ("sp", "pool", "act")}
            sig = {}
            for e in self.ops:
                cnt = 0
                j = 0
                for i, o in enumerate(self.ops[e]):
                    if o.get("cc"):
                        s = es.enter_context(nc.semaphore("cc_%s%d" % (e, i)))
                        sig[(e, i)] = (s, 1)
                    elif o["dma"]:
                        s = dsems[e][j % ND]
                        sig[(e, i)] = (s, 16 * (j // ND + 1))
                        o["pre"] = (s, 16 * (j // ND)) if j >= ND else None
                        j += 1
                    elif (e, i) in flagged:
                        cnt += 1
                        sig[(e, i)] = (sems[e], cnt)

            def run(ename, eng):
                known = {}
                for i, o in enumerate(self.ops[ename]):
                    waits = {}
                    if o.get("pre"):
                        s, v = o["pre"]
                        waits[s] = (s, v)
                    for d in o["deps"]:
                        s, v = sig[d]
                        key = id(s)
                        if known.get(key, 0) >= v:
                            continue
                        if key not in waits or waits[key][1] < v:
                            waits[key] = (s, v)
                    for key, (s, v) in waits.items():
                        eng.wait_ge(s, v)
                        known[key] = v
                    if o["fn"] is None:
                        continue
                    ins = o["fn"](eng)
                    if (ename, i) in sig:
                        s, v = sig[(ename, i)]
                        ins.then_inc(s, 16 if o["dma"] else 1)

            with nc.Block() as block:
                @block.tensor
                def _(e):
                    run("pe", e)

                @block.scalar
                def _(e):
                    run("act", e)

                @block.vector
                def _(e):
                    run("dve", e)

                @block.gpsimd
                def _(e):
                    run("pool", e)

                @block.sync
                def _(e):
                    run("sp", e)


import ml_dtypes
NPBF = ml_dtypes.bfloat16
EPS = 1e-6
NT = 8
TOK = 1024


DECL = []


class Ctx:
    def __init__(self, fused=False):
        self.nc = bass.Bass("TRN2", target_bir_lowering=False)
        self.P = Prog(self.nc)
        self.es = contextlib.ExitStack()
        self.n = 0
        self.fused = fused
        self.io = {}

    def ext_in(self, name, shape, dt=F32):
        DECL.append(name)
        return self.nc.dram_tensor(name, list(shape), dt, kind="ExternalInput").ap()

    def ext_out(self, name, shape, dt=F32):
        return self.nc.dram_tensor(name, list(shape), dt, kind="ExternalOutput").ap()

    def internal(self, name, shape, dt=F32):
        return self.nc.dram_tensor(name, list(shape), dt).ap()

    def din(self, name, shape, dt=F32):
        if name in self.io:
            return self.io[name]
        assert not self.fused, name
        return self.ext_in(name, shape, dt)

    def dout(self, name, shape, dt=F32):
        if name in self.io:
            return self.io[name]
        assert not self.fused, name
        return self.ext_out(name, shape, dt)

    def sb(self, shape, dt, name=None):
        self.n += 1
        return self.es.enter_context(self.nc.sbuf_tensor(name or ("sb%d" % self.n), list(shape), dt))

    def ps(self, shape, dt, name=None):
        self.n += 1
        return self.es.enter_context(self.nc.psum_tensor(name or ("ps%d" % self.n), list(shape), dt))

    def finish(self):
        self.P.barrier()
        if self.fused:
            self.es.close()
            self.es = contextlib.ExitStack()
            self.io = {}
            return None
        self.P.emit()
        self.es.close()
        return self.nc


def norm_transpose(C, x_d, gT, xnT, identb, nchunk, D):
    P = C.P
    xt = [C.sb([128, D], F32) for _ in range(2)]
    xn = [C.sb([128, D], BF16) for _ in range(2)]
    junk = C.sb([128, D], BF16)
    ss = C.sb([128, NT], F32)
    rs = C.sb([128, NT], F32)
    pT = [C.ps([128, 8, 128], BF16) for _ in range(2)]
    for t in range(NT):
        s = t % 2
        P.dma(xt[s][:], x_d[t * 128:(t + 1) * 128, :], w=[("xt", s)])
        P.op("act", lambda e, s=s, t=t: e.activation(out=junk[:], in_=xt[s][:], func=AF.Square, accum_out=ss[:, t:t + 1]),
             r=[("xt", s)], w=["junk", ("ss", t)])
        P.op("act", lambda e, t=t: e.activation(out=rs[:, t:t + 1], in_=ss[:, t:t + 1], func=AF.Sqrt, scale=1.0 / D, bias=EPS),
             r=[("ss", t)], w=[("rs", t)])
        P.op("dve", lambda e, t=t: e.reciprocal(out=rs[:, t:t + 1], in_=rs[:, t:t + 1]), r=[("rs", t)], w=[("rs", t)])
        P.op("dve", lambda e, s=s, t=t: e.tensor_scalar(out=xn[s][:], in0=xt[s][:], scalar1=rs[:, t:t + 1], scalar2=None, op0=ALU.mult),
             r=[("xt", s), ("rs", t)], w=[("xn", s)])
        for c0 in range(0, nchunk, 8):
            b = (c0 // 8) % 2
            for c in range(c0, min(c0 + 8, nchunk)):
                P.op("pe", lambda e, c=c, s=s, b=b: e.transpose(out=pT[b][:, c % 8, :], in_=xn[s][:, c * 128:(c + 1) * 128], identity=identb[:]),
                     r=[("xn", s), "identb"], w=[("pT", b)])
            for c in range(c0, min(c0 + 8, nchunk)):
                P.op("dve", lambda e, c=c, t=t, b=b: e.tensor_scalar(out=xnT[:, c, t * 128:(t + 1) * 128], in0=pT[b][:, c % 8, :],
                                                                  scalar1=gT[:, c:c + 1], scalar2=None, op0=ALU.mult),
                     r=[("pT", b), "gT"], w=[("xnT", t)])


def load_const(C, dram, shape, dt, key, q="sp"):
    t = C.sb(shape, dt)
    C.P.dma(t[:], dram, w=[key], q=q)
    return t


def build_E1(C=None):
    C = C or Ctx()
    P = C.P
    x_d = C.din("x", [TOK, 2048])
    gT_d = C.din("gT", [128, 16])
    w_d = C.din("w", [2048, 7168])
    bgT_d = C.din("bgT", [128, 16])
    id_d = C.din("ident", [128, 128])
    outs = {n: C.dout(n, [1024, 1024], BF16) for n in ("uT", "gaT", "qT", "kT", "v", "gb")}
    identf = load_const(C, id_d, [128, 128], F32, "identf")
    gT = load_const(C, gT_d, [128, 16], F32, "gT")
    bgT = load_const(C, bgT_d, [128, 16], F32, "bgT")
    identb = C.sb([128, 128], BF16)
    P.op("dve", lambda e: e.tensor_copy(out=identb[:], in_=identf[:]), r=["identf"], w=["identb"])
    xnT = C.sb([128, 16, TOK], BF16)
    norm_transpose(C, x_d, gT, xnT, identb, 16, 2048)
    XN = [("xnT", t) for t in range(NT)]
    wb = [C.sb([128, 16, 512], BF16) for _ in range(2)]
    psb = [C.ps([128, 512], F32) for _ in range(4)]
    ob = [C.sb([128, 512], BF16) for _ in range(4)]
    tv = [C.sb([128, 512], F32) for _ in range(2)]
    tg = [C.sb([128, 512], F32) for _ in range(2)]
    w_v = w_d.rearrange("(c p) n -> p c n", p=128)
    cnt = {"ps": 0, "ob": 0, "wb": 0, "tv": 0}

    def loadw(cols):
        s = cnt["wb"] % 2
        cnt["wb"] += 1
        off = 0
        for (c0, n) in cols:
            P.dma(wb[s][:, :, off:off + n], w_v[:, :, c0:c0 + n], w=[("wb", s)], q="pool")
            off += n
        return s

    def mm_fm(s, j, h):
        b = cnt["ps"] % 4
        cnt["ps"] += 1
        for c in range(16):
            P.op("pe", lambda e, c=c, b=b: e.matmul(psb[b][:], lhsT=wb[s][:, c, j * 128:(j + 1) * 128], rhs=xnT[:, c, h * 512:(h + 1) * 512],
                                                  start=(c == 0), stop=(c == 15)),
                 r=[("wb", s)] + XN, w=[("ps", b)])
        return b

    def mm_tm(s, t):
        b = cnt["ps"] % 4
        cnt["ps"] += 1
        for c in range(16):
            P.op("pe", lambda e, c=c, b=b: e.matmul(psb[b][:], lhsT=xnT[:, c, t * 128:(t + 1) * 128], rhs=wb[s][:, c, :],
                                                  start=(c == 0), stop=(c == 15)),
                 r=[("wb", s)] + XN, w=[("ps", b)])
        return b

    def store(dst, o):
        P.dma(dst, ob[o][:], r=[("ob", o)])

    def newob():
        o = cnt["ob"] % 4
        cnt["ob"] += 1
        return o

    for g in range(4):
        s = loadw([(g * 256, 256), (1024 + g * 256, 256)])
        for jj in range(2):
            ch = 2 * g + jj
            for h in range(2):
                k = cnt["tv"] % 2
                cnt["tv"] += 1
                b = mm_fm(s, jj, h)
                P.op("act", lambda e, b=b, k=k, ch=ch: e.activation(out=tv[k][:], in_=psb[b][:], func=AF.Identity, bias=bgT[:, ch:ch + 1]),
                     r=[("ps", b), "bgT"], w=[("tv", k)])
                b2 = mm_fm(s, 2 + jj, h)
                P.op("act", lambda e, b2=b2, k=k, ch=ch: e.activation(out=tg[k][:], in_=psb[b2][:], func=AF.Sigmoid, bias=bgT[:, 8 + ch:9 + ch]),
                     r=[("ps", b2), "bgT"], w=[("tg", k)])
                o = newob()
                P.op("dve", lambda e, o=o, k=k: e.tensor_tensor(out=ob[o][:], in0=tv[k][:], in1=tg[k][:], op=ALU.mult),
                     r=[("tv", k), ("tg", k)], w=[("ob", o)])
                store(outs["uT"][ch * 128:(ch + 1) * 128, h * 512:(h + 1) * 512], o)
    for name, col0, fn in (("gaT", 2048, AF.Silu), ("qT", 3072, AF.Copy), ("kT", 4096, AF.Copy)):
        for g in range(2):
            s = loadw([(col0 + g * 512, 512)])
            for j in range(4):
                ch = g * 4 + j
                for h in range(2):
                    b = mm_fm(s, j, h)
                    o = newob()
                    P.op("act", lambda e, b=b, o=o, fn=fn: e.activation(out=ob[o][:], in_=psb[b][:], func=fn),
                         r=[("ps", b)], w=[("ob", o)])
                    store(outs[name][ch * 128:(ch + 1) * 128, h * 512:(h + 1) * 512], o)
    for name, col0, fn in (("v", 5120, AF.Copy), ("gb", 6144, AF.Silu)):
        for g in range(2):
            s = loadw([(col0 + g * 512, 512)])
            for t in range(NT):
                b = mm_tm(s, t)
                o = newob()
                P.op("act", lambda e, b=b, o=o, fn=fn: e.activation(out=ob[o][:], in_=psb[b][:], func=fn),
                     r=[("ps", b)], w=[("ob", o)])
                store(outs[name][t * 128:(t + 1) * 128, g * 512:(g + 1) * 512], o)
    return C.finish()


def build_E2a(C=None):
    C = C or Ctx()
    P = C.P
    uT_d = C.din("uT", [1024, 1024], BF16)
    uh_d = C.din("uh", [1024, 4, 32], BF16)
    gaT_d = C.din("gaT", [1024, 1024], BF16)
    wdw_d = C.din("wdwT", [128, 8, 31])
    vec_d = C.din("vecs", [128, 4, 8])
    wpw_d = C.din("wpw", [1024, 1024])
    id_d = C.din("ident", [128, 128])
    ya_d = C.dout("yaT", [1024, 1024], BF16)
    identf = load_const(C, id_d, [128, 128], F32, "identf")
    wdw = load_const(C, wdw_d, [128, 8, 31], F32, "wdw")
    vecs = load_const(C, vec_d, [128, 4, 8], F32, "vecs")
    identb = C.sb([128, 128], BF16)
    onesf = C.sb([128, 128], F32)
    P.op("dve", lambda e: e.tensor_copy(out=identb[:], in_=identf[:]), r=["identf"], w=["identb"])
    P.op("pool", lambda e: e.memset(onesf[:], 1.0), w=["onesf"])
    ucv = C.sb([128, 8, 4, 288], BF16)
    gaT = C.sb([128, 8, 1024], BF16)
    for c in range(8):
        P.dma(ucv[:, c, :, 32:288], uT_d[c * 128:(c + 1) * 128, :].rearrange("p (i t) -> p i t", i=4), w=[("ucv", c)])
        P.dma(ucv[:, c, :, 0:32], uh_d[c * 128:(c + 1) * 128, :, :], w=[("ucv", c)])
        P.dma(gaT[:, c, :], gaT_d[c * 128:(c + 1) * 128, :], w=[("gaT", c)])
    D = [C.sb([128, 31, 128], BF16) for _ in range(2)]
    ycv = C.sb([128, 8, 1024], F32)
    ysq = C.sb([128, 8, 1024], F32)
    psb = [C.ps([128, 512], F32) for _ in range(6)]
    pc = [0]

    def bank():
        b = pc[0] % 6
        pc[0] += 1
        return b
    for c in range(8):
        s = c % 2
        for k in range(31):
            P.op("pool", lambda e, s=s, c=c, k=k: e.tensor_scalar(out=D[s][:, k, :], in0=identb[:], scalar1=wdw[:, c, k:k + 1], scalar2=0.0,
                                                                op0=ALU.mult, op1=ALU.add),
                 r=["identb", "wdw"], w=[("D", s)])
        for half in range(2):
            b = bank()
            for ii in range(2):
                i = half * 2 + ii
                for k in range(31):
                    P.op("pe", lambda e, s=s, c=c, k=k, i=i, ii=ii, b=b: e.matmul(psb[b][:, ii * 256:(ii + 1) * 256], lhsT=D[s][:, k, :],
                                                                             rhs=ucv[:, c, i, 2 + k:2 + k + 256], start=(k == 0), stop=(k == 30)),
                         r=[("D", s), ("ucv", c)], w=[("ps", b)])
            P.op("act", lambda e, c=c, half=half, b=b: e.activation(out=ycv[:, c, half * 512:(half + 1) * 512], in_=psb[b][:], func=AF.Identity,
                                                                  bias=vecs[:, 0, c:c + 1]),
                 r=[("ps", b), "vecs"], w=[("ycv", c, half)])
            P.op("act", lambda e, c=c, half=half, b=b: e.activation(out=ysq[:, c, half * 512:(half + 1) * 512], in_=psb[b][:], func=AF.Square,
                                                                  bias=vecs[:, 0, c:c + 1]),
                 r=[("ps", b), "vecs"], w=[("ysq", c, half)])
    mean = C.sb([128, 1024], F32)
    msq = C.sb([128, 1024], F32)
    rstd = C.sb([128, 1024], F32)
    for half in range(2):
        sl = slice(half * 512, (half + 1) * 512)
        b1 = bank()
        for c in range(8):
            P.op("pe", lambda e, c=c, b1=b1, sl=sl: e.matmul(psb[b1][:], lhsT=onesf[:], rhs=ycv[:, c, sl], start=(c == 0), stop=(c == 7)),
                 r=["onesf", ("ycv", c, half)], w=[("ps", b1)])
        b2 = bank()
        for c in range(8):
            P.op("pe", lambda e, c=c, b2=b2, sl=sl: e.matmul(psb[b2][:], lhsT=onesf[:], rhs=ysq[:, c, sl], start=(c == 0), stop=(c == 7)),
                 r=["onesf", ("ysq", c, half)], w=[("ps", b2)])
        P.op("dve", lambda e, b1=b1, sl=sl: e.tensor_scalar(out=mean[:, sl], in0=psb[b1][:], scalar1=1.0 / 1024, scalar2=None, op0=ALU.mult),
             r=[("ps", b1)], w=[("mean", half)])
        P.op("dve", lambda e, sl=sl: e.tensor_tensor(out=msq[:, sl], in0=mean[:, sl], in1=mean[:, sl], op=ALU.mult),
             r=[("mean", half)], w=[("msq", half)])
        P.op("dve", lambda e, b2=b2, sl=sl: e.scalar_tensor_tensor(out=rstd[:, sl], in0=psb[b2][:], scalar=1.0 / 1024, in1=msq[:, sl],
                                                                 op0=ALU.mult, op1=ALU.subtract),
             r=[("ps", b2), ("msq", half)], w=[("rstd", half)])
        P.op("act", lambda e, sl=sl: e.activation(out=rstd[:, sl], in_=rstd[:, sl], func=AF.Sqrt, bias=EPS), r=[("rstd", half)], w=[("rstd", half)])
        P.op("dve", lambda e, sl=sl: e.reciprocal(out=rstd[:, sl], in_=rstd[:, sl]), r=[("rstd", half)], w=[("rstd", half)])
    yact = C.sb([128, 8, 1024], BF16)
    for c in range(8):
        for half in range(2):
            sl = slice(half * 512, (half + 1) * 512)
            P.op("dve", lambda e, c=c, sl=sl: e.tensor_tensor(out=ycv[:, c, sl], in0=ycv[:, c, sl], in1=mean[:, sl], op=ALU.subtract),
                 r=[("ycv", c, half), ("mean", half)], w=[("ycv", c, half)])
            P.op("dve", lambda e, c=c, sl=sl: e.tensor_tensor(out=ycv[:, c, sl], in0=ycv[:, c, sl], in1=rstd[:, sl], op=ALU.mult),
                 r=[("ycv", c, half), ("rstd", half)], w=[("ycv", c, half)])
            P.op("act", lambda e, c=c, sl=sl: e.activation(out=yact[:, c, sl], in_=ycv[:, c, sl], func=AF.Silu, scale=vecs[:, 1, c:c + 1],
                                                         bias=vecs[:, 2, c:c + 1]),
                 r=[("ycv", c, half), "vecs"], w=[("yact", c, half)])
    YA = [("yact", c, h) for c in range(8) for h in range(2)]
    wb = [C.sb([128, 8, 512], BF16) for _ in range(2)]
    ob = [C.sb([128, 512], BF16) for _ in range(4)]
    w_v = wpw_d.rearrange("(c p) n -> p c n", p=128)
    oc = 0
    for g in range(2):
        P.dma(wb[g][:], w_v[:, :, g * 512:(g + 1) * 512], w=[("wb", g)], q="pool")
        for j in range(4):
            ch = g * 4 + j
            for half in range(2):
                sl = slice(half * 512, (half + 1) * 512)
                b = bank()
                for c in range(8):
                    P.op("pe", lambda e, c=c, b=b, g=g, j=j, sl=sl: e.matmul(psb[b][:], lhsT=wb[g][:, c, j * 128:(j + 1) * 128], rhs=yact[:, c, sl],
                                                                          start=(c == 0), stop=(c == 7)),
                         r=[("wb", g)] + YA, w=[("ps", b)])
                o = oc % 4
                oc += 1
                P.op("dve", lambda e, b=b, o=o, ch=ch, sl=sl: e.scalar_tensor_tensor(out=ob[o][:], in0=psb[b][:], scalar=vecs[:, 3, ch:ch + 1],
                                                                                in1=gaT[:, ch, sl], op0=ALU.add, op1=ALU.mult),
                     r=[("ps", b), "vecs", ("gaT", ch)], w=[("ob", o)])
                P.dma(ya_d[ch * 128:(ch + 1) * 128, sl], ob[o][:], r=[("ob", o)])
    return C.finish()


def build_E2c(C=None):
    C = C or Ctx()
    P = C.P
    cat_d = C.din("catT", [2048, 1024], BF16)
    x_d = C.din("x", [TOK, 2048])
    w_d = C.din("w", [2048, 2048])
    xo_d = C.dout("xo", [TOK, 2048])
    cat = C.sb([128, 16, 1024], BF16)
    for c in range(16):
        P.dma(cat[:, c, :], cat_d[c * 128:(c + 1) * 128, :], w=[("cat", c)])
    CAT = [("cat", c) for c in range(16)]
    xs = C.sb([128, NT, 2048], F32)
    for t in range(NT):
        P.dma(xs[:, t, :], x_d[t * 128:(t + 1) * 128, :], w=[("xs", t)])
    wb = [C.sb([128, 16, 512], BF16) for _ in range(2)]
    psb = [C.ps([128, 512], F32) for _ in range(4)]
    ob = [C.sb([128, 512], F32) for _ in range(4)]
    w_v = w_d.rearrange("(c p) n -> p c n", p=128)
    n = 0
    for g in range(4):
        s = g % 2
        P.dma(wb[s][:], w_v[:, :, g * 512:(g + 1) * 512], w=[("wb", s)], q="pool")
        for t in range(NT):
            b = n % 4
            n += 1
            for c in range(16):
                P.op("pe", lambda e, c=c, b=b, s=s, t=t: e.matmul(psb[b][:], lhsT=cat[:, c, t * 128:(t + 1) * 128], rhs=wb[s][:, c, :],
                                                               start=(c == 0), stop=(c == 15)),
                     r=[("wb", s)] + CAT, w=[("ps", b)])
            P.op("dve", lambda e, b=b, t=t, g=g: e.tensor_tensor(out=ob[b][:], in0=psb[b][:], in1=xs[:, t, g * 512:(g + 1) * 512], op=ALU.add),
                 r=[("ps", b), ("xs", t)], w=[("ob", b)])
            P.dma(xo_d[t * 128:(t + 1) * 128, g * 512:(g + 1) * 512], ob[b][:], r=[("ob", b)])
    return C.finish()


class Gath:
    def __init__(self, parts, hr):
        self.parts = parts
        self.hr = hr

    def rows(self, j, r0, r1):
        h = r0 // self.hr
        assert (r1 - 1) // self.hr == h
        o = j * self.hr + r0 % self.hr
        return self.parts[h][o:o + (r1 - r0), :]


def as_gath(a, rows):
    if isinstance(a, Gath):
        return a
    return Gath([a.rearrange("j r c -> (j r) c")], rows)


def build_E2b(C=None):
    C = C or Ctx()
    P = C.P
    SC = 128 ** -0.5
    qT_d = C.din("qT", [1024, 1024], BF16)
    kT_d = C.din("kTall", [4, 1024, 1024], BF16)
    v_d = C.din("vall", [4, 1024, 1024], BF16)
    kT_g = as_gath(kT_d, 1024)
    v_g = as_gath(v_d, 1024)
    gb_d = C.din("gb", [1024, 1024], BF16)
    gbias_d = C.din("gbias", [128, 4, 16])
    pflag_d = C.din("pflag", [128, 4, 16])
    tmask_d = C.din("tmask", [128, 2, 4, 256])
    id_d = C.din("ident", [128, 128])
    yb_d = C.dout("ybT", [1024, 1024], BF16)
    identf = load_const(C, id_d, [128, 128], F32, "identf")
    gbias = load_const(C, gbias_d, [128, 4, 16], F32, "gbias")
    pflag = load_const(C, pflag_d, [128, 4, 16], F32, "pflag")
    tmask = load_const(C, tmask_d, [128, 2, 4, 256], F32, "tmask")
    identb = C.sb([128, 128], BF16)
    onesb = C.sb([128, 128], BF16)
    P.op("dve", lambda e: e.tensor_copy(out=identb[:], in_=identf[:]), r=["identf"], w=["identb"])
    P.op("pool", lambda e: e.memset(onesb[:], 1.0), w=["onesb"])
    gbt = C.sb([128, NT, 1024], BF16)
    for t in range(NT):
        P.dma(gbt[:, t, :], gb_d[t * 128:(t + 1) * 128, :], w=[("gbt", t)])
    kTh = [C.sb([128, 4, 1024], BF16) for _ in range(2)]
    vh = [C.sb([128, 32, 128], BF16) for _ in range(2)]
    qTh = [C.sb([128, 1024], BF16) for _ in range(2)]
    sq = C.sb([128, 4096], BF16)
    qsq = C.sb([128, 1024], BF16)
    kmf = C.sb([128, 16], F32)
    kmb = C.sb([128, 16], BF16)
    kmx = C.sb([128, 8], F32)
    kmax2 = C.sb([128, 1], F32)
    ybTh = [C.sb([128, 1024], BF16) for _ in range(2)]
    ps_g = C.ps([128, 512], F32)
    ps_s = [C.ps([128, 512], F32) for _ in range(2)]
    ps_t = [C.ps([128, 2, 2, 128], BF16) for _ in range(2)]
    ps_o = [C.ps([128, 512], F32) for _ in range(2)]
    ps_y = C.ps([128, 8, 128], BF16)
    NR = 3
    gm = [C.sb([128, 16], F32) for _ in range(NR)]
    top8 = [C.sb([128, 8], F32) for _ in range(NR)]
    sel = [C.sb([128, 16], F32) for _ in range(NR)]
    ebias = [C.sb([128, 16], F32) for _ in range(NR)]
    mq = [C.sb([128, 1], F32) for _ in range(NR)]
    rsum = [C.sb([128, 16], F32) for _ in range(NR)]
    rtot = [C.sb([128, 1], F32) for _ in range(NR)]
    s2 = [C.sb([128, 512], F32) for _ in range(2)]
    pb = [C.sb([128, 512], BF16) for _ in range(3)]
    pTs = [C.sb([128, 2, 2, 128], BF16) for _ in range(3)]
    ybt = [C.sb([128, 128], BF16) for _ in range(2)]
    cn = dict(s=0, t=0, o=0, pb=0, pts=0, s2=0, y=0)
    for h in range(8):
        hs = h % 2
        for j in range(4):
            P.dma(kTh[hs][:, j, :], kT_g.rows(j, h * 128, (h + 1) * 128), w=[("kTh", hs)])
            for hv in range(2):
                P.dma(vh[hs][:, j * 8 + hv * 4:j * 8 + hv * 4 + 4, :],
                      v_g.rows(j, hv * 512, hv * 512 + 512).rearrange("(t p) c -> p t c", p=128)[:, :, h * 128:(h + 1) * 128], w=[("vh", hs)])
        P.dma(qTh[hs][:], qT_d[h * 128:(h + 1) * 128, :], w=[("qTh", hs)])
        P.op("dve", lambda e, hs=hs: e.tensor_reduce(out=kmf[:], in_=kTh[hs][:].rearrange("p j (i t) -> p (j i) t", i=4), axis=AX.X, op=ALU.add),
             r=[("kTh", hs)], w=["kmf"])
        P.op("dve", lambda e: e.tensor_copy(out=kmb[:], in_=kmf[:]), r=["kmf"], w=["kmb"])
        P.op("act", lambda e, hs=hs: e.activation(out=sq[:], in_=kTh[hs][:].rearrange("p j t -> p (j t)"), func=AF.Square), r=[("kTh", hs)], w=["sq"])
        P.op("act", lambda e, hs=hs: e.activation(out=qsq[:], in_=qTh[hs][:], func=AF.Square), r=[("qTh", hs)], w=["qsq"])
        for cc in range(8):
            P.op("pe", lambda e, cc=cc: e.matmul(ps_g[:], lhsT=onesb[:], rhs=sq[:, cc * 512:(cc + 1) * 512], start=True, stop=True),
                 r=["onesb", "sq"], w=["ps_g"])
            P.op("dve", lambda e, cc=cc: e.tensor_reduce(out=kmx[:, cc:cc + 1], in_=ps_g[:], axis=AX.X, op=ALU.max), r=["ps_g"], w=["kmx"])
        P.op("dve", lambda e: e.tensor_reduce(out=kmax2[:], in_=kmx[:], axis=AX.X, op=ALU.max), r=["kmx"], w=["kmax2"])
        for t in range(NT):
            i, qi = t // 2, t % 2
            z = (h * NT + t) % NR
            tq = slice(t * 128, (t + 1) * 128)
            P.op("pe", lambda e, hs=hs, tq=tq: e.matmul(ps_g[:, 0:16], lhsT=qTh[hs][:, tq], rhs=kmb[:], start=True, stop=True),
                 r=[("qTh", hs), "kmb"], w=["ps_g"])
            P.op("pe", lambda e, tq=tq: e.matmul(ps_g[:, 16:17], lhsT=qsq[:, tq], rhs=onesb[:, 0:1], start=True, stop=True),
                 r=["qsq", "onesb"], w=["ps_g"])
            P.op("dve", lambda e, z=z, i=i: e.tensor_tensor(out=gm[z][:], in0=ps_g[:, 0:16], in1=gbias[:, i, :], op=ALU.add),
                 r=["ps_g", "gbias"], w=[("gm", z)])
            P.op("dve", lambda e, z=z: e.tensor_tensor(out=mq[z][:], in0=ps_g[:, 16:17], in1=kmax2[:], op=ALU.mult),
                 r=["ps_g", "kmax2"], w=[("mq", z)])
            P.op("act", lambda e, z=z: e.activation(out=mq[z][:], in_=mq[z][:], func=AF.Sqrt, scale=SC * SC), r=[("mq", z)], w=[("mq", z)])
            P.op("dve", lambda e, z=z: e.max(out=top8[z][:], in_=gm[z][:]), r=[("gm", z)], w=[("top8", z)])
            P.op("dve", lambda e, z=z: e.tensor_scalar(out=sel[z][:], in0=gm[z][:], scalar1=top8[z][:, 2:3], scalar2=None, op0=ALU.is_ge),
                 r=[("gm", z), ("top8", z)], w=[("sel", z)])
            P.op("dve", lambda e, z=z: e.tensor_scalar(out=sel[z][:], in0=sel[z][:], scalar1=-1.0, scalar2=30000.0, op0=ALU.add, op1=ALU.mult),
                 r=[("sel", z)], w=[("sel", z)])
            P.op("dve", lambda e, z=z, i=i: e.tensor_tensor(out=sel[z][:], in0=sel[z][:], in1=pflag[:, i, :], op=ALU.mult),
                 r=[("sel", z), "pflag"], w=[("sel", z)])
            P.op("dve", lambda e, z=z: e.tensor_scalar(out=ebias[z][:], in0=sel[z][:], scalar1=mq[z][:, 0:1], scalar2=None, op0=ALU.subtract),
                 r=[("sel", z), ("mq", z)], w=[("ebias", z)])
            P.op("pool", lambda e, z=z: e.memset(rsum[z][:], 0.0), w=[("rsum", z)])
            ob_ = cn["o"] % 2
            cn["o"] += 1
            pairs = [(ip, jp) for ip in range(i + 1) for jp in range(2)]
            nmm = len(pairs) * 4
            mi = 0
            for (ip, jp) in pairs:
                sb_ = cn["s"] % 2
                cn["s"] += 1
                for jj in range(2):
                    j = jp * 2 + jj
                    P.op("pe", lambda e, hs=hs, tq=tq, sb_=sb_, jj=jj, j=j, ip=ip: e.matmul(ps_s[sb_][:, jj * 256:(jj + 1) * 256], lhsT=qTh[hs][:, tq],
                                                                                     rhs=kTh[hs][:, j, ip * 256:(ip + 1) * 256], start=True, stop=True),
                         r=[("qTh", hs), ("kTh", hs)], w=[("ps_s", sb_)])
                p_ = cn["pb"] % 3
                cn["pb"] += 1
                for jj in range(2):
                    j = jp * 2 + jj
                    m = j * 4 + ip
                    src = ps_s[sb_][:, jj * 256:(jj + 1) * 256]
                    rk = [("ps_s", sb_)]
                    if ip == i:
                        k2 = cn["s2"] % 2
                        cn["s2"] += 1
                        P.op("dve", lambda e, k2=k2, src=src, qi=qi, j=j: e.tensor_tensor(out=s2[k2][:, 0:256], in0=src, in1=tmask[:, qi, j, :], op=ALU.add),
                             r=[("ps_s", sb_), "tmask"], w=[("s2", k2)])
                        src = s2[k2][:, 0:256]
                        rk = [("s2", k2)]
                    P.op("act", lambda e, src=src, p_=p_, jj=jj, z=z, m=m: e.activation(out=pb[p_][:, jj * 256:(jj + 1) * 256], in_=src, func=AF.Exp, scale=SC,
                                                                                   bias=ebias[z][:, m:m + 1], accum_out=rsum[z][:, m:m + 1]),
                         r=rk + [("ebias", z)], w=[("pb", p_), ("rsum", z)])
                tb_ = cn["t"] % 2
                cn["t"] += 1
                for jj in range(2):
                    for hf in range(2):
                        P.op("pe", lambda e, p_=p_, jj=jj, hf=hf, tb_=tb_: e.transpose(out=ps_t[tb_][:, jj, hf, :], in_=pb[p_][:, jj * 256 + hf * 128:jj * 256 + hf * 128 + 128],
                                                                                identity=identb[:]),
                             r=[("pb", p_), "identb"], w=[("ps_t", tb_)])
                q_ = cn["pts"] % 3
                cn["pts"] += 1
                P.op("dve", lambda e, q_=q_, tb_=tb_: e.tensor_copy(out=pTs[q_][:], in_=ps_t[tb_][:]), r=[("ps_t", tb_)], w=[("pTs", q_)])
                for jj in range(2):
                    j = jp * 2 + jj
                    for hf in range(2):
                        kt = j * 8 + ip * 2 + hf
                        P.op("pe", lambda e, q_=q_, jj=jj, hf=hf, kt=kt, hs=hs, ob_=ob_, mi=mi: e.matmul(ps_o[ob_][:, 0:128], lhsT=pTs[q_][:, jj, hf, :], rhs=vh[hs][:, kt, :],
                                                                                               start=(mi == 0), stop=(mi == nmm - 1)),
                             r=[("pTs", q_), ("vh", hs)], w=[("ps_o", ob_)])
                        mi += 1
            P.op("dve", lambda e, z=z: e.tensor_reduce(out=rtot[z][:], in_=rsum[z][:], axis=AX.X, op=ALU.add), r=[("rsum", z)], w=[("rtot", z)])
            P.op("dve", lambda e, z=z: e.reciprocal(out=rtot[z][:], in_=rtot[z][:]), r=[("rtot", z)], w=[("rtot", z)])
            y_ = cn["y"] % 2
            cn["y"] += 1
            P.op("dve", lambda e, z=z, ob_=ob_, y_=y_, t=t, h=h: e.scalar_tensor_tensor(out=ybt[y_][:], in0=ps_o[ob_][:, 0:128], scalar=rtot[z][:, 0:1],
                                                                                 in1=gbt[:, t, h * 128:(h + 1) * 128], op0=ALU.mult, op1=ALU.mult),
                 r=[("ps_o", ob_), ("rtot", z), ("gbt", t)], w=[("ybt", y_)])
            P.op("pe", lambda e, y_=y_, t=t: e.transpose(out=ps_y[:, t, :], in_=ybt[y_][:], identity=identb[:]), r=[("ybt", y_), "identb"], w=["ps_y"])
        P.op("act", lambda e, hs=hs: e.copy(out=ybTh[hs][:], in_=ps_y[:].rearrange("p t q -> p (t q)")), r=["ps_y"], w=[("ybTh", hs)])
        P.dma(yb_d[h * 128:(h + 1) * 128, :], ybTh[hs][:], r=[("ybTh", hs)])
    return C.finish()


def local_tokens(c):
    b, r = c // 4, c % 4
    idx = np.concatenate([np.arange((4 * ii + r) * 256, (4 * ii + r + 1) * 256) for ii in range(4)])
    return b, idx


_CACHE = {}


def prog(name):
    if name not in _CACHE:
        _CACHE[name] = globals()["build_" + name]()
    return _CACHE[name]


def launch(name, ins):
    res = run_bass_kernel_spmd(prog(name), ins, core_ids=list(range(8)))
    return res.results


IDENT = np.eye(128, dtype=np.float32)


def fmvec(v):
    return np.ascontiguousarray(v.reshape(-1, 128).T)


def moba_consts(r):
    gbias = np.zeros((4, 16), np.float32)
    pflag = np.zeros((4, 16), np.float32)
    for i in range(4):
        for j in range(4):
            for ip in range(4):
                past = (4 * ip + j) < (4 * i + r)
                gbias[i, j * 4 + ip] = 0.0 if past else -1e30
                pflag[i, j * 4 + ip] = 1.0 if past else 0.0
    tm = np.zeros((128, 2, 4, 256), np.float32)
    for qi in range(2):
        for j in range(4):
            if j > r:
                tm[:, qi, j, :] = -30000.0
            elif j == r:
                qpos = qi * 128 + np.arange(128)[:, None]
                tm[:, qi, j, :] = np.where(np.arange(256)[None, :] <= qpos, 0.0, -30000.0)
    return (np.ascontiguousarray(np.broadcast_to(gbias, (128, 4, 16))), np.ascontiguousarray(np.broadcast_to(pflag, (128, 4, 16))), tm)


def even_layer(xl, p):
    ins = [dict(x=xl[c], gT=fmvec(p["norm"]), w=p["w_in"], bgT=fmvec(p["b_glu"]), ident=IDENT) for c in range(8)]
    e1 = launch("E1", ins)
    ins = []
    wdwT = np.ascontiguousarray(p["w_dw"].T.reshape(8, 128, 31).transpose(1, 0, 2))
    vecs = np.ascontiguousarray(np.stack([fmvec(p["b_dw"]), fmvec(p["ln_g"]), fmvec(p["ln_b"]), fmvec(p["b_pw"])], axis=1))
    for c in range(8):
        b, r = c // 4, c % 4
        uh = np.zeros((1024, 4, 32), NPBF)
        for i in range(4):
            gblk = 4 * i + r - 1
            if gblk < 0:
                continue
            src = e1[b * 4 + gblk % 4]["uT"]
            li = gblk // 4
            uh[:, i, :] = src[:, li * 256 + 224: li * 256 + 256]
        ins.append(dict(uT=e1[c]["uT"], uh=uh, gaT=e1[c]["gaT"], wdwT=wdwT, vecs=vecs, wpw=p["w_pw"], ident=IDENT))
    e2a = launch("E2a", ins)
    ins = []
    for c in range(8):
        b, r = c // 4, c % 4
        kT = np.stack([e1[b * 4 + j]["kT"] for j in range(4)])
        vv = np.stack([e1[b * 4 + j]["v"] for j in range(4)])
        gbias, pflag, tm = moba_consts(r)
        ins.append(dict(qT=e1[c]["qT"], kTall=kT, vall=vv, gb=e1[c]["gb"], gbias=gbias, pflag=pflag, tmask=tm, ident=IDENT))
    e2b = launch("E2b", ins)
    ins = [dict(catT=np.concatenate([e2a[c]["yaT"], e2b[c]["ybT"]], axis=0), x=xl[c], w=p["w_out"]) for c in range(8)]
    e2c = launch("E2c", ins)
    return [e2c[c]["xo"] for c in range(8)]


def build_O1(C=None):
    C = C or Ctx()
    P = C.P
    x_d = C.din("x", [TOK, 2048])
    gT_d = C.din("gT", [128, 16])
    w_d = C.din("w", [2048, 2896])
    qnT_d = C.din("qnT", [128, 4])
    rows_d = C.din("rows", [128, 3, 256])
    wqb_d = C.din("wqb", [512, 2048])
    wiq_d = C.din("wiq", [512, 1024])
    wuk_d = C.din("wuk", [16, 128, 256])
    id_d = C.din("ident", [128, 128])
    ckv_o = C.dout("ckv", [1024, 256], BF16)
    ckvT_o = C.dout("ckvT", [256, 1024], BF16)
    ikT_o = C.dout("ikT", [64, 1024], BF16)
    iw_o = C.dout("iw", [1024, 16])
    gT_o = C.dout("gateT", [2048, 1024], BF16)
    ql_o = C.dout("qlatT", [16, 256, 1024], BF16)
    iq_o = C.dout("iqT", [1024, 1024], BF16)
    identf = load_const(C, id_d, [128, 128], F32, "identf")
    gT = load_const(C, gT_d, [128, 16], F32, "gT")
    qnT = load_const(C, qnT_d, [128, 4], F32, "qnT")
    rows = load_const(C, rows_d, [128, 3, 256], F32, "rows")
    identb = C.sb([128, 128], BF16)
    P.op("dve", lambda e: e.tensor_copy(out=identb[:], in_=identf[:]), r=["identf"], w=["identb"])
    xnT = C.sb([128, 16, TOK], BF16)
    norm_transpose(C, x_d, gT, xnT, identb, 16, 2048)
    XN = [("xnT", t) for t in range(NT)]
    w_v = w_d.rearrange("(c p) n -> p c n", p=128)
    wA = C.sb([128, 16, 512], BF16)
    wB = C.sb([128, 16, 336], BF16)
    P.dma(wA[:], w_v[:, :, 0:512], w=["wA"], q="pool")
    P.dma(wB[:], w_v[:, :, 512:848], w=["wB"], q="pool")
    psb = [C.ps([128, 512], F32) for _ in range(4)]
    psT = [C.ps([128, 8, 128], BF16) for _ in range(2)]
    pc = [0]

    def bank():
        b = pc[0] % 4
        pc[0] += 1
        return b
    cqT = C.sb([128, 4, TOK], BF16)
    cqf = C.sb([128, 512], F32)
    cqn = C.sb([128, 512], BF16)
    bf = C.sb([128, 336], F32)
    junk = C.sb([128, 512], F32)
    st = C.sb([128, 8], F32)
    ckvn = C.sb([128, 256], BF16)
    ckvTt = C.sb([128, 2, 128], BF16)
    ikc = C.sb([128, 64], F32)
    ikn = C.sb([128, 64], BF16)
    ikTt = C.sb([64, 128], BF16)
    iws = C.sb([128, 16], F32)
    for t in range(NT):
        tq = slice(t * 128, (t + 1) * 128)
        b = bank()
        for c in range(16):
            P.op("pe", lambda e, c=c, b=b, tq=tq: e.matmul(psb[b][:], lhsT=xnT[:, c, tq], rhs=wA[:, c, :], start=(c == 0), stop=(c == 15)),
                 r=["wA"] + XN, w=[("ps", b)])
        P.op("act", lambda e, b=b: e.copy(out=cqf[:], in_=psb[b][:]), r=[("ps", b)], w=["cqf"])
        P.op("act", lambda e: e.activation(out=junk[:], in_=cqf[:], func=AF.Square, accum_out=st[:, 0:1]), r=["cqf"], w=["junk", "st0"])
        P.op("act", lambda e: e.activation(out=st[:, 0:1], in_=st[:, 0:1], func=AF.Sqrt, scale=1.0 / 512, bias=EPS), r=["st0"], w=["st0"])
        P.op("dve", lambda e: e.reciprocal(out=st[:, 0:1], in_=st[:, 0:1]), r=["st0"], w=["st0"])
        P.op("dve", lambda e: e.tensor_scalar(out=cqn[:], in0=cqf[:], scalar1=st[:, 0:1], scalar2=None, op0=ALU.mult), r=["cqf", "st0"], w=["cqn"])
        for c in range(4):
            P.op("pe", lambda e, c=c: e.transpose(out=psT[0][:, c, :], in_=cqn[:, c * 128:(c + 1) * 128], identity=identb[:]), r=["cqn", "identb"], w=["psT0"])
        for c in range(4):
            P.op("dve", lambda e, c=c, tq=tq: e.tensor_scalar(out=cqT[:, c, tq], in0=psT[0][:, c, :], scalar1=qnT[:, c:c + 1], scalar2=None, op0=ALU.mult),
                 r=["psT0", "qnT"], w=[("cqT", t)])
        b = bank()
        for c in range(16):
            P.op("pe", lambda e, c=c, b=b, tq=tq: e.matmul(psb[b][:, 0:336], lhsT=xnT[:, c, tq], rhs=wB[:, c, :], start=(c == 0), stop=(c == 15)),
                 r=["wB"] + XN, w=[("ps", b)])
        P.op("act", lambda e, b=b: e.copy(out=bf[:], in_=psb[b][:, 0:336]), r=[("ps", b)], w=["bf"])
        P.op("act", lambda e: e.activation(out=junk[:, 0:256], in_=bf[:, 0:256], func=AF.Square, accum_out=st[:, 1:2]), r=["bf"], w=["junk", "st1"])
        P.op("act", lambda e: e.activation(out=st[:, 1:2], in_=st[:, 1:2], func=AF.Sqrt, scale=1.0 / 256, bias=EPS), r=["st1"], w=["st1"])
        P.op("dve", lambda e: e.reciprocal(out=st[:, 1:2], in_=st[:, 1:2]), r=["st1"], w=["st1"])
        P.op("dve", lambda e: e.scalar_tensor_tensor(out=ckvn[:], in0=bf[:, 0:256], scalar=st[:, 1:2], in1=rows[:, 0, :], op0=ALU.mult, op1=ALU.mult),
             r=["bf", "st1", "rows"], w=["ckvn"])
        P.dma(ckv_o[tq, :], ckvn[:], r=["ckvn"])
        for c in range(2):
            P.op("pe", lambda e, c=c: e.transpose(out=psT[1][:, c, :], in_=ckvn[:, c * 128:(c + 1) * 128], identity=identb[:]), r=["ckvn", "identb"], w=["psT1"])
        P.op("act", lambda e: e.copy(out=ckvTt[:], in_=psT[1][:, 0:2, :]), r=["psT1"], w=["ckvTt"])
        P.dma(ckvT_o[:, tq].rearrange("(c p) t -> p c t", p=128), ckvTt[:], r=["ckvTt"])
        P.op("dve", lambda e: e.tensor_reduce(out=st[:, 2:3], in_=bf[:, 256:320], axis=AX.X, op=ALU.add), r=["bf"], w=["st2"])
        P.op("dve", lambda e: e.tensor_scalar(out=st[:, 2:3], in0=st[:, 2:3], scalar1=1.0 / 64, scalar2=None, op0=ALU.mult), r=["st2"], w=["st2"])
        P.op("dve", lambda e: e.tensor_scalar(out=ikc[:], in0=bf[:, 256:320], scalar1=st[:, 2:3], scalar2=None, op0=ALU.subtract), r=["bf", "st2"], w=["ikc"])
        P.op("act", lambda e: e.activation(out=junk[:, 0:64], in_=ikc[:], func=AF.Square, accum_out=st[:, 3:4]), r=["ikc"], w=["junk", "st3"])
        P.op("act", lambda e: e.activation(out=st[:, 3:4], in_=st[:, 3:4], func=AF.Sqrt, scale=1.0 / 64, bias=EPS), r=["st3"], w=["st3"])
        P.op("dve", lambda e: e.reciprocal(out=st[:, 3:4], in_=st[:, 3:4]), r=["st3"], w=["st3"])
        P.op("dve", lambda e: e.scalar_tensor_tensor(out=ikc[:], in0=ikc[:], scalar=st[:, 3:4], in1=rows[:, 1, 0:64], op0=ALU.mult, op1=ALU.mult),
             r=["ikc", "st3", "rows"], w=["ikc"])
        P.op("dve", lambda e: e.tensor_tensor(out=ikn[:], in0=ikc[:], in1=rows[:, 2, 0:64], op=ALU.add), r=["ikc", "rows"], w=["ikn"])
        P.op("pe", lambda e: e.transpose(out=psT[1][0:64, 4, :], in_=ikn[:], identity=identb[:]), r=["ikn", "identb"], w=["psT1"])
        P.op("act", lambda e: e.copy(out=ikTt[:], in_=psT[1][0:64, 4, :]), r=["psT1"], w=["ikTt"])
        P.dma(ikT_o[:, tq], ikTt[:], r=["ikTt"])
        P.op("dve", lambda e: e.tensor_scalar(out=iws[:], in0=bf[:, 320:336], scalar1=1.0 / 32, scalar2=None, op0=ALU.mult), r=["bf"], w=["iws"])
        P.dma(iw_o[tq, :], iws[:], r=["iws"])
    CQ = [("cqT", t) for t in range(NT)]
    wb = [C.sb([128, 16, 512], BF16) for _ in range(2)]
    ob = [C.sb([128, 512], BF16) for _ in range(4)]
    oc = [0]

    def newob():
        o = oc[0] % 4
        oc[0] += 1
        return o
    for g in range(4):
        s = g % 2
        P.dma(wb[s][:], w_v[:, :, 848 + g * 512:848 + (g + 1) * 512], w=[("wb", s)], q="pool")
        for j in range(4):
            ch = g * 4 + j
            for h in range(2):
                b = bank()
                for c in range(16):
                    P.op("pe", lambda e, c=c, b=b, s=s, j=j, h=h: e.matmul(psb[b][:], lhsT=wb[s][:, c, j * 128:(j + 1) * 128], rhs=xnT[:, c, h * 512:(h + 1) * 512],
                                                                        start=(c == 0), stop=(c == 15)),
                         r=[("wb", s)] + XN, w=[("ps", b)])
                o = newob()
                P.op("act", lambda e, b=b, o=o: e.activation(out=ob[o][:], in_=psb[b][:], func=AF.Silu), r=[("ps", b)], w=[("ob", o)])
                P.dma(gT_o[ch * 128:(ch + 1) * 128, h * 512:(h + 1) * 512], ob[o][:], r=[("ob", o)])
    wqb = C.sb([128, 4, 2048], BF16)
    wiq = C.sb([128, 4, 1024], BF16)
    wuk = C.sb([128, 16, 256], BF16)
    P.dma(wqb[:], wqb_d.rearrange("(c p) n -> p c n", p=128), w=["wqb"], q="pool")
    P.dma(wiq[:], wiq_d.rearrange("(c p) n -> p c n", p=128), w=["wiq"], q="pool")
    P.dma(wuk[:], wuk_d.rearrange("h d c -> d h c"), w=["wuk"], q="pool")
    qTh = [C.sb([128, 1024], BF16) for _ in range(2)]
    for h in range(16):
        s = h % 2
        for hf in range(2):
            b = bank()
            for c in range(4):
                P.op("pe", lambda e, c=c, b=b, h=h, hf=hf: e.matmul(psb[b][:], lhsT=wqb[:, c, h * 128:(h + 1) * 128], rhs=cqT[:, c, hf * 512:(hf + 1) * 512],
                                                                 start=(c == 0), stop=(c == 3)),
                     r=["wqb"] + CQ, w=[("ps", b)])
            P.op("act", lambda e, b=b, s=s, hf=hf: e.copy(out=qTh[s][:, hf * 512:(hf + 1) * 512], in_=psb[b][:]), r=[("ps", b)], w=[("qTh", s, hf)])
        for cc in range(2):
            for hf in range(2):
                b = bank()
                P.op("pe", lambda e, b=b, h=h, cc=cc, hf=hf, s=s: e.matmul(psb[b][:], lhsT=wuk[:, h, cc * 128:(cc + 1) * 128], rhs=qTh[s][:, hf * 512:(hf + 1) * 512],
                                                                        start=True, stop=True),
                     r=["wuk", ("qTh", s, hf)], w=[("ps", b)])
                o = newob()
                P.op("act", lambda e, b=b, o=o: e.copy(out=ob[o][:], in_=psb[b][:]), r=[("ps", b)], w=[("ob", o)])
                P.dma(ql_o[h, cc * 128:(cc + 1) * 128, hf * 512:(hf + 1) * 512], ob[o][:], r=[("ob", o)])
    for ch in range(8):
        for hf in range(2):
            b = bank()
            for c in range(4):
                P.op("pe", lambda e, c=c, b=b, ch=ch, hf=hf: e.matmul(psb[b][:], lhsT=wiq[:, c, ch * 128:(ch + 1) * 128], rhs=cqT[:, c, hf * 512:(hf + 1) * 512],
                                                                   start=(c == 0), stop=(c == 3)),
                     r=["wiq"] + CQ, w=[("ps", b)])
            o = newob()
            P.op("act", lambda e, b=b, o=o: e.copy(out=ob[o][:], in_=psb[b][:]), r=[("ps", b)], w=[("ob", o)])
            P.dma(iq_o[ch * 128:(ch + 1) * 128, hf * 512:(hf + 1) * 512], ob[o][:], r=[("ob", o)])
    return C.finish()


def interleave(g1, g2):
    gens = [g for g in (g1, g2) if g is not None]
    while gens:
        for g in list(gens):
            try:
                next(g)
            except StopIteration:
                gens.remove(g)


NIT = 18


def build_O2(C=None):
    C = C or Ctx()
    P = C.P
    SC = 128 ** -0.5
    ql_d = C.din("qlatT", [16, 256, 1024], BF16)
    iq_d = C.din("iqT", [1024, 1024], BF16)
    iw_d = C.din("iw", [1024, 16])
    g_d = C.din("gateT", [2048, 1024], BF16)
    ckvT_d = C.din("ckvTall", [4, 256, 1024], BF16)
    ckv_d = C.din("ckvall", [4, 1024, 256], BF16)
    ikT_d = C.din("ikTall", [4, 64, 1024], BF16)
    tmask_d = C.din("tmask", [128, 2, 4, 256])
    wuv_d = C.din("wuv", [16, 256, 128])
    id_d = C.din("ident", [128, 128])
    cat_o = C.dout("catT", [2048, 1024], BF16)
    identf = load_const(C, id_d, [128, 128], F32, "identf")
    tmask = load_const(C, tmask_d, [128, 2, 4, 256], F32, "tmask")
    identb = C.sb([128, 128], BF16)
    onesb = C.sb([128, 128], BF16)
    P.op("dve", lambda e: e.tensor_copy(out=identb[:], in_=identf[:]), r=["identf"], w=["identb"])
    P.op("pool", lambda e: e.memset(onesb[:], 1.0), w=["onesb"])
    ckvT = C.sb([128, 2, 4, 1024], BF16)
    ckva = C.sb([128, 32, 257], BF16)
    ikT2 = C.sb([128, 4, 1024], BF16)
    iqT = C.sb([128, 8, 1024], BF16)
    iwt = C.sb([128, NT, 16], F32)
    wuv = C.sb([128, 16, 2, 128], BF16)
    P.op("pool", lambda e: e.memset(ckva[:], 1.0), w=["ckva"])
    for j in range(4):
        P.dma(ckvT[:, :, j, :], ckvT_d[j].rearrange("(c p) t -> p c t", p=128), w=["ckvT"])
        P.dma(ckva[:, j * 8:(j + 1) * 8, 0:256], ckv_d[j].rearrange("(t p) c -> p t c", p=128), w=["ckva"])
        P.dma(ikT2[0:64, j, :], ikT_d[j], w=["ikT2"])
        P.dma(ikT2[64:128, j, :], ikT_d[j], w=["ikT2"])
    for c in range(8):
        P.dma(iqT[:, c, :], iq_d[c * 128:(c + 1) * 128, :], w=["iqT"])
    P.dma(iwt[:], iw_d.rearrange("(t p) h -> p t h", p=128), w=["iwt"])
    P.dma(wuv[:], wuv_d.rearrange("h (cc c) d -> c h cc d", cc=2), w=["wuv"], q="pool")
    junk = C.sb([128, 4096], BF16)
    kmx = C.sb([128, 16], F32)
    kmax2 = C.sb([128, 1], F32)
    ps_g = C.ps([128, 512], F32)
    ps_s = [C.ps([128, 512], F32) for _ in range(2)]
    ps_t = [C.ps([128, 4, 128], BF16) for _ in range(2)]
    ps_o = [C.ps([128, 512], F32) for _ in range(2)]
    ps_y = C.ps([128, 512], F32)
    for cc in range(2):
        P.op("act", lambda e, cc=cc: e.activation(out=junk[:], in_=ckvT[:, cc, :, :].rearrange("p j t -> p (j t)"), func=AF.Square), r=["ckvT"], w=["junk"])
        for k in range(8):
            P.op("pe", lambda e, k=k: e.matmul(ps_g[:], lhsT=onesb[:], rhs=junk[:, k * 512:(k + 1) * 512], start=True, stop=True), r=["onesb", "junk"], w=["ps_g"])
            P.op("dve", lambda e, k=k, cc=cc: e.tensor_reduce(out=kmx[:, cc * 8 + k:cc * 8 + k + 1], in_=ps_g[:], axis=AX.X, op=ALU.max), r=["ps_g"], w=["kmx"])
    km2 = C.sb([128, 2], F32)
    P.op("dve", lambda e: e.tensor_reduce(out=km2[:], in_=kmx[:].rearrange("p (a b) -> p a b", a=2), axis=AX.X, op=ALU.max), r=["kmx"], w=["km2"])
    P.op("dve", lambda e: e.tensor_reduce(out=kmax2[:], in_=km2[:], axis=AX.X, op=ALU.add), r=["km2"], w=["kmax2"])
    score = [C.sb([128, 4096], F32) for _ in range(2)]
    m01 = [C.sb([128, 4096], BF16) for _ in range(2)]
    tmp = [C.sb([128, 512], F32) for _ in range(3)]
    lo = [C.sb([128, 1], F32) for _ in range(2)]
    mid = [C.sb([128, 1], F32) for _ in range(2)]
    cntt = [C.sb([128, 1], F32) for _ in range(2)]
    ge = [C.sb([128, 1], F32) for _ in range(2)]
    qlt = [C.sb([128, 16, 2, 128], BF16) for _ in range(2)]
    gTt = [C.sb([128, 16, 128], BF16) for _ in range(2)]
    qsq = C.sb([128, 16, 2, 128], BF16)
    mq = [C.sb([128, 16], F32) for _ in range(2)]
    pb = [C.sb([128, 512], BF16) for _ in range(3)]
    pTs = [C.sb([128, 4, 128], BF16) for _ in range(3)]
    on = [C.sb([128, 256], BF16) for _ in range(2)]
    rinv = [C.sb([128, 1], F32) for _ in range(2)]
    olT = [C.sb([128, 2, 128], BF16) for _ in range(2)]
    catt = [C.sb([128, 16, 128], BF16) for _ in range(2)]
    cn = dict(s=0, t=0, o=0, pb=0, pts=0, tmp=0)

    def chunks_of(i):
        nk = (i + 1) * 256
        return nk, [(j, k0, min(512, nk - k0)) for j in range(4) for k0 in range(0, nk, 512)]

    def stageA(t):
        i, qi = t // 2, t % 2
        par = t % 2
        tq = slice(t * 128, (t + 1) * 128)
        nk, chunks = chunks_of(i)
        SK, MK = ("score", par), ("m01", par)
        P.dma(qlt[par][:], ql_d[:, :, tq].rearrange("h (cc c) q -> c h cc q", cc=2), w=[("qlt", par)])
        P.dma(gTt[par][:], g_d[:, tq].rearrange("(h p) q -> p h q", p=128), w=[("gTt", par)])
        for (j, k0, n) in chunks:
            for hh in range(16):
                sb_ = cn["s"] % 2
                cn["s"] += 1
                pr = slice((hh % 2) * 64, (hh % 2) * 64 + 64)
                P.op("pe", lambda e, sb_=sb_, hh=hh, pr=pr, j=j, k0=k0, n=n, tq=tq: e.matmul(ps_s[sb_][:, 0:n], lhsT=iqT[pr, hh // 2, tq], rhs=ikT2[pr, j, k0:k0 + n],
                                                                                      start=True, stop=True),
                     r=["iqT", "ikT2"], w=[("ps_s", sb_)])
                dst = score[par][:, j * nk + k0:j * nk + k0 + n]
                if hh == 0:
                    P.op("dve", lambda e, sb_=sb_, n=n, dst=dst, t=t, hh=hh: e.tensor_scalar(out=dst, in0=ps_s[sb_][:, 0:n], scalar1=0.0, scalar2=iwt[:, t, hh:hh + 1],
                                                                                      op0=ALU.max, op1=ALU.mult),
                         r=[("ps_s", sb_), "iwt"], w=[SK])
                else:
                    k2 = cn["tmp"] % 3
                    cn["tmp"] += 1
                    P.op("dve", lambda e, sb_=sb_, n=n, k2=k2, t=t, hh=hh: e.tensor_scalar(out=tmp[k2][:, 0:n], in0=ps_s[sb_][:, 0:n], scalar1=0.0, scalar2=iwt[:, t, hh:hh + 1],
                                                                                    op0=ALU.max, op1=ALU.mult),
                         r=[("ps_s", sb_), "iwt"], w=[("tmp", k2)])
                    P.op("pool", lambda e, dst=dst, k2=k2, n=n: e.tensor_tensor(out=dst, in0=dst, in1=tmp[k2][:, 0:n], op=ALU.add), r=[("tmp", k2), SK], w=[SK])
                yield
        for j in range(4):
            dst = score[par][:, j * nk + i * 256:j * nk + (i + 1) * 256]
            P.op("dve", lambda e, dst=dst, j=j, qi=qi: e.tensor_tensor(out=dst, in0=dst, in1=tmask[:, qi, j, :], op=ALU.add), r=[SK, "tmask"], w=[SK])
        sc = score[par][:, 0:4 * nk]
        P.op("dve", lambda e: e.tensor_reduce(out=lo[par][:], in_=sc, axis=AX.X, op=ALU.max), r=[SK], w=[("lo", par)])
        P.op("dve", lambda e: e.tensor_scalar(out=lo[par][:], in0=lo[par][:], scalar1=-16.0, scalar2=None, op0=ALU.add), r=[("lo", par)], w=[("lo", par)])
        yield
        for it in range(NIT):
            step = 16.0 / 2 ** (it + 1)
            P.op("dve", lambda e, step=step: e.tensor_scalar(out=mid[par][:], in0=lo[par][:], scalar1=step, scalar2=None, op0=ALU.add), r=[("lo", par)], w=[("mid", par)])
            P.op("dve", lambda e: e.tensor_scalar(out=junk[:, 0:4 * nk], in0=sc, scalar1=mid[par][:, 0:1], scalar2=None, op0=ALU.is_ge, op1=ALU.add, accum_out=cntt[par][:]),
                 r=[SK, ("mid", par)], w=[("cntt", par)])
            P.op("dve", lambda e, step=step: e.tensor_scalar(out=ge[par][:], in0=cntt[par][:], scalar1=255.5, scalar2=step, op0=ALU.is_ge, op1=ALU.mult),
                 r=[("cntt", par)], w=[("ge", par)])
            P.op("dve", lambda e: e.tensor_tensor(out=lo[par][:], in0=lo[par][:], in1=ge[par][:], op=ALU.add), r=[("lo", par), ("ge", par)], w=[("lo", par)])
            yield
        P.op("dve", lambda e: e.tensor_scalar(out=m01[par][:, 0:4 * nk], in0=sc, scalar1=lo[par][:, 0:1], scalar2=None, op0=ALU.is_ge), r=[SK, ("lo", par)], w=[MK])
        P.op("act", lambda e: e.activation(out=qsq[:], in_=qlt[par][:], func=AF.Square), r=[("qlt", par)], w=["qsq"])
        for hh in range(16):
            for cc in range(2):
                P.op("pe", lambda e, hh=hh, cc=cc: e.matmul(ps_g[:, hh:hh + 1], lhsT=qsq[:, hh, cc, :], rhs=onesb[:, 0:1], start=(cc == 0), stop=(cc == 1)),
                     r=["qsq", "onesb"], w=["ps_g"])
        P.op("dve", lambda e: e.tensor_scalar(out=mq[par][:], in0=ps_g[:, 0:16], scalar1=kmax2[:, 0:1], scalar2=None, op0=ALU.mult), r=["ps_g", "kmax2"], w=[("mq", par)])
        P.op("act", lambda e: e.activation(out=mq[par][:], in_=mq[par][:], func=AF.Sqrt, scale=SC * SC), r=[("mq", par)], w=[("mq", par)])
        P.op("dve", lambda e: e.tensor_scalar(out=mq[par][:], in0=mq[par][:], scalar1=-1.0, scalar2=None, op0=ALU.mult), r=[("mq", par)], w=[("mq", par)])
        yield

    def stageB(t):
        i = t // 2
        par = t % 2
        tq = slice(t * 128, (t + 1) * 128)
        nk, chunks = chunks_of(i)
        MK = ("m01", par)
        nmm = sum(n // 128 for (_, _, n) in chunks)
        for hh in range(16):
            ob_ = cn["o"] % 2
            cn["o"] += 1
            mi = 0
            for (j, k0, n) in chunks:
                sb_ = cn["s"] % 2
                cn["s"] += 1
                for cc in range(2):
                    P.op("pe", lambda e, sb_=sb_, hh=hh, cc=cc, j=j, k0=k0, n=n: e.matmul(ps_s[sb_][:, 0:n], lhsT=qlt[par][:, hh, cc, :], rhs=ckvT[:, cc, j, k0:k0 + n],
                                                                                   start=(cc == 0), stop=(cc == 1)),
                         r=[("qlt", par), "ckvT"], w=[("ps_s", sb_)])
                p_ = cn["pb"] % 3
                cn["pb"] += 1
                P.op("act", lambda e, sb_=sb_, p_=p_, n=n, hh=hh: e.activation(out=pb[p_][:, 0:n], in_=ps_s[sb_][:, 0:n], func=AF.Exp, scale=SC, bias=mq[par][:, hh:hh + 1]),
                     r=[("ps_s", sb_), ("mq", par)], w=[("pb", p_)])
                P.op("pool", lambda e, p_=p_, n=n, j=j, k0=k0: e.tensor_tensor(out=pb[p_][:, 0:n], in0=pb[p_][:, 0:n], in1=m01[par][:, j * nk + k0:j * nk + k0 + n], op=ALU.mult),
                     r=[("pb", p_), MK], w=[("pb", p_)])
                tb_ = cn["t"] % 2
                cn["t"] += 1
                for a in range(n // 128):
                    P.op("pe", lambda e, p_=p_, a=a, tb_=tb_: e.transpose(out=ps_t[tb_][:, a, :], in_=pb[p_][:, a * 128:(a + 1) * 128], identity=identb[:]),
                         r=[("pb", p_), "identb"], w=[("ps_t", tb_)])
                q_ = cn["pts"] % 3
                cn["pts"] += 1
                P.op("act", lambda e, q_=q_, tb_=tb_, n=n: e.copy(out=pTs[q_][:, 0:n // 128, :], in_=ps_t[tb_][:, 0:n // 128, :]), r=[("ps_t", tb_)], w=[("pTs", q_)])
                for a in range(n // 128):
                    kt = j * 8 + k0 // 128 + a
                    P.op("pe", lambda e, q_=q_, a=a, kt=kt, ob_=ob_, mi=mi: e.matmul(ps_o[ob_][:, 0:257], lhsT=pTs[q_][:, a, :], rhs=ckva[:, kt, :],
                                                                              start=(mi == 0), stop=(mi == nmm - 1)),
                         r=[("pTs", q_), "ckva"], w=[("ps_o", ob_)])
                    mi += 1
                yield
            z = hh % 2
            P.op("dve", lambda e, z=z, ob_=ob_: e.reciprocal(out=rinv[z][:], in_=ps_o[ob_][:, 256:257]), r=[("ps_o", ob_)], w=[("rinv", z)])
            P.op("dve", lambda e, z=z, ob_=ob_: e.tensor_scalar(out=on[z][:], in0=ps_o[ob_][:, 0:256], scalar1=rinv[z][:, 0:1], scalar2=None, op0=ALU.mult),
                 r=[("ps_o", ob_), ("rinv", z)], w=[("on", z)])
            tb_ = cn["t"] % 2
            cn["t"] += 1
            for cc in range(2):
                P.op("pe", lambda e, z=z, cc=cc, tb_=tb_: e.transpose(out=ps_t[tb_][:, cc, :], in_=on[z][:, cc * 128:(cc + 1) * 128], identity=identb[:]),
                     r=[("on", z), "identb"], w=[("ps_t", tb_)])
            P.op("act", lambda e, z=z, tb_=tb_: e.copy(out=olT[z][:], in_=ps_t[tb_][:, 0:2, :]), r=[("ps_t", tb_)], w=[("olT", z)])
            for cc in range(2):
                P.op("pe", lambda e, z=z, cc=cc, hh=hh: e.matmul(ps_y[:, 0:128], lhsT=wuv[:, hh, cc, :], rhs=olT[z][:, cc, :], start=(cc == 0), stop=(cc == 1)),
                     r=[("olT", z), "wuv"], w=["ps_y"])
            P.op("dve", lambda e, hh=hh: e.tensor_tensor(out=catt[par][:, hh, :], in0=ps_y[:, 0:128], in1=gTt[par][:, hh, :], op=ALU.mult),
                 r=["ps_y", ("gTt", par)], w=[("catt", par)])
            yield
        P.dma(cat_o[:, tq].rearrange("(h p) q -> p h q", p=128), catt[par][:], r=[("catt", par)])
        yield

    interleave(stageA(0), None)
    for t in range(NT):
        interleave(stageB(t), stageA(t + 1) if t + 1 < NT else None)
    return C.finish()


def build_F(C=None):
    C = C or Ctx()
    P = C.P
    x_d = C.din("x", [TOK, 2048])
    g_d = C.din("grow", [128, 2048])
    o_d = C.dout("y", [TOK, 2048])
    grow = load_const(C, g_d, [128, 2048], F32, "grow")
    xt = [C.sb([128, 2048], F32) for _ in range(2)]
    yo = [C.sb([128, 2048], F32) for _ in range(2)]
    junk = C.sb([128, 2048], BF16)
    ss = C.sb([128, NT], F32)
    for t in range(NT):
        s = t % 2
        P.dma(xt[s][:], x_d[t * 128:(t + 1) * 128, :], w=[("xt", s)])
        P.op("act", lambda e, s=s, t=t: e.activation(out=junk[:], in_=xt[s][:], func=AF.Square, accum_out=ss[:, t:t + 1]), r=[("xt", s)], w=["junk", ("ss", t)])
        P.op("act", lambda e, t=t: e.activation(out=ss[:, t:t + 1], in_=ss[:, t:t + 1], func=AF.Sqrt, scale=1.0 / 2048, bias=EPS), r=[("ss", t)], w=[("ss", t)])
        P.op("dve", lambda e, t=t: e.reciprocal(out=ss[:, t:t + 1], in_=ss[:, t:t + 1]), r=[("ss", t)], w=[("ss", t)])
        P.op("dve", lambda e, s=s, t=t: e.scalar_tensor_tensor(out=yo[s][:], in0=xt[s][:], scalar=ss[:, t:t + 1], in1=grow[:], op0=ALU.mult, op1=ALU.mult),
             r=[("xt", s), ("ss", t), "grow"], w=[("yo", s)])
        P.dma(o_d[t * 128:(t + 1) * 128, :], yo[s][:], r=[("yo", s)])
    return C.finish()


def odd_layer(xl, p):
    rows = np.zeros((128, 3, 256), np.float32)
    rows[:, 0, :] = p["kv_norm"][None, :]
    rows[:, 1, :64] = p["ik_g"][None, :]
    rows[:, 2, :64] = p["ik_b"][None, :]
    ins = [dict(x=xl[c], gT=fmvec(p["norm"]), w=p["w_in"], qnT=fmvec(p["q_norm"]), rows=rows, wqb=p["w_qb"], wiq=p["w_iq"], wuk=p["w_uk"], ident=IDENT)
           for c in range(8)]
    o1 = launch("O1", ins)
    ins = []
    for c in range(8):
        b, r = c // 4, c % 4
        _, _, tm = moba_consts(r)
        ins.append(dict(qlatT=o1[c]["qlatT"], iqT=o1[c]["iqT"], iw=o1[c]["iw"], gateT=o1[c]["gateT"],
                        ckvTall=np.stack([o1[b * 4 + j]["ckvT"] for j in range(4)]), ckvall=np.stack([o1[b * 4 + j]["ckv"] for j in range(4)]),
                        ikTall=np.stack([o1[b * 4 + j]["ikT"] for j in range(4)]), tmask=tm, wuv=p["w_uv"], ident=IDENT))
    o2 = launch("O2", ins)
    ins = [dict(catT=o2[c]["catT"], x=xl[c], w=p["w_out"]) for c in range(8)]
    e2c = launch("E2c", ins)
    return [e2c[c]["xo"] for c in range(8)]


def kernel_unfused(**z):
    x = np.asarray(z["x"], np.float32)
    xl = []
    for c in range(8):
        b, idx = local_tokens(c)
        xl.append(np.ascontiguousarray(x[b, idx]))
    for layer in range(4):
        i = layer // 2
        if layer % 2 == 0:
            p = dict(norm=z["even_norm"][i], w_in=z["even_w_in"][i], b_glu=z["even_b_glu"][i], w_dw=z["even_w_dw"][i], b_dw=z["even_b_dw"][i],
                     ln_g=z["even_conv_ln_g"][i], ln_b=z["even_conv_ln_b"][i], w_pw=z["even_w_pw"][i], b_pw=z["even_b_pw"][i], w_out=z["even_w_out"][i])
            p = {k: np.ascontiguousarray(np.asarray(v, np.float32)) for k, v in p.items()}
            xl = even_layer(xl, p)
        else:
            p = dict(norm=z["odd_norm"][i], w_in=z["odd_w_in"][i], q_norm=z["odd_q_norm"][i], w_qb=z["odd_w_qb"][i], kv_norm=z["odd_kv_norm"][i],
                     w_uk=z["odd_w_uk"][i], w_uv=z["odd_w_uv"][i], w_iq=z["odd_w_iq"][i], ik_g=z["odd_ik_ln_g"][i], ik_b=z["odd_ik_ln_b"][i], w_out=z["odd_w_out"][i])
            p = {k: np.ascontiguousarray(np.asarray(v, np.float32)) for k, v in p.items()}
            xl = odd_layer(xl, p)
    grow = np.ascontiguousarray(np.broadcast_to(np.asarray(z["final_norm"], np.float32)[None, :], (128, 2048)))
    f = launch("F", [dict(x=xl[c], grow=grow) for c in range(8)])
    out = np.zeros_like(x)
    for c in range(8):
        b, idx = local_tokens(c)
        out[b, idx] = f[c]["y"]
    return out


RG = [[0, 1, 2, 3], [4, 5, 6, 7]]


def build_HALO(C):
    P = C.P
    uall = C.din("uTall", [4, 1024, 1024], BF16)
    selw_d = C.din("selw", [128, 5])
    uh_o = C.dout("uh", [1024, 4, 32], BF16)
    selw = load_const(C, selw_d, [128, 5], F32, "selw")
    H = C.sb([128, 4, 8, 4, 32], BF16)
    for j in range(4):
        for c in range(8):
            P.dma(H[:, j, c, :, :], uall.rows(j, c * 128, (c + 1) * 128).rearrange("p (i t) -> p i t", i=4)[:, :, 224:256], w=["H"])
    acc = C.sb([128, 8, 4, 32], F32)
    ob = C.sb([128, 8, 4, 32], BF16)
    P.op("dve", lambda e: e.tensor_scalar(out=acc[:], in0=H[:, 0], scalar1=selw[:, 0:1], scalar2=None, op0=ALU.mult), r=["H", "selw"], w=["acc"])
    for j in range(1, 4):
        P.op("dve", lambda e, j=j: e.scalar_tensor_tensor(out=acc[:], in0=H[:, j], scalar=selw[:, j:j + 1], in1=acc[:], op0=ALU.mult, op1=ALU.add),
             r=["H", "selw", "acc"], w=["acc"])
    for c in range(8):
        P.op("dve", lambda e, c=c: e.scalar_tensor_tensor(out=acc[:, c, 1:4, :], in0=H[:, 3, c, 0:3, :], scalar=selw[:, 4:5], in1=acc[:, c, 1:4, :],
                                                       op0=ALU.mult, op1=ALU.add),
             r=["H", "selw", "acc"], w=["acc"])
    P.op("dve", lambda e: e.tensor_copy(out=ob[:], in_=acc[:]), r=["acc"], w=["ob"])
    P.dma(uh_o.rearrange("(c p) i t -> p c i t", p=128), ob[:], r=["ob"])
    return C.finish()


def allgather(C, src, dst):
    C.P.op("pool", lambda e: e.collective_compute("AllGather", ALU.bypass, replica_groups=RG, ins=[src.opt()], outs=[dst.opt()]), cc=True)


def allgather2(C, src, dsts):
    for h in range(2):
        allgather(C, src[h * 512:(h + 1) * 512, :], dsts[h])
    return Gath(dsts, 512)


EVEN_W = dict(gT=([128, 16], F32), w_in=([2048, 7168], F32), bgT=([128, 16], F32), wdwT=([128, 8, 31], F32), vecs=([128, 4, 8], F32),
              w_pw=([1024, 1024], F32), w_out=([2048, 2048], F32))
ODD_W = dict(gT=([128, 16], F32), w_in=([2048, 2896], F32), qnT=([128, 4], F32), rows=([128, 3, 256], F32), w_qb=([512, 2048], F32),
             w_iq=([512, 1024], F32), w_uk=([16, 128, 256], F32), w_uv=([16, 256, 128], F32), w_out=([2048, 2048], F32))


def build_FUSED(stop=None):
    C = Ctx(fused=True)
    ph = [0]

    def done():
        ph[0] += 1
        return stop is not None and ph[0] >= stop
    x_in = C.ext_in("x", [TOK, 2048])
    ident = C.ext_in("ident", [128, 128])
    gbias = C.ext_in("gbias", [128, 4, 16])
    pflag = C.ext_in("pflag", [128, 4, 16])
    tmask = C.ext_in("tmask", [128, 2, 4, 256])
    selw = C.ext_in("selw", [128, 5])
    grow = C.ext_in("grow", [128, 2048])
    y_out = C.ext_out("y", [TOK, 2048])
    I = C.internal
    xa, xb = I("xa", [TOK, 2048]), I("xb", [TOK, 2048])
    uT, gaT, qT, kT, v, gb = (I(n, [1024, 1024], BF16) for n in ("uT_i", "gaT_i", "qT_i", "kT_i", "v_i", "gb_i"))
    kTall, vall, uTall = ([I(n + str(h), [2048, 1024], BF16) for h in range(2)] for n in ("kTall_i", "vall_i", "uTall_i"))
    uh = I("uh_i", [1024, 4, 32], BF16)
    catT = I("catT_i", [2048, 1024], BF16)
    ckv, ckvT, ikT, iw = I("ckv_i", [1024, 256], BF16), I("ckvT_i", [256, 1024], BF16), I("ikT_i", [64, 1024], BF16), I("iw_i", [1024, 16])
    gateT, qlatT, iqT = I("gateT_i", [2048, 1024], BF16), I("qlatT_i", [16, 256, 1024], BF16), I("iqT_i", [1024, 1024], BF16)
    ckvall, ckvTall, ikTall = I("ckvall_i", [4096, 256], BF16), I("ckvTall_i", [1024, 1024], BF16), I("ikTall_i", [256, 1024], BF16)
    cur = x_in
    for layer in range(4):
        nxt = xa if layer % 2 == 0 else xb
        L = "L%d_" % layer
        if layer % 2 == 0:
            W = {k: C.ext_in(L + k, s, d) for k, (s, d) in EVEN_W.items()}
            C.io = dict(x=cur, gT=W["gT"], w=W["w_in"], bgT=W["bgT"], ident=ident, uT=uT, gaT=gaT, qT=qT, kT=kT, v=v, gb=gb)
            build_E1(C)
            if done():
                break
            kT_g = allgather2(C, kT, kTall)
            v_g = allgather2(C, v, vall)
            uT_g = allgather2(C, uT, uTall)
            C.P.barrier()
            if done():
                break
            C.io = dict(uTall=uT_g, selw=selw, uh=uh)
            build_HALO(C)
            if done():
                break
            C.io = dict(uT=uT, uh=uh, gaT=gaT, wdwT=W["wdwT"], vecs=W["vecs"], wpw=W["w_pw"], ident=ident, yaT=catT[0:1024, :])
            build_E2a(C)
            if done():
                break
            C.io = dict(qT=qT, kTall=kT_g, vall=v_g, gb=gb,
                        gbias=gbias, pflag=pflag, tmask=tmask, ident=ident, ybT=catT[1024:2048, :])
            build_E2b(C)
            if done():
                break
        else:
            W = {k: C.ext_in(L + k, s, d) for k, (s, d) in ODD_W.items()}
            C.io = dict(x=cur, gT=W["gT"], w=W["w_in"], qnT=W["qnT"], rows=W["rows"], wqb=W["w_qb"], wiq=W["w_iq"], wuk=W["w_uk"], ident=ident,
                        ckv=ckv, ckvT=ckvT, ikT=ikT, iw=iw, gateT=gateT, qlatT=qlatT, iqT=iqT)
            build_O1(C)
            if done():
                break
            allgather(C, ckv, ckvall)
            allgather(C, ckvT, ckvTall)
            allgather(C, ikT, ikTall)
            C.P.barrier()
            if done():
                break
            C.io = dict(qlatT=qlatT, iqT=iqT, iw=iw, gateT=gateT, ckvTall=ckvTall.rearrange("(j r) c -> j r c", j=4),
                        ckvall=ckvall.rearrange("(j r) c -> j r c", j=4), ikTall=ikTall.rearrange("(j r) c -> j r c", j=4),
                        tmask=tmask, wuv=W["w_uv"], ident=ident, catT=catT)
            build_O2(C)
            if done():
                break
        C.io = dict(catT=catT, x=cur, w=W["w_out"], xo=nxt)
        build_E2c(C)
        if done():
            break
        cur = nxt
    C.io = dict(x=cur, grow=grow, y=y_out)
    if stop is None:
        build_F(C)
    else:
        C.P.barrier()
    C.P.emit()
    C.es.close()
    return C.nc


def kernel(**z):
    x = np.asarray(z["x"], np.float32)
    f32 = lambda a: np.ascontiguousarray(np.asarray(a, np.float32))
    common = dict(ident=IDENT, grow=np.ascontiguousarray(np.broadcast_to(f32(z["final_norm"])[None, :], (128, 2048))))
    for layer in range(4):
        i = layer // 2
        L = "L%d_" % layer
        if layer % 2 == 0:
            common[L + "gT"] = fmvec(f32(z["even_norm"][i]))
            common[L + "w_in"] = f32(z["even_w_in"][i])
            common[L + "bgT"] = fmvec(f32(z["even_b_glu"][i]))
            common[L + "wdwT"] = np.ascontiguousarray(f32(z["even_w_dw"][i]).T.reshape(8, 128, 31).transpose(1, 0, 2))
            common[L + "vecs"] = np.ascontiguousarray(np.stack([fmvec(f32(z["even_b_dw"][i])), fmvec(f32(z["even_conv_ln_g"][i])),
                                                                fmvec(f32(z["even_conv_ln_b"][i])), fmvec(f32(z["even_b_pw"][i]))], axis=1))
            common[L + "w_pw"] = f32(z["even_w_pw"][i])
            common[L + "w_out"] = f32(z["even_w_out"][i])
        else:
            rows = np.zeros((128, 3, 256), np.float32)
            rows[:, 0, :] = f32(z["odd_kv_norm"][i])[None, :]
            rows[:, 1, :64] = f32(z["odd_ik_ln_g"][i])[None, :]
            rows[:, 2, :64] = f32(z["odd_ik_ln_b"][i])[None, :]
            common[L + "gT"] = fmvec(f32(z["odd_norm"][i]))
            common[L + "w_in"] = f32(z["odd_w_in"][i])
            common[L + "qnT"] = fmvec(f32(z["odd_q_norm"][i]))
            common[L + "rows"] = rows
            common[L + "w_qb"] = f32(z["odd_w_qb"][i])
            common[L + "w_iq"] = f32(z["odd_w_iq"][i])
            common[L + "w_uk"] = f32(z["odd_w_uk"][i])
            common[L + "w_uv"] = f32(z["odd_w_uv"][i])
            common[L + "w_out"] = f32(z["odd_w_out"][i])
    ins = []
    for c in range(8):
        b, idx = local_tokens(c)
        r = c % 4
        gbias, pflag, tm = moba_consts(r)
        selw = np.zeros((128, 5), np.float32)
        if r >= 1:
            selw[:, r - 1] = 1.0
        else:
            selw[:, 4] = 1.0
        d = dict(common)
        d.update(x=np.ascontiguousarray(x[b, idx]), gbias=gbias, pflag=pflag, tmask=tm, selw=selw)
        ins.append(d)
    if z.get("_ins_only"):
        return ins
    res = launch("FUSED", ins)
    out = np.zeros_like(x)
    for c in range(8):
        b, idx = local_tokens(c)
        out[b, idx] = res[c]["y"]
    return out
```

```python
import contextlib
import numpy as np
import concourse.bass as bass
import concourse.mybir as mybir
from concourse.bass_utils import run_bass_kernel_spmd

F32 = mybir.dt.float32
BF16 = mybir.dt.bfloat16
AF = mybir.ActivationFunctionType
ALU = mybir.AluOpType
AX = mybir.AxisListType

COMPUTE = ("pe", "act", "dve", "pool")
ND = 12


class Prog:
    def __init__(self, nc):
        self.nc = nc
        self.ops = {e: [] for e in ("pe", "act", "dve", "pool", "sp")}
        self.last_w = {}
        self.readers = {}
        self.dmas_since_bar = []

    def op(self, eng, fn, r=(), w=(), dma=False, cc=False):
        idx = len(self.ops[eng])
        me = (eng, idx)
        deps = set()
        for k in r:
            if k in self.last_w:
                deps.add(self.last_w[k])
        for k in w:
            if k in self.last_w:
                deps.add(self.last_w[k])
            rd = self.readers.get(k)
            if rd:
                for d in rd[0].items():
                    deps.add(d)
                for d in rd[1]:
                    deps.add(d)
        if eng == "pe" and not dma:
            deps = {d for d in deps if d[0] != "pe"}
        deps.discard(me)
        self.ops[eng].append(dict(fn=fn, deps=deps, dma=dma, cc=cc))
        for k in r:
            rd = self.readers.setdefault(k, ({}, []))
            if dma or cc:
                rd[1].append(me)
            else:
                rd[0][eng] = idx
        for k in w:
            self.last_w[k] = me
            self.readers[k] = ({}, [])
        if dma or cc:
            self.dmas_since_bar.append(me)
        return me

    def barrier(self):
        deps = set()
        for e in self.ops:
            for i in range(len(self.ops[e]) - 1, -1, -1):
                if not self.ops[e][i]["dma"] and not self.ops[e][i].get("cc") and self.ops[e][i]["fn"] is not None:
                    deps.add((e, i))
                    break
        deps.update(self.dmas_since_bar)
        for e in self.ops:
            self.ops[e].append(dict(fn=None, deps=set(deps), dma=False))
        self.last_w = {}
        self.readers = {}
        self.dmas_since_bar = []

    def dma(self, out, in_, r=(), w=(), q="sp"):
        return self.op(q, lambda e: e.dma_start(out=out, in_=in_), r=r, w=w, dma=True)

    def emit(self):
        nc = self.nc
        flagged = set()
        for e in self.ops:
            for o in self.ops[e]:
                flagged.update(o["deps"])
        with contextlib.ExitStack() as es:
            sems = {e: es.enter_context(nc.semaphore("s_" + e)) for e in COMPUTE}
            dsems = {q: [es.enter_context(nc.semaphore("d_%s%d" % (q, i))) for i in range(ND)]
                     for q in ("sp", "pool", "act")}
            sig = {}
            for e in self.ops:
                cnt = 0
                j = 0
                for i, o in enumerate(self.ops[e]):
                    if o.get("cc"):
                        s = es.enter_context(nc.semaphore("cc_%s%d" % (e, i)))
                        sig[(e, i)] = (s, 1)
                    elif o["dma"]:
                        s = dsems[e][j % ND]
                        sig[(e, i)] = (s, 16 * (j // ND + 1))
                        o["pre"] = (s, 16 * (j // ND)) if j >= ND else None
                        j += 1
                    elif (e, i) in flagged:
                        cnt += 1
                        sig[(e, i)] = (sems[e], cnt)

            def run(ename, eng):
                known = {}
                for i, o in enumerate(self.ops[ename]):
                    waits = {}
                    if o.get("pre"):
                        s, v = o["pre"]
                        waits[s] = (s, v)
                    for d in o["deps"]:
                        s, v = sig[d]
                        key = id(s)
                        if known.get(key, 0) >= v:
                            continue
                        if key not in waits or waits[key][1] < v:
                            waits[key] = (s, v)
                    for key, (s, v) in waits.items():
                        eng.wait_ge(s, v)
                        known[key] = v
                    if o["fn"] is None:
                        continue
                    ins = o["fn"](eng)
                    if (ename, i) in sig:
                        s, v = sig[(ename, i)]
                        ins.then_inc(s, 16 if o["dma"] else 1)

            with nc.Block() as block:
                @block.tensor
                def _(e):
                    run("pe", e)

                @block.scalar
                def _(e):
                    run("act", e)

                @block.vector
                def _(e):
                    run("dve", e)

                @block.gpsimd
                def _(e):
                    run("pool", e)

                @block.sync
                def _(e):
                    run("sp", e)


import ml_dtypes
NPBF = ml_dtypes.bfloat16
EPS = 1e-6
NT = 8
TOK = 1024


DECL = []


class Ctx:
    def __init__(self, fused=False):
        self.nc = bass.Bass("TRN2", target_bir_lowering=False)
        self.P = Prog(self.nc)
        self.es = contextlib.ExitStack()
        self.n = 0
        self.fused = fused
        self.io = {}

    def ext_in(self, name, shape, dt=F32):
        DECL.append(name)
        return self.nc.dram_tensor(name, list(shape), dt, kind="ExternalInput").ap()

    def ext_out(self, name, shape, dt=F32):
        return self.nc.dram_tensor(name, list(shape), dt, kind="ExternalOutput").ap()

    def internal(self, name, shape, dt=F32):
        return self.nc.dram_tensor(name, list(shape), dt).ap()

    def din(self, name, shape, dt=F32):
        if name in self.io:
            return self.io[name]
        assert not self.fused, name
        return self.ext_in(name, shape, dt)

    def dout(self, name, shape, dt=F32):
        if name in self.io:
            return self.io[name]
        assert not self.fused, name
        return self.ext_out(name, shape, dt)

    def sb(self, shape, dt, name=None):
        self.n += 1
        return self.es.enter_context(self.nc.sbuf_tensor(name or ("sb%d" % self.n), list(shape), dt))

    def ps(self, shape, dt, name=None):
        self.n += 1
        return self.es.enter_context(self.nc.psum_tensor(name or ("ps%d" % self.n), list(shape), dt))

    def finish(self):
        self.P.barrier()
        if self.fused:
            self.es.close()
            self.es = contextlib.ExitStack()
            self.io = {}
            return None
        self.P.emit()
        self.es.close()
        return self.nc


def norm_transpose(C, x_d, gT, xnT, identb, nchunk, D):
    P = C.P
    xt = [C.sb([128, D], F32) for _ in range(2)]
    xn = [C.sb([128, D], BF16) for _ in range(2)]
    junk = C.sb([128, D], BF16)
    ss = C.sb([128, NT], F32)
    rs = C.sb([128, NT], F32)
    pT = [C.ps([128, 8, 128], BF16) for _ in range(2)]
    for t in range(NT):
        s = t % 2
        P.dma(xt[s][:], x_d[t * 128:(t + 1) * 128, :], w=[("xt", s)])
        P.op("act", lambda e, s=s, t=t: e.activation(out=junk[:], in_=xt[s][:], func=AF.Square, accum_out=ss[:, t:t + 1]),
             r=[("xt", s)], w=["junk", ("ss", t)])
        P.op("act", lambda e, t=t: e.activation(out=rs[:, t:t + 1], in_=ss[:, t:t + 1], func=AF.Sqrt, scale=1.0 / D, bias=EPS),
             r=[("ss", t)], w=[("rs", t)])
        P.op("dve", lambda e, t=t: e.reciprocal(out=rs[:, t:t + 1], in_=rs[:, t:t + 1]), r=[("rs", t)], w=[("rs", t)])
        P.op("dve", lambda e, s=s, t=t: e.tensor_scalar(out=xn[s][:], in0=xt[s][:], scalar1=rs[:, t:t + 1], scalar2=None, op0=ALU.mult),
             r=[("xt", s), ("rs", t)], w=[("xn", s)])
        for c0 in range(0, nchunk, 8):
            b = (c0 // 8) % 2
            for c in range(c0, min(c0 + 8, nchunk)):
                P.op("pe", lambda e, c=c, s=s, b=b: e.transpose(out=pT[b][:, c % 8, :], in_=xn[s][:, c * 128:(c + 1) * 128], identity=identb[:]),
                     r=[("xn", s), "identb"], w=[("pT", b)])
            for c in range(c0, min(c0 + 8, nchunk)):
                P.op("dve", lambda e, c=c, t=t, b=b: e.tensor_scalar(out=xnT[:, c, t * 128:(t + 1) * 128], in0=pT[b][:, c % 8, :],
                                                                  scalar1=gT[:, c:c + 1], scalar2=None, op0=ALU.mult),
                     r=[("pT", b), "gT"], w=[("xnT", t)])


def load_const(C, dram, shape, dt, key, q="sp"):
    t = C.sb(shape, dt)
    C.P.dma(t[:], dram, w=[key], q=q)
    return t


def build_E1(C=None):
    C = C or Ctx()
    P = C.P
    x_d = C.din("x", [TOK, 2048])
    gT_d = C.din("gT", [128, 16])
    w_d = C.din("w", [2048, 7168])
    bgT_d = C.din("bgT", [128, 16])
    id_d = C.din("ident", [128, 128])
    outs = {n: C.dout(n, [1024, 1024], BF16) for n in ("uT", "gaT", "qT", "kT", "v", "gb")}
    identf = load_const(C, id_d, [128, 128], F32, "identf")
    gT = load_const(C, gT_d, [128, 16], F32, "gT")
    bgT = load_const(C, bgT_d, [128, 16], F32, "bgT")
    identb = C.sb([128, 128], BF16)
    P.op("dve", lambda e: e.tensor_copy(out=identb[:], in_=identf[:]), r=["identf"], w=["identb"])
    xnT = C.sb([128, 16, TOK], BF16)
    norm_transpose(C, x_d, gT, xnT, identb, 16, 2048)
    XN = [("xnT", t) for t in range(NT)]
    wb = [C.sb([128, 16, 512], BF16) for _ in range(2)]
    psb = [C.ps([128, 512], F32) for _ in range(4)]
    ob = [C.sb([128, 512], BF16) for _ in range(4)]
    tv = [C.sb([128, 512], F32) for _ in range(2)]
    tg = [C.sb([128, 512], F32) for _ in range(2)]
    w_v = w_d.rearrange("(c p) n -> p c n", p=128)
    cnt = {"ps": 0, "ob": 0, "wb": 0, "tv": 0}

    def loadw(cols):
        s = cnt["wb"] % 2
        cnt["wb"] += 1
        off = 0
        for (c0, n) in cols:
            P.dma(wb[s][:, :, off:off + n], w_v[:, :, c0:c0 + n], w=[("wb", s)], q="pool")
            off += n
        return s

    def mm_fm(s, j, h):
        b = cnt["ps"] % 4
        cnt["ps"] += 1
        for c in range(16):
            P.op("pe", lambda e, c=c, b=b: e.matmul(psb[b][:], lhsT=wb[s][:, c, j * 128:(j + 1) * 128], rhs=xnT[:, c, h * 512:(h + 1) * 512],
                                                  start=(c == 0), stop=(c == 15)),
                 r=[("wb", s)] + XN, w=[("ps", b)])
        return b

    def mm_tm(s, t):
        b = cnt["ps"] % 4
        cnt["ps"] += 1
        for c in range(16):
            P.op("pe", lambda e, c=c, b=b: e.matmul(psb[b][:], lhsT=xnT[:, c, t * 128:(t + 1) * 128], rhs=wb[s][:, c, :],
                                                  start=(c == 0), stop=(c == 15)),
                 r=[("wb", s)] + XN, w=[("ps", b)])
        return b

    def store(dst, o):
        P.dma(dst, ob[o][:], r=[("ob", o)])

    def newob():
        o = cnt["ob"] % 4
        cnt["ob"] += 1
        return o

    for g in range(4):
        s = loadw([(g * 256, 256), (1024 + g * 256, 256)])
        for jj in range(2):
            ch = 2 * g + jj
            for h in range(2):
                k = cnt["tv"] % 2
                cnt["tv"] += 1
                b = mm_fm(s, jj, h)
                P.op("act", lambda e, b=b, k=k, ch=ch: e.activation(out=tv[k][:], in_=psb[b][:], func=AF.Identity, bias=bgT[:, ch:ch + 1]),
                     r=[("ps", b), "bgT"], w=[("tv", k)])
                b2 = mm_fm(s, 2 + jj, h)
                P.op("act", lambda e, b2=b2, k=k, ch=ch: e.activation(out=tg[k][:], in_=psb[b2][:], func=AF.Sigmoid, bias=bgT[:, 8 + ch:9 + ch]),
                     r=[("ps", b2), "bgT"], w=[("tg", k)])
                o = newob()
                P.op("dve", lambda e, o=o, k=k: e.tensor_tensor(out=ob[o][:], in0=tv[k][:], in1=tg[k][:], op=ALU.mult),
                     r=[("tv", k), ("tg", k)], w=[("ob", o)])
                store(outs["uT"][ch * 128:(ch + 1) * 128, h * 512:(h + 1) * 512], o)
    for name, col0, fn in (("gaT", 2048, AF.Silu), ("qT", 3072, AF.Copy), ("kT", 4096, AF.Copy)):
        for g in range(2):
            s = loadw([(col0 + g * 512, 512)])
            for j in range(4):
                ch = g * 4 + j
                for h in range(2):
                    b = mm_fm(s, j, h)
                    o = newob()
                    P.op("act", lambda e, b=b, o=o, fn=fn: e.activation(out=ob[o][:], in_=psb[b][:], func=fn),
                         r=[("ps", b)], w=[("ob", o)])
                    store(outs[name][ch * 128:(ch + 1) * 128, h * 512:(h + 1) * 512], o)
    for name, col0, fn in (("v", 5120, AF.Copy), ("gb", 6144, AF.Silu)):
        for g in range(2):
            s = loadw([(col0 + g * 512, 512)])
            for t in range(NT):
                b = mm_tm(s, t)
                o = newob()
                P.op("act", lambda e, b=b, o=o, fn=fn: e.activation(out=ob[o][:], in_=psb[b][:], func=fn),
                     r=[("ps", b)], w=[("ob", o)])
                store(outs[name][t * 128:(t + 1) * 128, g * 512:(g + 1) * 512], o)
    return C.finish()


def build_E2a(C=None):
    C = C or Ctx()
    P = C.P
    uT_d = C.din("uT", [1024, 1024], BF16)
    uh_d = C.din("uh", [1024, 4, 32], BF16)
    gaT_d = C.din("gaT", [1024, 1024], BF16)
    wdw_d = C.din("wdwT", [128, 8, 31])
    vec_d = C.din("vecs", [128, 4, 8])
    wpw_d = C.din("wpw", [1024, 1024])
    id_d = C.din("ident", [128, 128])
    ya_d = C.dout("yaT", [1024, 1024], BF16)
    identf = load_const(C, id_d, [128, 128], F32, "identf")
    wdw = load_const(C, wdw_d, [128, 8, 31], F32, "wdw")
    vecs = load_const(C, vec_d, [128, 4, 8], F32, "vecs")
    identb = C.sb([128, 128], BF16)
    onesf = C.sb([128, 128], F32)
    P.op("dve", lambda e: e.tensor_copy(out=identb[:], in_=identf[:]), r=["identf"], w=["identb"])
    P.op("pool", lambda e: e.memset(onesf[:], 1.0), w=["onesf"])
    ucv = C.sb([128, 8, 4, 288], BF16)
    gaT = C.sb([128, 8, 1024], BF16)
    for c in range(8):
        P.dma(ucv[:, c, :, 32:288], uT_d[c * 128:(c + 1) * 128, :].rearrange("p (i t) -> p i t", i=4), w=[("ucv", c)])
        P.dma(ucv[:, c, :, 0:32], uh_d[c * 128:(c + 1) * 128, :, :], w=[("ucv", c)])
        P.dma(gaT[:, c, :], gaT_d[c * 128:(c + 1) * 128, :], w=[("gaT", c)])
    D = [C.sb([128, 31, 128], BF16) for _ in range(2)]
    ycv = C.sb([128, 8, 1024], F32)
    ysq = C.sb([128, 8, 1024], F32)
    psb = [C.ps([128, 512], F32) for _ in range(6)]
    pc = [0]

    def bank():
        b = pc[0] % 6
        pc[0] += 1
        return b
    for c in range(8):
        s = c % 2
        for k in range(31):
            P.op("pool", lambda e, s=s, c=c, k=k: e.tensor_scalar(out=D[s][:, k, :], in0=identb[:], scalar1=wdw[:, c, k:k + 1], scalar2=0.0,
                                                                op0=ALU.mult, op1=ALU.add),
                 r=["identb", "wdw"], w=[("D", s)])
        for half in range(2):
            b = bank()
            for ii in range(2):
                i = half * 2 + ii
                for k in range(31):
                    P.op("pe", lambda e, s=s, c=c, k=k, i=i, ii=ii, b=b: e.matmul(psb[b][:, ii * 256:(ii + 1) * 256], lhsT=D[s][:, k, :],
                                                                             rhs=ucv[:, c, i, 2 + k:2 + k + 256], start=(k == 0), stop=(k == 30)),
                         r=[("D", s), ("ucv", c)], w=[("ps", b)])
            P.op("act", lambda e, c=c, half=half, b=b: e.activation(out=ycv[:, c, half * 512:(half + 1) * 512], in_=psb[b][:], func=AF.Identity,
                                                                  bias=vecs[:, 0, c:c + 1]),
                 r=[("ps", b), "vecs"], w=[("ycv", c, half)])
            P.op("act", lambda e, c=c, half=half, b=b: e.activation(out=ysq[:, c, half * 512:(half + 1) * 512], in_=psb[b][:], func=AF.Square,
                                                                  bias=vecs[:, 0, c:c + 1]),
                 r=[("ps", b), "vecs"], w=[("ysq", c, half)])
    mean = C.sb([128, 1024], F32)
    msq = C.sb([128, 1024], F32)
    rstd = C.sb([128, 1024], F32)
    for half in range(2):
        sl = slice(half * 512, (half + 1) * 512)
        b1 = bank()
        for c in range(8):
            P.op("pe", lambda e, c=c, b1=b1, sl=sl: e.matmul(psb[b1][:], lhsT=onesf[:], rhs=ycv[:, c, sl], start=(c == 0), stop=(c == 7)),
                 r=["onesf", ("ycv", c, half)], w=[("ps", b1)])
        b2 = bank()
        for c in range(8):
            P.op("pe", lambda e, c=c, b2=b2, sl=sl: e.matmul(psb[b2][:], lhsT=onesf[:], rhs=ysq[:, c, sl], start=(c == 0), stop=(c == 7)),
                 r=["onesf", ("ysq", c, half)], w=[("ps", b2)])
        P.op("dve", lambda e, b1=b1, sl=sl: e.tensor_scalar(out=mean[:, sl], in0=psb[b1][:], scalar1=1.0 / 1024, scalar2=None, op0=ALU.mult),
             r=[("ps", b1)], w=[("mean", half)])
        P.op("dve", lambda e, sl=sl: e.tensor_tensor(out=msq[:, sl], in0=mean[:, sl], in1=mean[:, sl], op=ALU.mult),
             r=[("mean", half)], w=[("msq", half)])
        P.op("dve", lambda e, b2=b2, sl=sl: e.scalar_tensor_tensor(out=rstd[:, sl], in0=psb[b2][:], scalar=1.0 / 1024, in1=msq[:, sl],
                                                                 op0=ALU.mult, op1=ALU.subtract),
             r=[("ps", b2), ("msq", half)], w=[("rstd", half)])
        P.op("act", lambda e, sl=sl: e.activation(out=rstd[:, sl], in_=rstd[:, sl], func=AF.Sqrt, bias=EPS), r=[("rstd", half)], w=[("rstd", half)])
        P.op("dve", lambda e, sl=sl: e.reciprocal(out=rstd[:, sl], in_=rstd[:, sl]), r=[("rstd", half)], w=[("rstd", half)])
    yact = C.sb([128, 8, 1024], BF16)
    for c in range(8):
        for half in range(2):
            sl = slice(half * 512, (half + 1) * 512)
            P.op("dve", lambda e, c=c, sl=sl: e.tensor_tensor(out=ycv[:, c, sl], in0=ycv[:, c, sl], in1=mean[:, sl], op=ALU.subtract),
                 r=[("ycv", c, half), ("mean", half)], w=[("ycv", c, half)])
            P.op("dve", lambda e, c=c, sl=sl: e.tensor_tensor(out=ycv[:, c, sl], in0=ycv[:, c, sl], in1=rstd[:, sl], op=ALU.mult),
                 r=[("ycv", c, half), ("rstd", half)], w=[("ycv", c, half)])
            P.op("act", lambda e, c=c, sl=sl: e.activation(out=yact[:, c, sl], in_=ycv[:, c, sl], func=AF.Silu, scale=vecs[:, 1, c:c + 1],
                                                         bias=vecs[:, 2, c:c + 1]),
                 r=[("ycv", c, half), "vecs"], w=[("yact", c, half)])
    YA = [("yact", c, h) for c in range(8) for h in range(2)]
    wb = [C.sb([128, 8, 512], BF16) for _ in range(2)]
    ob = [C.sb([128, 512], BF16) for _ in range(4)]
    w_v = wpw_d.rearrange("(c p) n -> p c n", p=128)
    oc = 0
    for g in range(2):
        P.dma(wb[g][:], w_v[:, :, g * 512:(g + 1) * 512], w=[("wb", g)], q="pool")
        for j in range(4):
            ch = g * 4 + j
            for half in range(2):
                sl = slice(half * 512, (half + 1) * 512)
                b = bank()
                for c in range(8):
                    P.op("pe", lambda e, c=c, b=b, g=g, j=j, sl=sl: e.matmul(psb[b][:], lhsT=wb[g][:, c, j * 128:(j + 1) * 128], rhs=yact[:, c, sl],
                                                                          start=(c == 0), stop=(c == 7)),
                         r=[("wb", g)] + YA, w=[("ps", b)])
                o = oc % 4
                oc += 1
                P.op("dve", lambda e, b=b, o=o, ch=ch, sl=sl: e.scalar_tensor_tensor(out=ob[o][:], in0=psb[b][:], scalar=vecs[:, 3, ch:ch + 1],
                                                                                in1=gaT[:, ch, sl], op0=ALU.add, op1=ALU.mult),
                     r=[("ps", b), "vecs", ("gaT", ch)], w=[("ob", o)])
                P.dma(ya_d[ch * 128:(ch + 1) * 128, sl], ob[o][:], r=[("ob", o)])
    return C.finish()


def build_E2c(C=None):
    C = C or Ctx()
    P = C.P
    cat_d = C.din("catT", [2048, 1024], BF16)
    x_d = C.din("x", [TOK, 2048])
    w_d = C.din("w", [2048, 2048])
    xo_d = C.dout("xo", [TOK, 2048])
    cat = C.sb([128, 16, 1024], BF16)
    for c in range(16):
        P.dma(cat[:, c, :], cat_d[c * 128:(c + 1) * 128, :], w=[("cat", c)])
    CAT = [("cat", c) for c in range(16)]
    xs = C.sb([128, NT, 2048], F32)
    for t in range(NT):
        P.dma(xs[:, t, :], x_d[t * 128:(t + 1) * 128, :], w=[("xs", t)])
    wb = [C.sb([128, 16, 512], BF16) for _ in range(2)]
    psb = [C.ps([128, 512], F32) for _ in range(4)]
    ob = [C.sb([128, 512], F32) for _ in range(4)]
    w_v = w_d.rearrange("(c p) n -> p c n", p=128)
    n = 0
    for g in range(4):
        s = g % 2
        P.dma(wb[s][:], w_v[:, :, g * 512:(g + 1) * 512], w=[("wb", s)], q="pool")
        for t in range(NT):
            b = n % 4
            n += 1
            for c in range(16):
                P.op("pe", lambda e, c=c, b=b, s=s, t=t: e.matmul(psb[b][:], lhsT=cat[:, c, t * 128:(t + 1) * 128], rhs=wb[s][:, c, :],
                                                               start=(c == 0), stop=(c == 15)),
                     r=[("wb", s)] + CAT, w=[("ps", b)])
            P.op("dve", lambda e, b=b, t=t, g=g: e.tensor_tensor(out=ob[b][:], in0=psb[b][:], in1=xs[:, t, g * 512:(g + 1) * 512], op=ALU.add),
                 r=[("ps", b), ("xs", t)], w=[("ob", b)])
            P.dma(xo_d[t * 128:(t + 1) * 128, g * 512:(g + 1) * 512], ob[b][:], r=[("ob", b)])
    return C.finish()


class Gath:
    def __init__(self, parts, hr):
        self.parts = parts
        self.hr = hr

    def rows(self, j, r0, r1):
        h = r0 // self.hr
        assert (r1 - 1) // self.hr == h
        o = j * self.hr + r0 % self.hr
        return self.parts[h][o:o + (r1 - r0), :]


def as_gath(a, rows):
    if isinstance(a, Gath):
        return a
    return Gath([a.rearrange("j r c -> (j r) c")], rows)


def build_E2b(C=None):
    C = C or Ctx()
    P = C.P
    SC = 128 ** -0.5
    qT_d = C.din("qT", [1024, 1024], BF16)
    kT_d = C.din("kTall", [4, 1024, 1024], BF16)
    v_d = C.din("vall", [4, 1024, 1024], BF16)
    kT_g = as_gath(kT_d, 1024)
    v_g = as_gath(v_d, 1024)
    gb_d = C.din("gb", [1024, 1024], BF16)
    gbias_d = C.din("gbias", [128, 4, 16])
    pflag_d = C.din("pflag", [128, 4, 16])
    tmask_d = C.din("tmask", [128, 2, 4, 256])
    id_d = C.din("ident", [128, 128])
    yb_d = C.dout("ybT", [1024, 1024], BF16)
    identf = load_const(C, id_d, [128, 128], F32, "identf")
    gbias = load_const(C, gbias_d, [128, 4, 16], F32, "gbias")
    pflag = load_const(C, pflag_d, [128, 4, 16], F32, "pflag")
    tmask = load_const(C, tmask_d, [128, 2, 4, 256], F32, "tmask")
    identb = C.sb([128, 128], BF16)
    onesb = C.sb([128, 128], BF16)
    P.op("dve", lambda e: e.tensor_copy(out=identb[:], in_=identf[:]), r=["identf"], w=["identb"])
    P.op("pool", lambda e: e.memset(onesb[:], 1.0), w=["onesb"])
    gbt = C.sb([128, NT, 1024], BF16)
    for t in range(NT):
        P.dma(gbt[:, t, :], gb_d[t * 128:(t + 1) * 128, :], w=[("gbt", t)])
    kTh = [C.sb([128, 4, 1024], BF16) for _ in range(2)]
    vh = [C.sb([128, 32, 128], BF16) for _ in range(2)]
    qTh = [C.sb([128, 1024], BF16) for _ in range(2)]
    sq = C.sb([128, 4096], BF16)
    qsq = C.sb([128, 1024], BF16)
    kmf = C.sb([128, 16], F32)
    kmb = C.sb([128, 16], BF16)
    kmx = C.sb([128, 8], F32)
    kmax2 = C.sb([128, 1], F32)
    ybTh = [C.sb([128, 1024], BF16) for _ in range(2)]
    ps_g = C.ps([128, 512], F32)
    ps_s = [C.ps([128, 512], F32) for _ in range(2)]
    ps_t = [C.ps([128, 2, 2, 128], BF16) for _ in range(2)]
    ps_o = [C.ps([128, 512], F32) for _ in range(2)]
    ps_y = C.ps([128, 8, 128], BF16)
    NR = 3
    gm = [C.sb([128, 16], F32) for _ in range(NR)]
    top8 = [C.sb([128, 8], F32) for _ in range(NR)]
    sel = [C.sb([128, 16], F32) for _ in range(NR)]
    ebias = [C.sb([128, 16], F32) for _ in range(NR)]
    mq = [C.sb([128, 1], F32) for _ in range(NR)]
    rsum = [C.sb([128, 16], F32) for _ in range(NR)]
    rtot = [C.sb([128, 1], F32) for _ in range(NR)]
    s2 = [C.sb([128, 512], F32) for _ in range(2)]
    pb = [C.sb([128, 512], BF16) for _ in range(3)]
    pTs = [C.sb([128, 2, 2, 128], BF16) for _ in range(3)]
    ybt = [C.sb([128, 128], BF16) for _ in range(2)]
    cn = dict(s=0, t=0, o=0, pb=0, pts=0, s2=0, y=0)
    def do_head(h, hs):
        for j in range(4):
            P.dma(kTh[hs][:, j, :], kT_g.rows(j, h * 128, (h + 1) * 128), w=[("kTh", hs)])
            for hv in range(2):
                P.dma(vh[hs][:, j * 8 + hv * 4:j * 8 + hv * 4 + 4, :],
                      v_g.rows(j, hv * 512, hv * 512 + 512).rearrange("(t p) c -> p t c", p=128)[:, :, h * 128:(h + 1) * 128], w=[("vh", hs)])
        P.dma(qTh[hs][:], qT_d[h * 128:(h + 1) * 128, :], w=[("qTh", hs)])
        P.op("dve", lambda e, hs=hs: e.tensor_reduce(out=kmf[:], in_=kTh[hs][:].rearrange("p j (i t) -> p (j i) t", i=4), axis=AX.X, op=ALU.add),
             r=[("kTh", hs)], w=["kmf"])
        P.op("dve", lambda e: e.tensor_copy(out=kmb[:], in_=kmf[:]), r=["kmf"], w=["kmb"])
        P.op("act", lambda e, hs=hs: e.activation(out=sq[:], in_=kTh[hs][:].rearrange("p j t -> p (j t)"), func=AF.Square), r=[("kTh", hs)], w=["sq"])
        P.op("act", lambda e, hs=hs: e.activation(out=qsq[:], in_=qTh[hs][:], func=AF.Square), r=[("qTh", hs)], w=["qsq"])
        for cc in range(8):
            P.op("pe", lambda e, cc=cc: e.matmul(ps_g[:], lhsT=onesb[:], rhs=sq[:, cc * 512:(cc + 1) * 512], start=True, stop=True),
                 r=["onesb", "sq"], w=["ps_g"])
            P.op("dve", lambda e, cc=cc: e.tensor_reduce(out=kmx[:, cc:cc + 1], in_=ps_g[:], axis=AX.X, op=ALU.max), r=["ps_g"], w=["kmx"])
        P.op("dve", lambda e: e.tensor_reduce(out=kmax2[:], in_=kmx[:], axis=AX.X, op=ALU.max), r=["kmx"], w=["kmax2"])
        def pro(t):
            i = t // 2
            z = (h * NT + t) % NR
            tq = slice(t * 128, (t + 1) * 128)
            P.op("pe", lambda e: e.matmul(ps_g[:, 0:16], lhsT=qTh[hs][:, tq], rhs=kmb[:], start=True, stop=True),
                 r=[("qTh", hs), "kmb"], w=["ps_g"])
            P.op("pe", lambda e: e.matmul(ps_g[:, 16:17], lhsT=qsq[:, tq], rhs=onesb[:, 0:1], start=True, stop=True),
                 r=["qsq", "onesb"], w=["ps_g"])
            P.op("dve", lambda e: e.tensor_tensor(out=gm[z][:], in0=ps_g[:, 0:16], in1=gbias[:, i, :], op=ALU.add),
                 r=["ps_g", "gbias"], w=[("gm", z)])
            P.op("dve", lambda e: e.tensor_tensor(out=mq[z][:], in0=ps_g[:, 16:17], in1=kmax2[:], op=ALU.mult),
                 r=["ps_g", "kmax2"], w=[("mq", z)])
            P.op("act", lambda e: e.activation(out=mq[z][:], in_=mq[z][:], func=AF.Sqrt, scale=SC * SC), r=[("mq", z)], w=[("mq", z)])
            P.op("dve", lambda e: e.max(out=top8[z][:], in_=gm[z][:]), r=[("gm", z)], w=[("top8", z)])
            P.op("dve", lambda e: e.tensor_scalar(out=sel[z][:], in0=gm[z][:], scalar1=top8[z][:, 2:3], scalar2=None, op0=ALU.is_ge),
                 r=[("gm", z), ("top8", z)], w=[("sel", z)])
            P.op("dve", lambda e: e.tensor_scalar(out=sel[z][:], in0=sel[z][:], scalar1=-1.0, scalar2=30000.0, op0=ALU.add, op1=ALU.mult),
                 r=[("sel", z)], w=[("sel", z)])
            P.op("dve", lambda e: e.tensor_tensor(out=sel[z][:], in0=sel[z][:], in1=pflag[:, i, :], op=ALU.mult),
                 r=[("sel", z), "pflag"], w=[("sel", z)])
            P.op("dve", lambda e: e.tensor_scalar(out=ebias[z][:], in0=sel[z][:], scalar1=mq[z][:, 0:1], scalar2=None, op0=ALU.subtract),
                 r=[("sel", z), ("mq", z)], w=[("ebias", z)])
            P.op("pool", lambda e: e.memset(rsum[z][:], 0.0), w=[("rsum", z)])

        units = []
        for t in range(NT):
            i = t // 2
            pairs = [(ip, jp) for ip in range(i + 1) for jp in range(2)]
            for pi, (ip, jp) in enumerate(pairs):
                units.append(dict(t=t, ip=ip, jp=jp, first=(pi == 0), last=(pi == len(pairs) - 1), mi=pi * 4, nmm=len(pairs) * 4))
        later = {}

        def st1(u):
            t, ip, jp = u["t"], u["ip"], u["jp"]
            i, qi = t // 2, t % 2
            z = (h * NT + t) % NR
            tq = slice(t * 128, (t + 1) * 128)
            sb_ = cn["s"] % 2
            cn["s"] += 1
            p_ = cn["pb"] % 3
            cn["pb"] += 1
            u["p"] = p_
            for jj in range(2):
                j = jp * 2 + jj
                P.op("pe", lambda e, jj=jj, j=j: e.matmul(ps_s[sb_][:, jj * 256:(jj + 1) * 256], lhsT=qTh[hs][:, tq],
                                                        rhs=kTh[hs][:, j, ip * 256:(ip + 1) * 256], start=True, stop=True),
                     r=[("qTh", hs), ("kTh", hs)], w=[("ps_s", sb_)])
            for jj in range(2):
                j = jp * 2 + jj
                m = j * 4 + ip
                src_ = ps_s[sb_][:, jj * 256:(jj + 1) * 256]
                rk = [("ps_s", sb_)]
                if ip == i:
                    k2 = cn["s2"] % 2
                    cn["s2"] += 1
                    P.op("dve", lambda e, k2=k2, src_=src_, j=j: e.tensor_tensor(out=s2[k2][:, 0:256], in0=src_, in1=tmask[:, qi, j, :], op=ALU.add),
                         r=[("ps_s", sb_), "tmask"], w=[("s2", k2)])
                    src_ = s2[k2][:, 0:256]
                    rk = [("s2", k2)]
                P.op("act", lambda e, src_=src_, jj=jj, m=m: e.activation(out=pb[p_][:, jj * 256:(jj + 1) * 256], in_=src_, func=AF.Exp, scale=SC,
                                                                      bias=ebias[z][:, m:m + 1], accum_out=rsum[z][:, m:m + 1]),
                     r=rk + [("ebias", z)], w=[("pb", p_), ("rsum", z)])

        def st2(u):
            p_ = u["p"]
            tb_ = cn["t"] % 2
            cn["t"] += 1
            q_ = cn["pts"] % 3
            cn["pts"] += 1
            u["q"] = q_
            for jj in range(2):
                for hf in range(2):
                    P.op("pe", lambda e, jj=jj, hf=hf: e.transpose(out=ps_t[tb_][:, jj, hf, :], in_=pb[p_][:, jj * 256 + hf * 128:jj * 256 + hf * 128 + 128],
                                                                identity=identb[:]),
                         r=[("pb", p_), "identb"], w=[("ps_t", tb_)])
            P.op("dve", lambda e: e.tensor_copy(out=pTs[q_][:], in_=ps_t[tb_][:]), r=[("ps_t", tb_)], w=[("pTs", q_)])

        def st3(u, k):
            t, ip, jp, q_ = u["t"], u["ip"], u["jp"], u["q"]
            z = (h * NT + t) % NR
            ob_ = t % 2
            for jj in range(2):
                j = jp * 2 + jj
                for hf in range(2):
                    kt = j * 8 + ip * 2 + hf
                    mi = u["mi"] + jj * 2 + hf
                    P.op("pe", lambda e, jj=jj, hf=hf, kt=kt, mi=mi: e.matmul(ps_o[ob_][:, 0:128], lhsT=pTs[q_][:, jj, hf, :], rhs=vh[hs][:, kt, :],
                                                                        start=(mi == 0), stop=(mi == u["nmm"] - 1)),
                         r=[("pTs", q_), ("vh", hs)], w=[("ps_o", ob_)])
            if u["last"]:
                y_ = cn["y"] % 2
                cn["y"] += 1
                P.op("dve", lambda e: e.tensor_reduce(out=rtot[z][:], in_=rsum[z][:], axis=AX.X, op=ALU.add), r=[("rsum", z)], w=[("rtot", z)])
                P.op("dve", lambda e: e.reciprocal(out=rtot[z][:], in_=rtot[z][:]), r=[("rtot", z)], w=[("rtot", z)])
                P.op("dve", lambda e: e.scalar_tensor_tensor(out=ybt[y_][:], in0=ps_o[ob_][:, 0:128], scalar=rtot[z][:, 0:1],
                                                          in1=gbt[:, t, h * 128:(h + 1) * 128], op0=ALU.mult, op1=ALU.mult),
                     r=[("ps_o", ob_), ("rtot", z), ("gbt", t)], w=[("ybt", y_)])

                def fin2():
                    P.op("pe", lambda e: e.transpose(out=ps_y[:, t, :], in_=ybt[y_][:], identity=identb[:]), r=[("ybt", y_), "identb"], w=["ps_y"])
                later.setdefault(k + 1, []).append(fin2)

        pro(0)
        K_ = len(units)
        for k in range(K_ + 4):
            if k < K_:
                u = units[k]
                if u["first"] and u["t"] + 1 < NT:
                    pro(u["t"] + 1)
                st1(u)
            if 0 <= k - 1 < K_:
                st2(units[k - 1])
            if 0 <= k - 2 < K_:
                st3(units[k - 2], k)
            for fn in later.pop(k, []):
                fn()
        assert not later
        P.op("act", lambda e, hs=hs: e.copy(out=ybTh[hs][:], in_=ps_y[:].rearrange("p t q -> p (t q)")), r=["ps_y"], w=[("ybTh", hs)])
        P.dma(yb_d[h * 128:(h + 1) * 128, :], ybTh[hs][:], r=[("ybTh", hs)])

    for h_ in range(8):
        do_head(h_, h_ % 2)
    return C.finish()


def local_tokens(c):
    b, r = c // 4, c % 4
    idx = np.concatenate([np.arange((4 * ii + r) * 256, (4 * ii + r + 1) * 256) for ii in range(4)])
    return b, idx


_CACHE = {}


def prog(name):
    if name not in _CACHE:
        _CACHE[name] = globals()["build_" + name]()
    return _CACHE[name]


def launch(name, ins):
    res = run_bass_kernel_spmd(prog(name), ins, core_ids=list(range(8)))
    return res.results


IDENT = np.eye(128, dtype=np.float32)


def fmvec(v):
    return np.ascontiguousarray(v.reshape(-1, 128).T)


def moba_consts(r):
    gbias = np.zeros((4, 16), np.float32)
    pflag = np.zeros((4, 16), np.float32)
    for i in range(4):
        for j in range(4):
            for ip in range(4):
                past = (4 * ip + j) < (4 * i + r)
                gbias[i, j * 4 + ip] = 0.0 if past else -1e30
                pflag[i, j * 4 + ip] = 1.0 if past else 0.0
    tm = np.zeros((128, 2, 4, 256), np.float32)
    for qi in range(2):
        for j in range(4):
            if j > r:
                tm[:, qi, j, :] = -30000.0
            elif j == r:
                qpos = qi * 128 + np.arange(128)[:, None]
                tm[:, qi, j, :] = np.where(np.arange(256)[None, :] <= qpos, 0.0, -30000.0)
    return (np.ascontiguousarray(np.broadcast_to(gbias, (128, 4, 16))), np.ascontiguousarray(np.broadcast_to(pflag, (128, 4, 16))), tm)


def even_layer(xl, p):
    ins = [dict(x=xl[c], gT=fmvec(p["norm"]), w=p["w_in"], bgT=fmvec(p["b_glu"]), ident=IDENT) for c in range(8)]
    e1 = launch("E1", ins)
    ins = []
    wdwT = np.ascontiguousarray(p["w_dw"].T.reshape(8, 128, 31).transpose(1, 0, 2))
    vecs = np.ascontiguousarray(np.stack([fmvec(p["b_dw"]), fmvec(p["ln_g"]), fmvec(p["ln_b"]), fmvec(p["b_pw"])], axis=1))
    for c in range(8):
        b, r = c // 4, c % 4
        uh = np.zeros((1024, 4, 32), NPBF)
        for i in range(4):
            gblk = 4 * i + r - 1
            if gblk < 0:
                continue
            src = e1[b * 4 + gblk % 4]["uT"]
            li = gblk // 4
            uh[:, i, :] = src[:, li * 256 + 224: li * 256 + 256]
        ins.append(dict(uT=e1[c]["uT"], uh=uh, gaT=e1[c]["gaT"], wdwT=wdwT, vecs=vecs, wpw=p["w_pw"], ident=IDENT))
    e2a = launch("E2a", ins)
    ins = []
    for c in range(8):
        b, r = c // 4, c % 4
        kT = np.stack([e1[b * 4 + j]["kT"] for j in range(4)])
        vv = np.stack([e1[b * 4 + j]["v"] for j in range(4)])
        gbias, pflag, tm = moba_consts(r)
        ins.append(dict(qT=e1[c]["qT"], kTall=kT, vall=vv, gb=e1[c]["gb"], gbias=gbias, pflag=pflag, tmask=tm, ident=IDENT))
    e2b = launch("E2b", ins)
    ins = [dict(catT=np.concatenate([e2a[c]["yaT"], e2b[c]["ybT"]], axis=0), x=xl[c], w=p["w_out"]) for c in range(8)]
    e2c = launch("E2c", ins)
    return [e2c[c]["xo"] for c in range(8)]


def build_O1(C=None):
    C = C or Ctx()
    P = C.P
    x_d = C.din("x", [TOK, 2048])
    gT_d = C.din("gT", [128, 16])
    w_d = C.din("w", [2048, 2896])
    qnT_d = C.din("qnT", [128, 4])
    rows_d = C.din("rows", [128, 3, 256])
    wqb_d = C.din("wqb", [512, 2048])
    wiq_d = C.din("wiq", [512, 1024])
    wuk_d = C.din("wuk", [16, 128, 256])
    id_d = C.din("ident", [128, 128])
    ckv_o = C.dout("ckv", [1024, 256], BF16)
    ckvT_o = C.dout("ckvT", [256, 1024], BF16)
    ikT_o = C.dout("ikT", [64, 1024], BF16)
    iw_o = C.dout("iw", [1024, 16])
    gT_o = C.dout("gateT", [2048, 1024], BF16)
    ql_o = C.dout("qlatT", [16, 256, 1024], BF16)
    iq_o = C.dout("iqT", [1024, 1024], BF16)
    identf = load_const(C, id_d, [128, 128], F32, "identf")
    gT = load_const(C, gT_d, [128, 16], F32, "gT")
    qnT = load_const(C, qnT_d, [128, 4], F32, "qnT")
    rows = load_const(C, rows_d, [128, 3, 256], F32, "rows")
    identb = C.sb([128, 128], BF16)
    P.op("dve", lambda e: e.tensor_copy(out=identb[:], in_=identf[:]), r=["identf"], w=["identb"])
    xnT = C.sb([128, 16, TOK], BF16)
    norm_transpose(C, x_d, gT, xnT, identb, 16, 2048)
    XN = [("xnT", t) for t in range(NT)]
    w_v = w_d.rearrange("(c p) n -> p c n", p=128)
    wA = C.sb([128, 16, 512], BF16)
    wB = C.sb([128, 16, 336], BF16)
    P.dma(wA[:], w_v[:, :, 0:512], w=["wA"], q="pool")
    P.dma(wB[:], w_v[:, :, 512:848], w=["wB"], q="pool")
    psb = [C.ps([128, 512], F32) for _ in range(4)]
    psT = [C.ps([128, 8, 128], BF16) for _ in range(2)]
    pc = [0]

    def bank():
        b = pc[0] % 4
        pc[0] += 1
        return b
    cqT = C.sb([128, 4, TOK], BF16)
    cqf = C.sb([128, 512], F32)
    cqn = C.sb([128, 512], BF16)
    bf = C.sb([128, 336], F32)
    junk = C.sb([128, 512], F32)
    st = C.sb([128, 8], F32)
    ckvn = C.sb([128, 256], BF16)
    ckvTt = C.sb([128, 2, 128], BF16)
    ikc = C.sb([128, 64], F32)
    ikn = C.sb([128, 64], BF16)
    ikTt = C.sb([64, 128], BF16)
    iws = C.sb([128, 16], F32)
    for t in range(NT):
        tq = slice(t * 128, (t + 1) * 128)
        b = bank()
        for c in range(16):
            P.op("pe", lambda e, c=c, b=b, tq=tq: e.matmul(psb[b][:], lhsT=xnT[:, c, tq], rhs=wA[:, c, :], start=(c == 0), stop=(c == 15)),
                 r=["wA"] + XN, w=[("ps", b)])
        P.op("act", lambda e, b=b: e.copy(out=cqf[:], in_=psb[b][:]), r=[("ps", b)], w=["cqf"])
        P.op("act", lambda e: e.activation(out=junk[:], in_=cqf[:], func=AF.Square, accum_out=st[:, 0:1]), r=["cqf"], w=["junk", "st0"])
        P.op("act", lambda e: e.activation(out=st[:, 0:1], in_=st[:, 0:1], func=AF.Sqrt, scale=1.0 / 512, bias=EPS), r=["st0"], w=["st0"])
        P.op("dve", lambda e: e.reciprocal(out=st[:, 0:1], in_=st[:, 0:1]), r=["st0"], w=["st0"])
        P.op("dve", lambda e: e.tensor_scalar(out=cqn[:], in0=cqf[:], scalar1=st[:, 0:1], scalar2=None, op0=ALU.mult), r=["cqf", "st0"], w=["cqn"])
        for c in range(4):
            P.op("pe", lambda e, c=c: e.transpose(out=psT[0][:, c, :], in_=cqn[:, c * 128:(c + 1) * 128], identity=identb[:]), r=["cqn", "identb"], w=["psT0"])
        for c in range(4):
            P.op("dve", lambda e, c=c, tq=tq: e.tensor_scalar(out=cqT[:, c, tq], in0=psT[0][:, c, :], scalar1=qnT[:, c:c + 1], scalar2=None, op0=ALU.mult),
                 r=["psT0", "qnT"], w=[("cqT", t)])
        b = bank()
        for c in range(16):
            P.op("pe", lambda e, c=c, b=b, tq=tq: e.matmul(psb[b][:, 0:336], lhsT=xnT[:, c, tq], rhs=wB[:, c, :], start=(c == 0), stop=(c == 15)),
                 r=["wB"] + XN, w=[("ps", b)])
        P.op("act", lambda e, b=b: e.copy(out=bf[:], in_=psb[b][:, 0:336]), r=[("ps", b)], w=["bf"])
        P.op("act", lambda e: e.activation(out=junk[:, 0:256], in_=bf[:, 0:256], func=AF.Square, accum_out=st[:, 1:2]), r=["bf"], w=["junk", "st1"])
        P.op("act", lambda e: e.activation(out=st[:, 1:2], in_=st[:, 1:2], func=AF.Sqrt, scale=1.0 / 256, bias=EPS), r=["st1"], w=["st1"])
        P.op("dve", lambda e: e.reciprocal(out=st[:, 1:2], in_=st[:, 1:2]), r=["st1"], w=["st1"])
        P.op("dve", lambda e: e.scalar_tensor_tensor(out=ckvn[:], in0=bf[:, 0:256], scalar=st[:, 1:2], in1=rows[:, 0, :], op0=ALU.mult, op1=ALU.mult),
             r=["bf", "st1", "rows"], w=["ckvn"])
        P.dma(ckv_o[tq, :], ckvn[:], r=["ckvn"], w=[("xch", t, 0)])
        for c in range(2):
            P.op("pe", lambda e, c=c: e.transpose(out=psT[1][:, c, :], in_=ckvn[:, c * 128:(c + 1) * 128], identity=identb[:]), r=["ckvn", "identb"], w=["psT1"])
        P.op("act", lambda e: e.copy(out=ckvTt[:], in_=psT[1][:, 0:2, :]), r=["psT1"], w=["ckvTt"])
        P.dma(ckvT_o[:, tq].rearrange("(c p) t -> p c t", p=128), ckvTt[:], r=["ckvTt"], w=[("xch", t, 1)])
        P.op("dve", lambda e: e.tensor_reduce(out=st[:, 2:3], in_=bf[:, 256:320], axis=AX.X, op=ALU.add), r=["bf"], w=["st2"])
        P.op("dve", lambda e: e.tensor_scalar(out=st[:, 2:3], in0=st[:, 2:3], scalar1=1.0 / 64, scalar2=None, op0=ALU.mult), r=["st2"], w=["st2"])
        P.op("dve", lambda e: e.tensor_scalar(out=ikc[:], in0=bf[:, 256:320], scalar1=st[:, 2:3], scalar2=None, op0=ALU.subtract), r=["bf", "st2"], w=["ikc"])
        P.op("act", lambda e: e.activation(out=junk[:, 0:64], in_=ikc[:], func=AF.Square, accum_out=st[:, 3:4]), r=["ikc"], w=["junk", "st3"])
        P.op("act", lambda e: e.activation(out=st[:, 3:4], in_=st[:, 3:4], func=AF.Sqrt, scale=1.0 / 64, bias=EPS), r=["st3"], w=["st3"])
        P.op("dve", lambda e: e.reciprocal(out=st[:, 3:4], in_=st[:, 3:4]), r=["st3"], w=["st3"])
        P.op("dve", lambda e: e.scalar_tensor_tensor(out=ikc[:], in0=ikc[:], scalar=st[:, 3:4], in1=rows[:, 1, 0:64], op0=ALU.mult, op1=ALU.mult),
             r=["ikc", "st3", "rows"], w=["ikc"])
        P.op("dve", lambda e: e.tensor_tensor(out=ikn[:], in0=ikc[:], in1=rows[:, 2, 0:64], op=ALU.add), r=["ikc", "rows"], w=["ikn"])
        P.op("pe", lambda e: e.transpose(out=psT[1][0:64, 4, :], in_=ikn[:], identity=identb[:]), r=["ikn", "identb"], w=["psT1"])
        P.op("act", lambda e: e.copy(out=ikTt[:], in_=psT[1][0:64, 4, :]), r=["psT1"], w=["ikTt"])
        P.dma(ikT_o[:, tq], ikTt[:], r=["ikTt"], w=[("xch", t, 2)])
        P.op("dve", lambda e: e.tensor_scalar(out=iws[:], in0=bf[:, 320:336], scalar1=1.0 / 32, scalar2=None, op0=ALU.mult), r=["bf"], w=["iws"])
        P.dma(iw_o[tq, :], iws[:], r=["iws"])
    CQ = [("cqT", t) for t in range(NT)]
    if getattr(C, "hook", None):
        C.hook([("xch", t, k) for t in range(NT) for k in range(3)])
        C.hook = None
    wb = [C.sb([128, 16, 512], BF16) for _ in range(2)]
    ob = [C.sb([128, 512], BF16) for _ in range(4)]
    oc = [0]

    def newob():
        o = oc[0] % 4
        oc[0] += 1
        return o
    for g in range(4):
        s = g % 2
        P.dma(wb[s][:], w_v[:, :, 848 + g * 512:848 + (g + 1) * 512], w=[("wb", s)], q="pool")
        for j in range(4):
            ch = g * 4 + j
            for h in range(2):
                b = bank()
                for c in range(16):
                    P.op("pe", lambda e, c=c, b=b, s=s, j=j, h=h: e.matmul(psb[b][:], lhsT=wb[s][:, c, j * 128:(j + 1) * 128], rhs=xnT[:, c, h * 512:(h + 1) * 512],
                                                                        start=(c == 0), stop=(c == 15)),
                         r=[("wb", s)] + XN, w=[("ps", b)])
                o = newob()
                P.op("act", lambda e, b=b, o=o: e.activation(out=ob[o][:], in_=psb[b][:], func=AF.Silu), r=[("ps", b)], w=[("ob", o)])
                P.dma(gT_o[ch * 128:(ch + 1) * 128, h * 512:(h + 1) * 512], ob[o][:], r=[("ob", o)])
    wqb = C.sb([128, 4, 2048], BF16)
    wiq = C.sb([128, 4, 1024], BF16)
    wuk = C.sb([128, 16, 256], BF16)
    P.dma(wqb[:], wqb_d.rearrange("(c p) n -> p c n", p=128), w=["wqb"], q="pool")
    P.dma(wiq[:], wiq_d.rearrange("(c p) n -> p c n", p=128), w=["wiq"], q="pool")
    P.dma(wuk[:], wuk_d.rearrange("h d c -> d h c"), w=["wuk"], q="pool")
    qTh = [C.sb([128, 1024], BF16) for _ in range(2)]
    for h in range(16):
        s = h % 2
        for hf in range(2):
            b = bank()
            for c in range(4):
                P.op("pe", lambda e, c=c, b=b, h=h, hf=hf: e.matmul(psb[b][:], lhsT=wqb[:, c, h * 128:(h + 1) * 128], rhs=cqT[:, c, hf * 512:(hf + 1) * 512],
                                                                 start=(c == 0), stop=(c == 3)),
                     r=["wqb"] + CQ, w=[("ps", b)])
            P.op("act", lambda e, b=b, s=s, hf=hf: e.copy(out=qTh[s][:, hf * 512:(hf + 1) * 512], in_=psb[b][:]), r=[("ps", b)], w=[("qTh", s, hf)])
        for cc in range(2):
            for hf in range(2):
                b = bank()
                P.op("pe", lambda e, b=b, h=h, cc=cc, hf=hf, s=s: e.matmul(psb[b][:], lhsT=wuk[:, h, cc * 128:(cc + 1) * 128], rhs=qTh[s][:, hf * 512:(hf + 1) * 512],
                                                                        start=True, stop=True),
                     r=["wuk", ("qTh", s, hf)], w=[("ps", b)])
                o = newob()
                P.op("act", lambda e, b=b, o=o: e.copy(out=ob[o][:], in_=psb[b][:]), r=[("ps", b)], w=[("ob", o)])
                P.dma(ql_o[h, cc * 128:(cc + 1) * 128, hf * 512:(hf + 1) * 512], ob[o][:], r=[("ob", o)])
    for ch in range(8):
        for hf in range(2):
            b = bank()
            for c in range(4):
                P.op("pe", lambda e, c=c, b=b, ch=ch, hf=hf: e.matmul(psb[b][:], lhsT=wiq[:, c, ch * 128:(ch + 1) * 128], rhs=cqT[:, c, hf * 512:(hf + 1) * 512],
                                                                   start=(c == 0), stop=(c == 3)),
                     r=["wiq"] + CQ, w=[("ps", b)])
            o = newob()
            P.op("act", lambda e, b=b, o=o: e.copy(out=ob[o][:], in_=psb[b][:]), r=[("ps", b)], w=[("ob", o)])
            P.dma(iq_o[ch * 128:(ch + 1) * 128, hf * 512:(hf + 1) * 512], ob[o][:], r=[("ob", o)])
    return C.finish()


def interleave(g1, g2):
    gens = [g for g in (g1, g2) if g is not None]
    while gens:
        for g in list(gens):
            try:
                next(g)
            except StopIteration:
                gens.remove(g)


NIT = 16
BRANGE = 8.0


def build_O2(C=None):
    C = C or Ctx()
    P = C.P
    SC = 128 ** -0.5
    ql_d = C.din("qlatT", [16, 256, 1024], BF16)
    iq_d = C.din("iqT", [1024, 1024], BF16)
    iw_d = C.din("iw", [1024, 16])
    g_d = C.din("gateT", [2048, 1024], BF16)
    ckvT_d = C.din("ckvTall", [4, 256, 1024], BF16)
    ckv_d = C.din("ckvall", [4, 1024, 256], BF16)
    ikT_d = C.din("ikTall", [4, 64, 1024], BF16)
    tmask_d = C.din("tmask", [128, 2, 4, 256])
    wuv_d = C.din("wuv", [16, 256, 128])
    id_d = C.din("ident", [128, 128])
    cat_o = C.dout("catT", [2048, 1024], BF16)
    identf = load_const(C, id_d, [128, 128], F32, "identf")
    tmask = load_const(C, tmask_d, [128, 2, 4, 256], F32, "tmask")
    identb = C.sb([128, 128], BF16)
    onesb = C.sb([128, 128], BF16)
    P.op("dve", lambda e: e.tensor_copy(out=identb[:], in_=identf[:]), r=["identf"], w=["identb"])
    P.op("pool", lambda e: e.memset(onesb[:], 1.0), w=["onesb"])
    ckvT = C.sb([128, 2, 4, 1024], BF16)
    ckva = C.sb([128, 32, 257], BF16)
    ikT2 = C.sb([128, 4, 1024], BF16)
    iqT = C.sb([128, 8, 1024], BF16)
    iwt = C.sb([128, NT, 16], F32)
    wuv = C.sb([128, 16, 2, 128], BF16)
    P.op("pool", lambda e: e.memset(ckva[:], 1.0), w=["ckva"])
    for j in range(4):
        P.dma(ckvT[:, :, j, :], ckvT_d[j].rearrange("(c p) t -> p c t", p=128), w=["ckvT"])
        P.dma(ckva[:, j * 8:(j + 1) * 8, 0:256], ckv_d[j].rearrange("(t p) c -> p t c", p=128), w=["ckva"])
        P.dma(ikT2[0:64, j, :], ikT_d[j], w=["ikT2"])
        P.dma(ikT2[64:128, j, :], ikT_d[j], w=["ikT2"])
    for c in range(8):
        P.dma(iqT[:, c, :], iq_d[c * 128:(c + 1) * 128, :], w=["iqT"])
    P.dma(iwt[:], iw_d.rearrange("(t p) h -> p t h", p=128), w=["iwt"])
    P.dma(wuv[:], wuv_d.rearrange("h (cc c) d -> c h cc d", cc=2), w=["wuv"], q="pool")
    junk = C.sb([128, 4096], BF16)
    kmx = C.sb([128, 16], F32)
    kmax2 = C.sb([128, 1], F32)
    ps_g = C.ps([128, 512], F32)
    ps_s = [C.ps([128, 512], F32) for _ in range(2)]
    ps_t = [C.ps([128, 4, 128], BF16) for _ in range(2)]
    ps_o = C.ps([128, 512], F32)
    ps_i = [C.ps([128, 512], F32) for _ in range(2)]
    for cc in range(2):
        P.op("act", lambda e, cc=cc: e.activation(out=junk[:], in_=ckvT[:, cc, :, :].rearrange("p j t -> p (j t)"), func=AF.Square), r=["ckvT"], w=["junk"])
        for k in range(8):
            P.op("pe", lambda e, k=k: e.matmul(ps_g[:], lhsT=onesb[:], rhs=junk[:, k * 512:(k + 1) * 512], start=True, stop=True), r=["onesb", "junk"], w=["ps_g"])
            P.op("dve", lambda e, k=k, cc=cc: e.tensor_reduce(out=kmx[:, cc * 8 + k:cc * 8 + k + 1], in_=ps_g[:], axis=AX.X, op=ALU.max), r=["ps_g"], w=["kmx"])
    km2 = C.sb([128, 2], F32)
    P.op("dve", lambda e: e.tensor_reduce(out=km2[:], in_=kmx[:].rearrange("p (a b) -> p a b", a=2), axis=AX.X, op=ALU.max), r=["kmx"], w=["km2"])
    P.op("dve", lambda e: e.tensor_reduce(out=kmax2[:], in_=km2[:], axis=AX.X, op=ALU.add), r=["km2"], w=["kmax2"])
    score = [C.sb([128, 4096], F32) for _ in range(2)]
    m01 = [C.sb([128, 4096], BF16) for _ in range(2)]
    tmp = [C.sb([128, 512], F32) for _ in range(3)]
    lo = [C.sb([128, 1], F32) for _ in range(2)]
    mid = [C.sb([128, 1], F32) for _ in range(2)]
    cntt = [C.sb([128, 1], F32) for _ in range(2)]
    ge = [C.sb([128, 1], F32) for _ in range(2)]
    qlt = [C.sb([128, 16, 2, 128], BF16) for _ in range(2)]
    gTt = [C.sb([128, 16, 128], BF16) for _ in range(2)]
    qsq = C.sb([128, 16, 2, 128], BF16)
    mq = [C.sb([128, 16], F32) for _ in range(2)]
    pb = [C.sb([128, 512], BF16) for _ in range(3)]
    pTs = [C.sb([128, 4, 128], BF16) for _ in range(3)]
    on = [C.sb([128, 256], BF16) for _ in range(2)]
    rinv = [C.sb([128, 1], F32) for _ in range(2)]
    olT = [C.sb([128, 2, 128], BF16) for _ in range(2)]
    catt = [C.sb([128, 16, 128], BF16) for _ in range(2)]
    cn = dict(s=0, t=0, o=0, pb=0, pts=0, tmp=0, i=0)

    def chunks_of(i):
        nk = (i + 1) * 256
        return nk, [(j, k0, min(512, nk - k0)) for j in range(4) for k0 in range(0, nk, 512)]

    def stageA(t):
        i, qi = t // 2, t % 2
        par = t % 2
        tq = slice(t * 128, (t + 1) * 128)
        nk, chunks = chunks_of(i)
        SK, MK = ("score", par), ("m01", par)
        P.dma(qlt[par][:], ql_d[:, :, tq].rearrange("h (cc c) q -> c h cc q", cc=2), w=[("qlt", par)])
        P.dma(gTt[par][:], g_d[:, tq].rearrange("(h p) q -> p h q", p=128), w=[("gTt", par)])
        for (j, k0, n) in chunks:
            for hh in range(16):
                sb_ = cn["i"] % 2
                cn["i"] += 1
                pr = slice((hh % 2) * 64, (hh % 2) * 64 + 64)
                P.op("pe", lambda e, sb_=sb_, hh=hh, pr=pr, j=j, k0=k0, n=n, tq=tq: e.matmul(ps_i[sb_][:, 0:n], lhsT=iqT[pr, hh // 2, tq], rhs=ikT2[pr, j, k0:k0 + n],
                                                                                      start=True, stop=True),
                     r=["iqT", "ikT2"], w=[("ps_i", sb_)])
                dst = score[par][:, j * nk + k0:j * nk + k0 + n]
                if hh == 0:
                    P.op("dve", lambda e, sb_=sb_, n=n, dst=dst, t=t, hh=hh: e.tensor_scalar(out=dst, in0=ps_i[sb_][:, 0:n], scalar1=0.0, scalar2=iwt[:, t, hh:hh + 1],
                                                                                      op0=ALU.max, op1=ALU.mult),
                         r=[("ps_i", sb_), "iwt"], w=[SK])
                else:
                    k2 = cn["tmp"] % 3
                    cn["tmp"] += 1
                    P.op("dve", lambda e, sb_=sb_, n=n, k2=k2, t=t, hh=hh: e.tensor_scalar(out=tmp[k2][:, 0:n], in0=ps_i[sb_][:, 0:n], scalar1=0.0, scalar2=iwt[:, t, hh:hh + 1],
                                                                                    op0=ALU.max, op1=ALU.mult),
                         r=[("ps_i", sb_), "iwt"], w=[("tmp", k2)])
                    P.op("pool", lambda e, dst=dst, k2=k2, n=n: e.tensor_tensor(out=dst, in0=dst, in1=tmp[k2][:, 0:n], op=ALU.add), r=[("tmp", k2), SK], w=[SK])
                yield
        for j in range(4):
            dst = score[par][:, j * nk + i * 256:j * nk + (i + 1) * 256]
            P.op("dve", lambda e, dst=dst, j=j, qi=qi: e.tensor_tensor(out=dst, in0=dst, in1=tmask[:, qi, j, :], op=ALU.add), r=[SK, "tmask"], w=[SK])
        sc = score[par][:, 0:4 * nk]
        P.op("dve", lambda e: e.tensor_reduce(out=lo[par][:], in_=sc, axis=AX.X, op=ALU.max), r=[SK], w=[("lo", par)])
        P.op("dve", lambda e: e.tensor_scalar(out=lo[par][:], in0=lo[par][:], scalar1=-BRANGE, scalar2=None, op0=ALU.add), r=[("lo", par)], w=[("lo", par)])
        yield
        for it in range(NIT):
            step = BRANGE / 2 ** (it + 1)
            P.op("dve", lambda e, step=step: e.tensor_scalar(out=mid[par][:], in0=lo[par][:], scalar1=step, scalar2=None, op0=ALU.add), r=[("lo", par)], w=[("mid", par)])
            P.op("dve", lambda e: e.tensor_scalar(out=junk[:, 0:4 * nk], in0=sc, scalar1=mid[par][:, 0:1], scalar2=None, op0=ALU.is_ge, op1=ALU.add, accum_out=cntt[par][:]),
                 r=[SK, ("mid", par)], w=[("cntt", par)])
            P.op("dve", lambda e, step=step: e.tensor_scalar(out=ge[par][:], in0=cntt[par][:], scalar1=255.5, scalar2=step, op0=ALU.is_ge, op1=ALU.mult),
                 r=[("cntt", par)], w=[("ge", par)])
            P.op("dve", lambda e: e.tensor_tensor(out=lo[par][:], in0=lo[par][:], in1=ge[par][:], op=ALU.add), r=[("lo", par), ("ge", par)], w=[("lo", par)])
            yield 0.5
        P.op("dve", lambda e: e.tensor_scalar(out=m01[par][:, 0:4 * nk], in0=sc, scalar1=lo[par][:, 0:1], scalar2=None, op0=ALU.is_ge), r=[SK, ("lo", par)], w=[MK])
        P.op("act", lambda e: e.activation(out=qsq[:], in_=qlt[par][:], func=AF.Square), r=[("qlt", par)], w=["qsq"])
        for hh in range(16):
            for cc in range(2):
                P.op("pe", lambda e, hh=hh, cc=cc: e.matmul(ps_g[:, hh:hh + 1], lhsT=qsq[:, hh, cc, :], rhs=onesb[:, 0:1], start=(cc == 0), stop=(cc == 1)),
                     r=["qsq", "onesb"], w=["ps_g"])
        P.op("dve", lambda e: e.tensor_scalar(out=mq[par][:], in0=ps_g[:, 0:16], scalar1=kmax2[:, 0:1], scalar2=None, op0=ALU.mult), r=["ps_g", "kmax2"], w=[("mq", par)])
        P.op("act", lambda e: e.activation(out=mq[par][:], in_=mq[par][:], func=AF.Sqrt, scale=SC * SC), r=[("mq", par)], w=[("mq", par)])
        P.op("dve", lambda e: e.tensor_scalar(out=mq[par][:], in0=mq[par][:], scalar1=-1.0, scalar2=None, op0=ALU.mult), r=[("mq", par)], w=[("mq", par)])
        yield

    def stageB(t):
        i = t // 2
        par = t % 2
        tq = slice(t * 128, (t + 1) * 128)
        nk, chunks = chunks_of(i)
        MK = ("m01", par)
        nmm = sum(n // 128 for (_, _, n) in chunks)
        units = []
        for hh in range(16):
            mi = 0
            for ci, (j, k0, n) in enumerate(chunks):
                units.append(dict(hh=hh, j=j, k0=k0, n=n, last=(ci == len(chunks) - 1), mi=mi))
                mi += n // 128
        later = {}

        def st1(u):
            hh, j, k0, n = u["hh"], u["j"], u["k0"], u["n"]
            sb_ = cn["s"] % 2
            cn["s"] += 1
            p_ = cn["pb"] % 3
            cn["pb"] += 1
            u["p"] = p_
            for cc in range(2):
                P.op("pe", lambda e, cc=cc: e.matmul(ps_s[sb_][:, 0:n], lhsT=qlt[par][:, hh, cc, :], rhs=ckvT[:, cc, j, k0:k0 + n], start=(cc == 0), stop=(cc == 1)),
                     r=[("qlt", par), "ckvT"], w=[("ps_s", sb_)])
            P.op("act", lambda e: e.activation(out=pb[p_][:, 0:n], in_=ps_s[sb_][:, 0:n], func=AF.Exp, scale=SC, bias=mq[par][:, hh:hh + 1]),
                 r=[("ps_s", sb_), ("mq", par)], w=[("pb", p_)])
            P.op("pool", lambda e: e.tensor_tensor(out=pb[p_][:, 0:n], in0=pb[p_][:, 0:n], in1=m01[par][:, j * nk + k0:j * nk + k0 + n], op=ALU.mult),
                 r=[("pb", p_), MK], w=[("pb", p_)])

        def st2(u):
            n, p_ = u["n"], u["p"]
            tb_ = cn["t"] % 2
            cn["t"] += 1
            q_ = cn["pts"] % 3
            cn["pts"] += 1
            u["q"] = q_
            for a in range(n // 128):
                P.op("pe", lambda e, a=a: e.transpose(out=ps_t[tb_][:, a, :], in_=pb[p_][:, a * 128:(a + 1) * 128], identity=identb[:]),
                     r=[("pb", p_), "identb"], w=[("ps_t", tb_)])
            P.op("act", lambda e: e.copy(out=pTs[q_][:, 0:n // 128, :], in_=ps_t[tb_][:, 0:n // 128, :]), r=[("ps_t", tb_)], w=[("pTs", q_)])

        def st3(u, k):
            hh, j, k0, n, q_ = u["hh"], u["j"], u["k0"], u["n"], u["q"]
            for a in range(n // 128):
                kt = j * 8 + k0 // 128 + a
                mi = u["mi"] + a
                P.op("pe", lambda e, a=a, kt=kt, mi=mi: e.matmul(ps_o[:, 0:257], lhsT=pTs[q_][:, a, :], rhs=ckva[:, kt, :], start=(mi == 0), stop=(mi == nmm - 1)),
                     r=[("pTs", q_), "ckva"], w=["ps_o"])
            if u["last"]:
                z = hh % 2
                P.op("dve", lambda e: e.reciprocal(out=rinv[z][:], in_=ps_o[:, 256:257]), r=["ps_o"], w=[("rinv", z)])
                P.op("dve", lambda e: e.tensor_scalar(out=on[z][:], in0=ps_o[:, 0:256], scalar1=rinv[z][:, 0:1], scalar2=None, op0=ALU.mult),
                     r=["ps_o", ("rinv", z)], w=[("on", z)])

                def fin2():
                    tb_ = cn["t"] % 2
                    cn["t"] += 1
                    for cc in range(2):
                        P.op("pe", lambda e, cc=cc: e.transpose(out=ps_t[tb_][:, cc, :], in_=on[z][:, cc * 128:(cc + 1) * 128], identity=identb[:]),
                             r=[("on", z), "identb"], w=[("ps_t", tb_)])
                    P.op("act", lambda e: e.copy(out=olT[z][:], in_=ps_t[tb_][:, 0:2, :]), r=[("ps_t", tb_)], w=[("olT", z)])

                def fin3():
                    for cc in range(2):
                        P.op("pe", lambda e, cc=cc: e.matmul(ps_g[:, 128:256], lhsT=wuv[:, hh, cc, :], rhs=olT[z][:, cc, :], start=(cc == 0), stop=(cc == 1)),
                             r=[("olT", z), "wuv"], w=["ps_g"])
                    P.op("dve", lambda e: e.tensor_tensor(out=catt[par][:, hh, :], in0=ps_g[:, 128:256], in1=gTt[par][:, hh, :], op=ALU.mult),
                         r=["ps_g", ("gTt", par)], w=[("catt", par)])
                later.setdefault(k + 1, []).append(fin2)
                later.setdefault(k + 2, []).append(fin3)

        K_ = len(units)
        for k in range(K_ + 5):
            if k < K_:
                st1(units[k])
            if 0 <= k - 1 < K_:
                st2(units[k - 1])
            if 0 <= k - 2 < K_:
                st3(units[k - 2], k)
            for fn in later.pop(k, []):
                fn()
            yield
        assert not later
        P.dma(cat_o[:, tq].rearrange("(h p) q -> p h q", p=128), catt[par][:], r=[("catt", par)])
        yield

    interleave(stageA(0), None)
    for t in range(NT):
        interleave(stageB(t), stageA(t + 1) if t + 1 < NT else None)
    return C.finish()


def build_F(C=None):
    C = C or Ctx()
    P = C.P
    x_d = C.din("x", [TOK, 2048])
    g_d = C.din("grow", [128, 2048])
    o_d = C.dout("y", [TOK, 2048])
    grow = load_const(C, g_d, [128, 2048], F32, "grow")
    xt = [C.sb([128, 2048], F32) for _ in range(2)]
    yo = [C.sb([128, 2048], F32) for _ in range(2)]
    junk = C.sb([128, 2048], BF16)
    ss = C.sb([128, NT], F32)
    for t in range(NT):
        s = t % 2
        P.dma(xt[s][:], x_d[t * 128:(t + 1) * 128, :], w=[("xt", s)])
        P.op("act", lambda e, s=s, t=t: e.activation(out=junk[:], in_=xt[s][:], func=AF.Square, accum_out=ss[:, t:t + 1]), r=[("xt", s)], w=["junk", ("ss", t)])
        P.op("act", lambda e, t=t: e.activation(out=ss[:, t:t + 1], in_=ss[:, t:t + 1], func=AF.Sqrt, scale=1.0 / 2048, bias=EPS), r=[("ss", t)], w=[("ss", t)])
        P.op("dve", lambda e, t=t: e.reciprocal(out=ss[:, t:t + 1], in_=ss[:, t:t + 1]), r=[("ss", t)], w=[("ss", t)])
        P.op("dve", lambda e, s=s, t=t: e.scalar_tensor_tensor(out=yo[s][:], in0=xt[s][:], scalar=ss[:, t:t + 1], in1=grow[:], op0=ALU.mult, op1=ALU.mult),
             r=[("xt", s), ("ss", t), "grow"], w=[("yo", s)])
        P.dma(o_d[t * 128:(t + 1) * 128, :], yo[s][:], r=[("yo", s)])
    return C.finish()


def odd_layer(xl, p):
    rows = np.zeros((128, 3, 256), np.float32)
    rows[:, 0, :] = p["kv_norm"][None, :]
    rows[:, 1, :64] = p["ik_g"][None, :]
    rows[:, 2, :64] = p["ik_b"][None, :]
    ins = [dict(x=xl[c], gT=fmvec(p["norm"]), w=p["w_in"], qnT=fmvec(p["q_norm"]), rows=rows, wqb=p["w_qb"], wiq=p["w_iq"], wuk=p["w_uk"], ident=IDENT)
           for c in range(8)]
    o1 = launch("O1", ins)
    ins = []
    for c in range(8):
        b, r = c // 4, c % 4
        _, _, tm = moba_consts(r)
        ins.append(dict(qlatT=o1[c]["qlatT"], iqT=o1[c]["iqT"], iw=o1[c]["iw"], gateT=o1[c]["gateT"],
                        ckvTall=np.stack([o1[b * 4 + j]["ckvT"] for j in range(4)]), ckvall=np.stack([o1[b * 4 + j]["ckv"] for j in range(4)]),
                        ikTall=np.stack([o1[b * 4 + j]["ikT"] for j in range(4)]), tmask=tm, wuv=p["w_uv"], ident=IDENT))
    o2 = launch("O2", ins)
    ins = [dict(catT=o2[c]["catT"], x=xl[c], w=p["w_out"]) for c in range(8)]
    e2c = launch("E2c", ins)
    return [e2c[c]["xo"] for c in range(8)]


def kernel_unfused(**z):
    x = np.asarray(z["x"], np.float32)
    xl = []
    for c in range(8):
        b, idx = local_tokens(c)
        xl.append(np.ascontiguousarray(x[b, idx]))
    for layer in range(4):
        i = layer // 2
        if layer % 2 == 0:
            p = dict(norm=z["even_norm"][i], w_in=z["even_w_in"][i], b_glu=z["even_b_glu"][i], w_dw=z["even_w_dw"][i], b_dw=z["even_b_dw"][i],
                     ln_g=z["even_conv_ln_g"][i], ln_b=z["even_conv_ln_b"][i], w_pw=z["even_w_pw"][i], b_pw=z["even_b_pw"][i], w_out=z["even_w_out"][i])
            p = {k: np.ascontiguousarray(np.asarray(v, np.float32)) for k, v in p.items()}
            xl = even_layer(xl, p)
        else:
            p = dict(norm=z["odd_norm"][i], w_in=z["odd_w_in"][i], q_norm=z["odd_q_norm"][i], w_qb=z["odd_w_qb"][i], kv_norm=z["odd_kv_norm"][i],
                     w_uk=z["odd_w_uk"][i], w_uv=z["odd_w_uv"][i], w_iq=z["odd_w_iq"][i], ik_g=z["odd_ik_ln_g"][i], ik_b=z["odd_ik_ln_b"][i], w_out=z["odd_w_out"][i])
            p = {k: np.ascontiguousarray(np.asarray(v, np.float32)) for k, v in p.items()}
            xl = odd_layer(xl, p)
    grow = np.ascontiguousarray(np.broadcast_to(np.asarray(z["final_norm"], np.float32)[None, :], (128, 2048)))
    f = launch("F", [dict(x=xl[c], grow=grow) for c in range(8)])
    out = np.zeros_like(x)
    for c in range(8):
        b, idx = local_tokens(c)
        out[b, idx] = f[c]["y"]
    return out


RG = [[0, 1, 2, 3], [4, 5, 6, 7]]


def build_HALO(C):
    P = C.P
    uall = C.din("uTall", [4, 1024, 1024], BF16)
    selw_d = C.din("selw", [128, 5])
    uh_o = C.dout("uh", [1024, 4, 32], BF16)
    selw = load_const(C, selw_d, [128, 5], F32, "selw")
    H = C.sb([128, 4, 8, 4, 32], BF16)
    for j in range(4):
        for c in range(8):
            P.dma(H[:, j, c, :, :], uall.rows(j, c * 128, (c + 1) * 128).rearrange("p (i t) -> p i t", i=4)[:, :, 224:256], w=["H"])
    acc = C.sb([128, 8, 4, 32], F32)
    ob = C.sb([128, 8, 4, 32], BF16)
    P.op("dve", lambda e: e.tensor_scalar(out=acc[:], in0=H[:, 0], scalar1=selw[:, 0:1], scalar2=None, op0=ALU.mult), r=["H", "selw"], w=["acc"])
    for j in range(1, 4):
        P.op("dve", lambda e, j=j: e.scalar_tensor_tensor(out=acc[:], in0=H[:, j], scalar=selw[:, j:j + 1], in1=acc[:], op0=ALU.mult, op1=ALU.add),
             r=["H", "selw", "acc"], w=["acc"])
    for c in range(8):
        P.op("dve", lambda e, c=c: e.scalar_tensor_tensor(out=acc[:, c, 1:4, :], in0=H[:, 3, c, 0:3, :], scalar=selw[:, 4:5], in1=acc[:, c, 1:4, :],
                                                       op0=ALU.mult, op1=ALU.add),
             r=["H", "selw", "acc"], w=["acc"])
    P.op("dve", lambda e: e.tensor_copy(out=ob[:], in_=acc[:]), r=["acc"], w=["ob"])
    P.dma(uh_o.rearrange("(c p) i t -> p c i t", p=128), ob[:], r=["ob"])
    return C.finish()


def allgather(C, src, dst, r=()):
    C.P.op("pool", lambda e: e.collective_compute("AllGather", ALU.bypass, replica_groups=RG, ins=[src.opt()], outs=[dst.opt()]), r=r, cc=True)


def allgather2(C, src, dsts):
    for h in range(2):
        allgather(C, src[h * 512:(h + 1) * 512, :], dsts[h])
    return Gath(dsts, 512)


EVEN_W = dict(gT=([128, 16], F32), w_in=([2048, 7168], F32), bgT=([128, 16], F32), wdwT=([128, 8, 31], F32), vecs=([128, 4, 8], F32),
              w_pw=([1024, 1024], F32), w_out=([2048, 2048], F32))
ODD_W = dict(gT=([128, 16], F32), w_in=([2048, 2896], F32), qnT=([128, 4], F32), rows=([128, 3, 256], F32), w_qb=([512, 2048], F32),
             w_iq=([512, 1024], F32), w_uk=([16, 128, 256], F32), w_uv=([16, 256, 128], F32), w_out=([2048, 2048], F32))


def build_FUSED(stop=None):
    C = Ctx(fused=True)
    ph = [0]

    def done():
        ph[0] += 1
        return stop is not None and ph[0] >= stop
    x_in = C.ext_in("x", [TOK, 2048])
    ident = C.ext_in("ident", [128, 128])
    gbias = C.ext_in("gbias", [128, 4, 16])
    pflag = C.ext_in("pflag", [128, 4, 16])
    tmask = C.ext_in("tmask", [128, 2, 4, 256])
    selw = C.ext_in("selw", [128, 5])
    grow = C.ext_in("grow", [128, 2048])
    y_out = C.ext_out("y", [TOK, 2048])
    I = C.internal
    xa, xb = I("xa", [TOK, 2048]), I("xb", [TOK, 2048])
    uT, gaT, qT, kT, v, gb = (I(n, [1024, 1024], BF16) for n in ("uT_i", "gaT_i", "qT_i", "kT_i", "v_i", "gb_i"))
    kTall, vall, uTall = ([I(n + str(h), [2048, 1024], BF16) for h in range(2)] for n in ("kTall_i", "vall_i", "uTall_i"))
    uh = I("uh_i", [1024, 4, 32], BF16)
    catT = I("catT_i", [2048, 1024], BF16)
    ckv, ckvT, ikT, iw = I("ckv_i", [1024, 256], BF16), I("ckvT_i", [256, 1024], BF16), I("ikT_i", [64, 1024], BF16), I("iw_i", [1024, 16])
    gateT, qlatT, iqT = I("gateT_i", [2048, 1024], BF16), I("qlatT_i", [16, 256, 1024], BF16), I("iqT_i", [1024, 1024], BF16)
    ckvall, ckvTall, ikTall = I("ckvall_i", [4096, 256], BF16), I("ckvTall_i", [1024, 1024], BF16), I("ikTall_i", [256, 1024], BF16)
    cur = x_in
    for layer in range(4):
        nxt = xa if layer % 2 == 0 else xb
        L = "L%d_" % layer
        if layer % 2 == 0:
            W = {k: C.ext_in(L + k, s, d) for k, (s, d) in EVEN_W.items()}
            C.io = dict(x=cur, gT=W["gT"], w=W["w_in"], bgT=W["bgT"], ident=ident, uT=uT, gaT=gaT, qT=qT, kT=kT, v=v, gb=gb)
            build_E1(C)
            if done():
                break
            uT_g = allgather2(C, uT, uTall)
            C.P.barrier()
            if done():
                break
            C.io = dict(uTall=uT_g, selw=selw, uh=uh)
            build_HALO(C)
            if done():
                break
            kT_g = allgather2(C, kT, kTall)
            v_g = allgather2(C, v, vall)
            C.io = dict(uT=uT, uh=uh, gaT=gaT, wdwT=W["wdwT"], vecs=W["vecs"], wpw=W["w_pw"], ident=ident, yaT=catT[0:1024, :])
            build_E2a(C)
            if done():
                break
            C.io = dict(qT=qT, kTall=kT_g, vall=v_g, gb=gb,
                        gbias=gbias, pflag=pflag, tmask=tmask, ident=ident, ybT=catT[1024:2048, :])
            build_E2b(C)
            if done():
                break
        else:
            W = {k: C.ext_in(L + k, s, d) for k, (s, d) in ODD_W.items()}
            C.io = dict(x=cur, gT=W["gT"], w=W["w_in"], qnT=W["qnT"], rows=W["rows"], wqb=W["w_qb"], wiq=W["w_iq"], wuk=W["w_uk"], ident=ident,
                        ckv=ckv, ckvT=ckvT, ikT=ikT, iw=iw, gateT=gateT, qlatT=qlatT, iqT=iqT)
            def hook(keys):
                allgather(C, ckv, ckvall, r=keys)
                allgather(C, ckvT, ckvTall, r=keys)
                allgather(C, ikT, ikTall, r=keys)
            C.hook = hook
            build_O1(C)
            if done():
                break
            C.io = dict(qlatT=qlatT, iqT=iqT, iw=iw, gateT=gateT, ckvTall=ckvTall.rearrange("(j r) c -> j r c", j=4),
                        ckvall=ckvall.rearrange("(j r) c -> j r c", j=4), ikTall=ikTall.rearrange("(j r) c -> j r c", j=4),
                        tmask=tmask, wuv=W["w_uv"], ident=ident, catT=catT)
            build_O2(C)
            if done():
                break
        C.io = dict(catT=catT, x=cur, w=W["w_out"], xo=nxt)
        build_E2c(C)
        if done():
            break
        cur = nxt
    C.io = dict(x=cur, grow=grow, y=y_out)
    if stop is None:
        build_F(C)
    else:
        C.P.barrier()
    C.P.emit()
    C.es.close()
    return C.nc


def kernel(**z):
    x = np.asarray(z["x"], np.float32)
    f32 = lambda a: np.ascontiguousarray(np.asarray(a, np.float32))
    common = dict(ident=IDENT, grow=np.ascontiguousarray(np.broadcast_to(f32(z["final_norm"])[None, :], (128, 2048))))
    for layer in range(4):
        i = layer // 2
        L = "L%d_" % layer
        if layer % 2 == 0:
            common[L + "gT"] = fmvec(f32(z["even_norm"][i]))
            common[L + "w_in"] = f32(z["even_w_in"][i])
            common[L + "bgT"] = fmvec(f32(z["even_b_glu"][i]))
            common[L + "wdwT"] = np.ascontiguousarray(f32(z["even_w_dw"][i]).T.reshape(8, 128, 31).transpose(1, 0, 2))
            common[L + "vecs"] = np.ascontiguousarray(np.stack([fmvec(f32(z["even_b_dw"][i])), fmvec(f32(z["even_conv_ln_g"][i])),
                                                                fmvec(f32(z["even_conv_ln_b"][i])), fmvec(f32(z["even_b_pw"][i]))], axis=1))
            common[L + "w_pw"] = f32(z["even_w_pw"][i])
            common[L + "w_out"] = f32(z["even_w_out"][i])
        else:
            rows = np.zeros((128, 3, 256), np.float32)
            rows[:, 0, :] = f32(z["odd_kv_norm"][i])[None, :]
            rows[:, 1, :64] = f32(z["odd_ik_ln_g"][i])[None, :]
            rows[:, 2, :64] = f32(z["odd_ik_ln_b"][i])[None, :]
            common[L + "gT"] = fmvec(f32(z["odd_norm"][i]))
            common[L + "w_in"] = f32(z["odd_w_in"][i])
            common[L + "qnT"] = fmvec(f32(z["odd_q_norm"][i]))
            common[L + "rows"] = rows
            common[L + "w_qb"] = f32(z["odd_w_qb"][i])
            common[L + "w_iq"] = f32(z["odd_w_iq"][i])
            common[L + "w_uk"] = f32(z["odd_w_uk"][i])
            common[L + "w_uv"] = f32(z["odd_w_uv"][i])
            common[L + "w_out"] = f32(z["odd_w_out"][i])
    ins = []
    for c in range(8):
        b, idx = local_tokens(c)
        r = c % 4
        gbias, pflag, tm = moba_consts(r)
        selw = np.zeros((128, 5), np.float32)
        if r >= 1:
            selw[:, r - 1] = 1.0
        else:
            selw[:, 4] = 1.0
        d = dict(common)
        d.update(x=np.ascontiguousarray(x[b, idx]), gbias=gbias, pflag=pflag, tmask=tm, selw=selw)
        ins.append(d)
    if z.get("_ins_only"):
        return ins
    res = launch("FUSED", ins)
    out = np.zeros_like(x)
    for c in range(8):
        b, idx = local_tokens(c)
        out[b, idx] = res[c]["y"]
    return out
```
